# Optimizing a Trainium2 kernel written in Bass

```python
import math
import jax, jax.numpy as jnp
from jax import lax
import numpy as np

D_MODEL = 2048
BATCH = 2
SEQ = 8192
DEPTH = 1

ATTN_HEADS = 8
ATTN_QK_DIM = 64
ATTN_V_DIM = 128
HGRN_HEADS = 8
HGRN_K_DIM = 128
HGRN_V_DIM = 128
ATTN_WIDTH = ATTN_HEADS * ATTN_V_DIM
HGRN_WIDTH = HGRN_HEADS * HGRN_V_DIM
MIX_WIDTH = ATTN_WIDTH + HGRN_WIDTH
IN_SIZES = (ATTN_HEADS * 2 * ATTN_QK_DIM, ATTN_HEADS * 2 * ATTN_QK_DIM, ATTN_WIDTH,
            HGRN_HEADS * HGRN_K_DIM, HGRN_WIDTH, HGRN_HEADS * HGRN_K_DIM,
            HGRN_HEADS * HGRN_K_DIM, HGRN_WIDTH)
IN_WIDTH = sum(IN_SIZES)
Q_BLOCK = 128
HGRN_CHUNK = 64
REL_BUCKETS = 32
REL_MAX_DIST = 128
N_EXPERTS = 256
TOP_K = 8
N_EXPERT_GROUPS = 8
TOPK_EXPERT_GROUPS = 4
EXPERT_DIM = 512
ROUTED_SCALE = 2.5
MOE_BLOCK = 128
ADA_SCALE = 0.2
EPS = 1e-6

kernel_name = 'hybrid_diffattn_hgrn2_moe_encoder'


def _rmsnorm(x, g):
    xf = x.astype(jnp.float32)
    y = xf * lax.rsqrt(jnp.mean(xf * xf, axis=-1, keepdims=True) + EPS)
    return (y * g.astype(jnp.float32)).astype(x.dtype)


def _t5_bucket(rel):
    half = REL_BUCKETS // 2
    max_exact = half // 2
    ret = jnp.where(rel > 0, half, 0)
    n = jnp.abs(rel)
    nf = jnp.maximum(n, 1).astype(jnp.float32)
    large = max_exact + (jnp.log(nf / max_exact) / math.log(REL_MAX_DIST / max_exact)
                         * (half - max_exact)).astype(jnp.int32)
    large = jnp.minimum(large, half - 1)
    return ret + jnp.where(n < max_exact, n, large)


def _diff_attention(q1, q2, k1, k2, v, pos, rel_bias, lam):
    B, H, S, _ = q1.shape
    nq = S // Q_BLOCK
    scale = ATTN_QK_DIM ** -0.5

    def blocks(t):
        return jnp.moveaxis(t.reshape(B, H, nq, Q_BLOCK, t.shape[-1]), 2, 0)

    pos_blocks = jnp.moveaxis(pos.reshape(B, nq, Q_BLOCK), 1, 0)

    def one_block(args):
        a1, a2, pq = args
        rel = pos[:, None, :] - pq[:, :, None]
        bias = jnp.moveaxis(rel_bias[_t5_bucket(rel)], -1, 1).astype(jnp.float32)
        s1 = jnp.einsum('bhqd,bhkd->bhqk', a1, k1).astype(jnp.float32) * scale + bias
        s2 = jnp.einsum('bhqd,bhkd->bhqk', a2, k2).astype(jnp.float32) * scale + bias
        p = jax.nn.softmax(s1, axis=-1) - lam * jax.nn.softmax(s2, axis=-1)
        return jnp.einsum('bhqk,bhkd->bhqd', p.astype(v.dtype), v)

    out = lax.map(one_block, (blocks(q1), blocks(q2), pos_blocks))
    return jnp.moveaxis(out, 0, 2).reshape(B, H, S, v.shape[-1])


def _hgrn2_scan(q, k, v, logf):
    B, H, S, dk = q.shape
    dv = v.shape[-1]
    nc = S // HGRN_CHUNK

    def chunks(t):
        return jnp.moveaxis(t.reshape(B, H, nc, HGRN_CHUNK, t.shape[-1]), 2, 0)

    mask = jnp.tril(jnp.ones((HGRN_CHUNK, HGRN_CHUNK), bool))

    def step(state, inp):
        qc, kc, vc, gc = inp
        b = jnp.cumsum(gc, axis=2)
        o_inter = jnp.einsum('bhtk,bhkv->bhtv', qc * jnp.exp(b), state)
        diff = b[:, :, :, None, :] - b[:, :, None, :, :]
        decay = jnp.exp(jnp.where(mask[:, :, None], diff, -jnp.inf))
        scores = jnp.einsum('bhtk,bhsk,bhtsk->bhts', qc, kc, decay)
        o_intra = jnp.einsum('bhts,bhsv->bhtv', scores, vc)
        b_last = b[:, :, -1:, :]
        state = (jnp.exp(b_last[:, :, 0, :, None]) * state
                 + jnp.einsum('bhsk,bhsv->bhkv', kc * jnp.exp(b_last - b), vc))
        return state, o_inter + o_intra

    s0 = jnp.zeros((B, H, dk, dv), q.dtype)
    _, o = lax.scan(step, s0, (chunks(q), chunks(k), chunks(v), chunks(logf)))
    return jnp.moveaxis(o, 0, 2).reshape(B, H, S, dv)


def _hgrn2_mixer(q, i, z_fwd, z_bwd, g, lb, g_norm):
    B, S = q.shape[:2]

    def heads(t, d):
        return jnp.transpose(t.reshape(B, S, HGRN_HEADS, d).astype(jnp.float32), (0, 2, 1, 3))

    qh = jax.nn.silu(heads(q, HGRN_K_DIM))
    vh = heads(i, HGRN_V_DIM)

    def gates(z, lb_dir):
        lbh = lb_dir.astype(jnp.float32).reshape(HGRN_HEADS, 1, HGRN_K_DIM)
        f = lbh + (1.0 - lbh) * jax.nn.sigmoid(heads(z, HGRN_K_DIM))
        return 1.0 - f, jnp.log(f)

    k_f, logf_f = gates(z_fwd, lb[0])
    k_b, logf_b = gates(z_bwd, lb[1])
    o_f = _hgrn2_scan(qh, k_f, vh, logf_f)
    flip = lambda t: jnp.flip(t, axis=2)
    o_b = flip(_hgrn2_scan(flip(qh), flip(k_b), flip(vh), flip(logf_b)))
    o = jnp.transpose(o_f + o_b, (0, 2, 1, 3))
    o = _rmsnorm(o, g_norm) * jax.nn.silu(g.reshape(B, S, HGRN_HEADS, HGRN_V_DIM).astype(jnp.float32))
    return o.reshape(B, S, HGRN_WIDTH)


def _swiglu(x, wg, wu, wd):
    return (jax.nn.silu(x @ wg) * (x @ wu)) @ wd


def _moe(h, w_router, router_bias, w_gate, w_up, w_down, ws_gate, ws_up, ws_down):
    B, S, D = h.shape
    N = B * S
    hf = h.reshape(N, D)
    scores = jax.nn.sigmoid((hf @ w_router).astype(jnp.float32))
    biased = scores + router_bias.astype(jnp.float32)
    per_group = N_EXPERTS // N_EXPERT_GROUPS
    group_score = lax.top_k(biased.reshape(N, N_EXPERT_GROUPS, per_group), 2)[0].sum(-1)
    _, gidx = lax.top_k(group_score, TOPK_EXPERT_GROUPS)
    gmask = jnp.any(gidx[:, :, None] == jnp.arange(N_EXPERT_GROUPS)[None, None, :], axis=1)
    emask = jnp.repeat(gmask, per_group, axis=1)
    _, eidx = lax.top_k(jnp.where(emask, biased, -jnp.inf), TOP_K)
    gate = jnp.take_along_axis(scores, eidx, axis=1)
    gate = gate / jnp.sum(gate, axis=-1, keepdims=True) * ROUTED_SCALE

    flat_e = eidx.reshape(-1).astype(jnp.int32)
    flat_tok = jnp.repeat(jnp.arange(N, dtype=jnp.int32), TOP_K)
    flat_w = gate.reshape(-1)
    order = jnp.argsort(flat_e)
    se = flat_e[order]
    counts = jnp.bincount(flat_e, length=N_EXPERTS)
    starts = jnp.cumsum(counts) - counts
    padded = (counts + MOE_BLOCK - 1) // MOE_BLOCK * MOE_BLOCK
    pends = jnp.cumsum(padded)
    pstarts = pends - padded
    dest = pstarts[se] + jnp.arange(N * TOP_K, dtype=pstarts.dtype) - starts[se]
    n_rows = N * TOP_K + N_EXPERTS * MOE_BLOCK
    n_blocks = n_rows // MOE_BLOCK
    row_tok = jnp.zeros((n_rows,), jnp.int32).at[dest].set(flat_tok[order])
    row_w = jnp.zeros((n_rows,), jnp.float32).at[dest].set(flat_w[order])
    block_start = jnp.arange(n_blocks, dtype=pends.dtype) * MOE_BLOCK
    block_e = jnp.minimum(jnp.searchsorted(pends, block_start, side='right'), N_EXPERTS - 1)

    def body(acc, blk):
        toks, wts, e = blk
        y = _swiglu(hf[toks], w_gate[e], w_up[e], w_down[e])
        return acc.at[toks].add(y * wts[:, None].astype(y.dtype)), None

    routed, _ = lax.scan(body, jnp.zeros_like(hf),
                         (row_tok.reshape(n_blocks, MOE_BLOCK), row_w.reshape(n_blocks, MOE_BLOCK), block_e))
    out = routed + _swiglu(hf, ws_gate, ws_up, ws_down)
    return out.reshape(B, S, D)


def setup_inputs(seed: int = 0) -> dict:
    key = jax.random.key(seed)
    ks = jax.random.split(key, 32)
    f32 = jnp.float32
    L, D, E, F = DEPTH, D_MODEL, N_EXPERTS, EXPERT_DIM

    def nrm(k, shape, scale):
        return jax.random.normal(k, shape, f32) * scale

    def gain(k, shape):
        return 1.0 + 0.02 * jax.random.normal(k, shape, f32)

    offs = jax.random.randint(ks[2], (BATCH, 1), 0, 4096, dtype=jnp.int32)
    positions = (jnp.arange(SEQ, dtype=jnp.int32)[None, :] + offs).astype(jnp.int32)
    return {
        'x': nrm(ks[0], (BATCH, SEQ, D), 1.0),
        'c': nrm(ks[1], (BATCH, D), 1.0),
        'positions': positions,
        'rel_bias': nrm(ks[3], (REL_BUCKETS, ATTN_HEADS), 0.2),
        'hgrn_lb_logits': nrm(ks[4], (2, DEPTH + 1, HGRN_HEADS * HGRN_K_DIM), 0.5),
        'w_ada': nrm(ks[5], (L, D, 6 * D), ADA_SCALE * D ** -0.5),
        'b_ada': nrm(ks[6], (L, 6 * D), 0.02),
        'g_mix': gain(ks[7], (L, D)),
        'w_in': nrm(ks[8], (L, D, IN_WIDTH), D ** -0.5),
        'g_q': gain(ks[9], (L, ATTN_QK_DIM)),
        'g_k': gain(ks[10], (L, ATTN_QK_DIM)),
        'lam_q1': nrm(ks[11], (L, ATTN_QK_DIM), 0.1),
        'lam_k1': nrm(ks[12], (L, ATTN_QK_DIM), 0.1),
        'lam_q2': nrm(ks[13], (L, ATTN_QK_DIM), 0.1),
        'lam_k2': nrm(ks[14], (L, ATTN_QK_DIM), 0.1),
        'g_sub': gain(ks[15], (L, ATTN_V_DIM)),
        'g_hgrn': gain(ks[16], (L, HGRN_V_DIM)),
        'w_out': nrm(ks[17], (L, MIX_WIDTH, D), MIX_WIDTH ** -0.5),
        'g_ffn': gain(ks[18], (L, D)),
        'w_router': nrm(ks[19], (L, D, E), D ** -0.5),
        'router_bias': nrm(ks[20], (L, E), 0.01),
        'w_exp_gate': nrm(ks[21], (L, E, D, F), D ** -0.5),
        'w_exp_up': nrm(ks[22], (L, E, D, F), D ** -0.5),
        'w_exp_down': nrm(ks[23], (L, E, F, D), F ** -0.5),
        'w_sh_gate': nrm(ks[24], (L, D, F), D ** -0.5),
        'w_sh_up': nrm(ks[25], (L, D, F), D ** -0.5),
        'w_sh_down': nrm(ks[26], (L, F, D), F ** -0.5),
    }


def reference(x, c, positions, rel_bias, hgrn_lb_logits, w_ada, b_ada, g_mix, w_in, g_q, g_k,
              lam_q1, lam_k1, lam_q2, lam_k2, g_sub, g_hgrn, w_out, g_ffn, w_router, router_bias,
              w_exp_gate, w_exp_up, w_exp_down, w_sh_gate, w_sh_up, w_sh_down):
    B, S, D = x.shape
    lbs = jnp.cumsum(jax.nn.softmax(hgrn_lb_logits.astype(jnp.float32), axis=1), axis=1)
    split_pts = np.cumsum(IN_SIZES)[:-1].tolist()
    for l in range(DEPTH):
        lam_init = 0.8 - 0.6 * math.exp(-0.3 * l)
        mod = jax.nn.silu(c) @ w_ada[l] + b_ada[l]
        sh1, sc1, gt1, sh2, sc2, gt2 = [m[:, None, :] for m in jnp.split(mod, 6, axis=-1)]

        h = _rmsnorm(x, g_mix[l]) * (1.0 + sc1) + sh1
        proj = h @ w_in[l]
        qa, ka, va, qh, ih, zf, zb, gh = jnp.split(proj, split_pts, axis=-1)

        qa = _rmsnorm(qa.reshape(B, S, ATTN_HEADS, 2, ATTN_QK_DIM), g_q[l])
        ka = _rmsnorm(ka.reshape(B, S, ATTN_HEADS, 2, ATTN_QK_DIM), g_k[l])
        to_bhsd = lambda t: jnp.transpose(t, (0, 2, 1, 3))
        q1, q2 = to_bhsd(qa[..., 0, :]), to_bhsd(qa[..., 1, :])
        k1, k2 = to_bhsd(ka[..., 0, :]), to_bhsd(ka[..., 1, :])
        vh = to_bhsd(va.reshape(B, S, ATTN_HEADS, ATTN_V_DIM))
        lam = (jnp.exp(jnp.sum(lam_q1[l].astype(jnp.float32) * lam_k1[l].astype(jnp.float32)))
               - jnp.exp(jnp.sum(lam_q2[l].astype(jnp.float32) * lam_k2[l].astype(jnp.float32)))
               + lam_init)
        oa = _diff_attention(q1, q2, k1, k2, vh, positions, rel_bias, lam)
        oa = _rmsnorm(to_bhsd(oa), g_sub[l]) * (1.0 - lam_init)
        oa = oa.reshape(B, S, ATTN_WIDTH).astype(x.dtype)

        oh = _hgrn2_mixer(qh, ih, zf, zb, gh, lbs[:, l], g_hgrn[l]).astype(x.dtype)

        x = x + gt1 * (jnp.concatenate([oa, oh], axis=-1) @ w_out[l])

        h2 = _rmsnorm(x, g_ffn[l]) * (1.0 + sc2) + sh2
        x = x + gt2 * _moe(h2, w_router[l], router_bias[l], w_exp_gate[l], w_exp_up[l],
                           w_exp_down[l], w_sh_gate[l], w_sh_up[l], w_sh_down[l])
    return x
```

```python
import contextlib
import math
import numpy as np
import concourse.bass as bass
import concourse.mybir as mybir
from concourse.bass_utils import run_bass_kernel_spmd

F32 = mybir.dt.float32
BF16 = mybir.dt.bfloat16
I32 = mybir.dt.int32
U32 = mybir.dt.uint32
AF = mybir.ActivationFunctionType
ALU = mybir.AluOpType
AX = mybir.AxisListType

D = 2048
S = 8192
EPS = 1e-6
NE = 256
FF = 512
CAP = 256
NSL = CAP // 128
ENGS = ("tensor", "vector", "scalar", "gpsimd", "sync")


class Tok:
    __slots__ = ("name", "last_w", "readers")

    def __init__(self, name=""):
        self.name = name
        self.last_w = None
        self.readers = []


class Op:
    __slots__ = ("eng", "fn", "deps", "dma_sem", "dma_val", "sig", "needs_sig", "idx")

    def __init__(self, eng, fn):
        self.eng = eng
        self.fn = fn
        self.deps = []
        self.dma_sem = None
        self.dma_val = 0
        self.sig = 0
        self.needs_sig = False


class Prog:
    def __init__(self, nc, stack):
        self.nc = nc
        self.stack = stack
        self.ops = []
        self.sems = {e: stack.enter_context(nc.semaphore("s_" + e)) for e in ENGS}
        self.dma_sem_count = {}
        self.dma_since_barrier = []
        self.nsem = 0

    def dsem(self, name):
        s = self.stack.enter_context(self.nc.semaphore(name))
        self.dma_sem_count[id(s)] = 0
        self.nsem += 1
        return s

    def op(self, eng, fn, reads=(), writes=(), dma_sem=None, extra_deps=()):
        o = Op(eng, fn)
        deps = []
        for r in reads:
            if r.last_w is not None:
                deps.append(r.last_w)
            r.readers.append(o)
        for w in writes:
            if w.last_w is not None:
                deps.append(w.last_w)
            deps.extend(w.readers)
            w.last_w = o
            w.readers = []
        deps.extend(extra_deps)
        seen = set()
        dd = []
        for d in deps:
            if d is o or id(d) in seen:
                continue
            seen.add(id(d))
            dd.append(d)
        best = {}
        keep = []
        for d in dd:
            if d.dma_sem is not None:
                keep.append(d)
            elif d.eng not in best or best[d.eng].idx < d.idx:
                best[d.eng] = d
        o.deps = keep + list(best.values())
        o.idx = len(self.ops)
        if dma_sem is not None:
            o.dma_sem = dma_sem
            self.dma_sem_count[id(dma_sem)] += 16
            o.dma_val = self.dma_sem_count[id(dma_sem)]
            self.dma_since_barrier.append(o)
        self.ops.append(o)
        return o

    def barrier(self):
        lasts = []
        for e in ENGS:
            for o in reversed(self.ops):
                if o.eng == e and o.dma_sem is None and o.fn is not None:
                    lasts.append(o)
                    break
        dmas = list(self.dma_since_barrier)
        self.dma_since_barrier = []
        for e in ENGS:
            self.op(e, None, extra_deps=lasts + dmas)

    def emit(self):
        nc = self.nc
        for o in self.ops:
            for d in o.deps:
                if d.dma_sem is None:
                    if d.eng == "tensor" and o.eng == "tensor":
                        continue
                    d.needs_sig = True
        counters = {e: 0 for e in ENGS}
        per_eng = {e: [] for e in ENGS}
        for o in self.ops:
            if o.dma_sem is None and o.needs_sig:
                counters[o.eng] += 1
                o.sig = counters[o.eng]
            per_eng[o.eng].append(o)
        sems = self.sems

        def run_engine(ename, eng):
            waited = {}
            for o in per_eng[ename]:
                for d in o.deps:
                    if d.dma_sem is not None:
                        key, sem, val = id(d.dma_sem), d.dma_sem, d.dma_val
                    else:
                        if d.eng == "tensor" and ename == "tensor":
                            continue
                        key, sem, val = d.eng, sems[d.eng], d.sig
                    if waited.get(key, 0) >= val:
                        continue
                    waited[key] = val
                    eng.wait_ge(sem, val)
                if o.fn is None:
                    continue
                inst = o.fn(eng)
                if o.dma_sem is not None:
                    inst.then_inc(o.dma_sem, 16)
                elif o.needs_sig:
                    inst.then_inc(sems[ename], 1)

        with nc.Block() as block:
            @block.tensor
            def _(e):
                run_engine("tensor", e)

            @block.vector
            def _(e):
                run_engine("vector", e)

            @block.scalar
            def _(e):
                run_engine("scalar", e)

            @block.gpsimd
            def _(e):
                run_engine("gpsimd", e)

            @block.sync
            def _(e):
                run_engine("sync", e)


class Ring:
    def __init__(self, P, st, nc, name, n, shape, dtype):
        self.t = [st.enter_context(nc.sbuf_tensor("%s%d" % (name, i), shape, dtype)) for i in range(n)]
        self.tok = [Tok("%s%d" % (name, i)) for i in range(n)]
        self.sem = [P.dsem("q_%s%d" % (name, i)) for i in range(n)]
        self.n = n
        self.i = -1

    def next(self):
        self.i = (self.i + 1) % self.n
        return self.t[self.i], self.tok[self.i], self.sem[self.i]


def _t5_bucket_np(rel):
    half, me = 16, 8
    ret = np.where(rel > 0, half, 0)
    n = np.abs(rel)
    nf = np.maximum(n, 1).astype(np.float32)
    large = me + (np.log(nf / np.float32(me)) / np.float32(math.log(128 / 8)) * np.float32(half - me)).astype(np.int32)
    large = np.minimum(large, half - 1)
    return ret + np.where(n < me, n, large)


def _consts():
    c = {}
    c["ident"] = np.eye(128, dtype=np.float32)
    c["antiid"] = np.eye(128, dtype=np.float32)[::-1].copy()
    ob = np.zeros((128, 128), np.float32)
    ob[:64, :64] = 1.0 / 64
    ob[64:, 64:] = 1.0 / 64
    c["onesblk"] = ob
    m = np.arange(1280)
    b = _t5_bucket_np(639 - m)
    oh = np.zeros((32, 1280), np.float32)
    oh[b, m] = 1.0
    c["ohg"] = oh
    s = np.arange(128)
    same = (s[:, None] // 64) == (s[None, :] // 64)
    c["mask_f"] = (same & (s[:, None] <= s[None, :])).astype(np.float32)
    c["mask_b"] = (same & (s[:, None] >= s[None, :])).astype(np.float32)
    return c


def build_l1(dbg=False):
    nc = bass.Bass("TRN2", target_bir_lowering=False)
    din = lambda n, s, d=F32: nc.dram_tensor(n, s, d, kind="ExternalInput").ap()
    xb = din("xb", [S, D] if dbg != "H" else [128, 128])
    cT = din("cT", [128, 16])
    w_ada = din("w_ada", [D, 6 * D] if dbg != "H" else [128, 128])
    b_adaT = din("b_adaT", [128, 96])
    g_mixT = din("g_mixT", [128, 16])
    w_own = din("w_own", [D, 2048])
    gq2 = din("gq2", [128, 1])
    gk2 = din("gk2", [128, 1])
    lamv = din("lamv", [1, 256])
    rb_own = din("rb_own", [32, 2])
    lbl = din("lbl", [128, 8])
    gsub = din("gsub", [1, 128])
    ghg = din("ghg", [1, 128])
    ident = din("ident", [128, 128])
    antiid = din("antiid", [128, 128])
    onesblk = din("onesblk", [128, 128])
    ohg = din("ohg", [32, 1280])
    mask_f = din("mask_f", [128, 128])
    mask_b = din("mask_b", [128, 128])
    mix = nc.dram_tensor("mix", [S, 512], BF16, kind="ExternalOutput").ap()
    modrow = nc.dram_tensor("modrow", [96, 128], F32, kind="ExternalOutput").ap()
    hT_d = nc.dram_tensor("hT_d", [128, 16, S], BF16).ap()
    G_d = nc.dram_tensor("G_d", [2, 1280], F32).ap()
    of_d = nc.dram_tensor("of_d", [2, S, 128], F32).ap()

    with contextlib.ExitStack() as st0:
        P = Prog(nc, st0)
        ps = [st0.enter_context(nc.psum_tensor("ps%d" % i, [128, 512], F32)) for i in range(8)]
        pst = [Tok("ps%d" % i) for i in range(8)]
        sb0 = lambda n, s, d=F32: st0.enter_context(nc.sbuf_tensor(n, s, d))
        identf = sb0("identf", [128, 128])
        identb = sb0("identb", [128, 128], BF16)
        antif = sb0("antif", [128, 128])
        onesb = sb0("onesb", [128, 128])
        modT = sb0("modT", [128, 96])
        A1 = sb0("A1", [128, 16])
        gq2t = sb0("gq2t", [128, 1])
        gk2t = sb0("gk2t", [128, 1])
        neglam = sb0("neglam", [128, 1])
        rbb = sb0("rbb", [128, 64])
        lbt = sb0("lbt", [128, 4])
        omlt = sb0("omlt", [128, 4])
        gsubb = sb0("gsubb", [128, 128])
        ghgb = sb0("ghgb", [128, 128])
        mskf = sb0("mskf", [128, 128])
        mskb = sb0("mskb", [128, 128])
        t_const = Tok("const")
        cs = P.dsem("q_const")
        ld = lambda dst, src: P.op("sync", lambda e: e.dma_start(out=dst, in_=src), writes=[t_const], dma_sem=cs)
        ld(identf[:], ident[:, :])
        ld(antif[:], antiid[:, :])
        ld(onesb[:], onesblk[:, :])
        ld(gq2t[:], gq2[:, :])
        ld(gk2t[:], gk2[:, :])
        ld(rbb[:], rb_own.rearrange("b h -> (b h)").rearrange("(o n) -> o n", o=1).partition_broadcast(128))
        ld(gsubb[:], gsub[0:1, :].partition_broadcast(128))
        ld(ghgb[:], ghg[0:1, :].partition_broadcast(128))
        ld(mskf[:], mask_f[:, :])
        ld(mskb[:], mask_b[:, :])
        P.barrier()
        P.op("vector", lambda e: e.tensor_copy(out=identb[:], in_=identf[:]), writes=[t_const])

        if dbg == "H":
            P.op("vector", lambda e: e.memset(lbt[:], 0.5), writes=[t_const])
            P.op("vector", lambda e: e.memset(omlt[:], 0.5), writes=[t_const])
            t_hTd = [Tok("hTd%d" % i) for i in range(16)]
            build_hgrn(nc, P, ps, pst, w_own, hT_d, t_hTd, of_d, mix, lbt, omlt, ghgb, mskf, mskb, identb, dbg_n=DBG_N[0])
            P.barrier()
            P.emit()
            return nc
        with contextlib.ExitStack() as st:
            sb = lambda n, s, d=F32: st.enter_context(nc.sbuf_tensor(n, s, d))
            ct = sb("ct", [128, 16])
            sct = sb("sct", [128, 16])
            bat = sb("bat", [128, 96])
            gmt = sb("gmt", [128, 16])
            lamt = sb("lamt", [128, 256])
            lamp = sb("lamp", [128, 128])
            lams = sb("lams", [128, 2])
            lbl_t = sb("lbl_t", [128, 8])
            modS = sb("modS", [96, 128])
            t0 = Tok("p0")
            s0 = P.dsem("q_p0")
            ld0 = lambda dst, src: P.op("sync", lambda e: e.dma_start(out=dst, in_=src), writes=[t0], dma_sem=s0)
            ld0(ct[:], cT[:, :])
            ld0(bat[:], b_adaT[:, :])
            ld0(gmt[:], g_mixT[:, :])
            ld0(lamt[:], lamv[0:1, :].partition_broadcast(128))
            ld0(lbl_t[:], lbl[:, :])
            P.barrier()
            t_sct = Tok("sct")
            P.op("scalar", lambda e: e.activation(out=sct[:], in_=ct[:], func=AF.Silu), writes=[t_sct])
            t_lam = Tok("lam")
            lam_init = 0.8 - 0.6 * math.exp(0.0)
            P.op("vector", lambda e: e.tensor_tensor(out=lamp[:, 0:64], in0=lamt[:, 0:64], in1=lamt[:, 64:128], op=ALU.mult), writes=[t_lam])
            P.op("vector", lambda e: e.tensor_tensor(out=lamp[:, 64:128], in0=lamt[:, 128:192], in1=lamt[:, 192:256], op=ALU.mult), writes=[t_lam])
            P.op("vector", lambda e: e.tensor_reduce(out=lams[:], in_=lamp[:].rearrange("p (a b) -> p a b", a=2), axis=AX.X, op=ALU.add), writes=[t_lam])
            P.op("scalar", lambda e: e.activation(out=lams[:], in_=lams[:], func=AF.Exp), writes=[t_lam])
            P.op("vector", lambda e: e.tensor_tensor(out=neglam[:], in0=lams[:, 1:2], in1=lams[:, 0:1], op=ALU.subtract), writes=[t_lam])
            P.op("vector", lambda e: e.tensor_scalar(out=neglam[:], in0=neglam[:], scalar1=-lam_init, scalar2=None, op0=ALU.add), writes=[t_lam])
            t_lb = Tok("lb")
            lv = lbl_t[:].rearrange("p (d s h) -> p d s h", d=2, s=2)
            P.op("vector", lambda e: e.tensor_tensor(out=lbt[:].rearrange("p (d h) -> p d h", d=2), in0=lv[:, :, 0, :], in1=lv[:, :, 1, :], op=ALU.subtract), writes=[t_lb])
            P.op("scalar", lambda e: e.activation(out=lbt[:], in_=lbt[:], func=AF.Sigmoid), writes=[t_lb])
            P.op("vector", lambda e: e.tensor_scalar(out=omlt[:], in0=lbt[:], scalar1=-1.0, scalar2=1.0, op0=ALU.mult, op1=ALU.add), writes=[t_lb])

            wring = Ring(P, st, nc, "wa", 2, [128, 16, 512], F32)
            t_mod = pst[0]
            for cb in range(24):
                wt, wtok, wsem = wring.next()
                P.op("sync", lambda e, wt=wt, cb=cb: e.dma_start(out=wt[:], in_=w_ada[:, cb * 512:(cb + 1) * 512].rearrange("(k p) n -> p k n", p=128)),
                     writes=[wtok], dma_sem=wsem)
                for t in range(4):
                    col = cb * 4 + t
                    for k in range(16):
                        P.op("tensor", lambda e, wt=wt, t=t, k=k, col=col: e.matmul(
                            out=ps[0][:, col:col + 1], lhsT=wt[:, k, t * 128:(t + 1) * 128], rhs=sct[:, k:k + 1],
                            start=(k == 0), stop=(k == 15)), reads=[wtok, t_sct], writes=[t_mod])
            t_modT = Tok("modT")
            P.op("vector", lambda e: e.tensor_tensor(out=modT[:], in0=ps[0][:, 0:96], in1=bat[:], op=ALU.add), reads=[t_mod], writes=[t_modT])
            P.op("vector", lambda e: e.scalar_tensor_tensor(out=A1[:], in0=modT[:, 16:32], scalar=1.0, in1=gmt[:], op0=ALU.add, op1=ALU.mult),
                 reads=[t_modT], writes=[t_modT])
            P.op("tensor", lambda e: e.transpose(out=ps[1][0:96, 0:128], in_=modT[:, 0:96], identity=identf[:]), reads=[t_modT], writes=[pst[1]])
            t_modS = Tok("modS")
            P.op("vector", lambda e: e.tensor_copy(out=modS[:], in_=ps[1][0:96, 0:128]), reads=[pst[1]], writes=[t_modS])
            P.op("sync", lambda e: e.dma_start(out=modrow[:, :], in_=modS[:]), reads=[t_modS], dma_sem=P.dsem("q_modrow"))
            P.barrier()
        sh1 = modT[:, 0:16]

        t_hTd = [Tok("hTd%d" % i) for i in range(16)]
        with contextlib.ExitStack() as st:
            xring = Ring(P, st, nc, "xr", 3, [128, D], F32)
            xnring = Ring(P, st, nc, "xn", 2, [128, D], BF16)
            hbring = Ring(P, st, nc, "hb", 2, [128, 16, 512], BF16)
            junk = st.enter_context(nc.sbuf_tensor("junk", [128, D], BF16))
            ssr = st.enter_context(nc.sbuf_tensor("ssr", [128, 64], F32))
            t_junk = Tok("junk")
            t_ss = Tok("ss")
            for nb in range(16):
                hb, hbtok, hbsem = hbring.next()
                for tt in range(4):
                    i = nb * 4 + tt
                    xt, xtok, xsem = xring.next()
                    xn, xntok, _ = xnring.next()
                    P.op("sync", lambda e, xt=xt, i=i: e.dma_start(out=xt[:], in_=xb[i * 128:(i + 1) * 128, :]), writes=[xtok], dma_sem=xsem)
                    P.op("scalar", lambda e, xt=xt, i=i: e.activation(out=junk[:], in_=xt[:], func=AF.Square, accum_out=ssr[:, i:i + 1]),
                         reads=[xtok], writes=[t_junk, t_ss])
                    P.op("vector", lambda e, i=i: e.tensor_scalar(out=ssr[:, i:i + 1], in0=ssr[:, i:i + 1], scalar1=1.0 / D, scalar2=EPS, op0=ALU.mult, op1=ALU.add), writes=[t_ss])
                    P.op("scalar", lambda e, i=i: e.activation(out=ssr[:, i:i + 1], in_=ssr[:, i:i + 1], func=AF.Sqrt), writes=[t_ss])
                    P.op("vector", lambda e, i=i: e.reciprocal(out=ssr[:, i:i + 1], in_=ssr[:, i:i + 1]), writes=[t_ss])
                    P.op("vector", lambda e, xt=xt, xn=xn, i=i: e.tensor_scalar(out=xn[:], in0=xt[:], scalar1=ssr[:, i:i + 1], scalar2=None, op0=ALU.mult),
                         reads=[xtok, t_ss], writes=[xntok])
                    for half in range(2):
                        pb = 2 + half
                        for c8 in range(8):
                            c = half * 8 + c8
                            P.op("tensor", lambda e, xn=xn, c=c, c8=c8, pb=pb: e.transpose(
                                out=ps[pb][:].bitcast(BF16)[:, c8 * 128:(c8 + 1) * 128], in_=xn[:, c * 128:(c + 1) * 128], identity=identb[:]),
                                reads=[xntok], writes=[pst[pb]])
                        for c8 in range(8):
                            c = half * 8 + c8
                            src = lambda pb=pb, c8=c8: ps[pb][:].bitcast(BF16)[:, c8 * 128:(c8 + 1) * 128]
                            if c % 2 == 0:
                                P.op("vector", lambda e, hb=hb, c=c, tt=tt, src=src: e.tensor_scalar(
                                    out=hb[:, c, tt * 128:(tt + 1) * 128], in0=src(), scalar1=A1[:, c:c + 1], scalar2=modT[:, c:c + 1], op0=ALU.mult, op1=ALU.add),
                                    reads=[pst[pb]], writes=[hbtok])
                            else:
                                P.op("scalar", lambda e, hb=hb, c=c, tt=tt, src=src: e.activation(
                                    out=hb[:, c, tt * 128:(tt + 1) * 128], in_=src(), func=AF.Identity, scale=A1[:, c:c + 1], bias=modT[:, c:c + 1]),
                                    reads=[pst[pb]], writes=[hbtok])
                P.op("sync", lambda e, hb=hb, nb=nb: e.dma_start(out=hT_d[:, :, nb * 512:(nb + 1) * 512], in_=hb[:]), reads=[hbtok], writes=[t_hTd[nb]], dma_sem=hbsem)
            P.barrier()

        if dbg == "1a":
            P.emit()
            return nc
        with contextlib.ExitStack() as st:
            sb = lambda n, s, d=F32: st.enter_context(nc.sbuf_tensor(n, s, d))
            wA = sb("wA", [128, 16, 768], BF16)
            qaT = [sb("qaT%d" % h, [128, S], BF16) for h in range(2)]
            kaT = [sb("kaT%d" % h, [128, S], BF16) for h in range(2)]
            va = sb("va", [128, 64, 2, 130], BF16)
            EB = sb("EB", [128, 6, 2, 512], BF16)
            t_wA = Tok("wA")
            t_q = [Tok("q0"), Tok("q1")]
            t_k = [Tok("k0"), Tok("k1")]
            t_va = Tok("va")
            t_EB = Tok("EB")
            P.op("vector", lambda e: e.memset(va[:, :, :, 128:130], 1.0), writes=[t_va])
            with contextlib.ExitStack() as st2:
                wsr = Ring(P, st2, nc, "wsA", 2, [128, 16, 256], F32)
                for g in range(3):
                    wt, wtok, wsem = wsr.next()
                    P.op("sync", lambda e, wt=wt, g=g: e.dma_start(out=wt[:], in_=w_own[:, g * 256:(g + 1) * 256].rearrange("(k p) n -> p k n", p=128)),
                         writes=[wtok], dma_sem=wsem)
                    P.op("vector", lambda e, wt=wt, g=g: e.tensor_copy(out=wA[:, :, g * 256:(g + 1) * 256], in_=wt[:]), reads=[wtok], writes=[t_wA])
                ohs = st2.enter_context(nc.sbuf_tensor("ohs", [32, 1280], F32))
                rbs = st2.enter_context(nc.sbuf_tensor("rbs", [32, 2], F32))
                Gs = st2.enter_context(nc.sbuf_tensor("Gs", [2, 1280], F32))
                Hk = st2.enter_context(nc.sbuf_tensor("Hk", [128, 512], F32))
                t_oh = Tok("oh")
                s_oh = P.dsem("q_oh")
                P.op("sync", lambda e: e.dma_start(out=ohs[:], in_=ohg[:, :]), writes=[t_oh], dma_sem=s_oh)
                s_rbs = P.dsem("q_rbs")
                t_rbs = Tok("rbs")
                P.op("sync", lambda e: e.dma_start(out=rbs[:], in_=rb_own[:, :]), writes=[t_rbs], dma_sem=s_rbs)
                t_Gs = Tok("Gs")
                for q3 in range(3):
                    w_ = 512 if q3 < 2 else 256
                    P.op("tensor", lambda e, q3=q3, w_=w_: e.matmul(out=ps[0][0:2, 0:w_], lhsT=rbs[:, :], rhs=ohs[:, q3 * 512:q3 * 512 + w_], start=True, stop=True),
                         reads=[t_oh, t_rbs], writes=[pst[0]])
                    P.op("vector", lambda e, q3=q3, w_=w_: e.tensor_copy(out=Gs[:, q3 * 512:q3 * 512 + w_], in_=ps[0][0:2, 0:w_]), reads=[pst[0]], writes=[t_Gs])
                t_Gd = Tok("Gd")
                s_G = P.dsem("q_G")
                P.op("sync", lambda e: e.dma_start(out=G_d[:, :], in_=Gs[:]), reads=[t_Gs], writes=[t_Gd], dma_sem=s_G)
                t_Hk = Tok("Hk")
                s_Hk = P.dsem("q_Hk")
                for oi in range(6):
                    base = 512 - (oi - 1) * 128
                    for hh in range(2):
                        src = bass.AP(tensor=G_d.tensor, offset=G_d[hh:hh + 1, base:base + 1].offset, ap=[[1, 128], [1, 512]])
                        P.op("sync", lambda e, src=src: e.dma_start(out=Hk[:], in_=src), reads=[t_Gd], writes=[t_Hk], dma_sem=s_Hk)
                        P.op("tensor", lambda e: e.matmul(out=ps[0][:, :], lhsT=antif[:], rhs=Hk[:], start=True, stop=True), reads=[t_Hk], writes=[pst[0]])
                        P.op("scalar", lambda e, oi=oi, hh=hh: e.activation(out=EB[:, oi, hh, :], in_=ps[0][:, :], func=AF.Exp), reads=[pst[0]], writes=[t_EB])
                P.barrier()
            hbring = Ring(P, st, nc, "hbA", 2, [128, 16, 512], BF16)
            sq = sb("sq", [128, 512])
            rs = sb("rs", [128, 512])
            t_sq, t_rs = Tok("sq"), Tok("rs")
            for nb in range(16):
                hb, hbtok, hbsem = hbring.next()
                P.op("sync", lambda e, hb=hb, nb=nb: e.dma_start(out=hb[:], in_=hT_d[:, :, nb * 512:(nb + 1) * 512]), reads=[t_hTd[nb]], writes=[hbtok], dma_sem=hbsem)
                for ci in range(4):
                    isk, hh = ci // 2, ci % 2
                    pb = ci % 2
                    for k in range(16):
                        P.op("tensor", lambda e, hb=hb, ci=ci, k=k, pb=pb: e.matmul(out=ps[pb][:, :], lhsT=wA[:, k, ci * 128:(ci + 1) * 128], rhs=hb[:, k, :],
                                                                                  start=(k == 0), stop=(k == 15)), reads=[t_wA, hbtok], writes=[pst[pb]])
                    P.op("scalar", lambda e, pb=pb: e.activation(out=sq[:], in_=ps[pb][:, :], func=AF.Square), reads=[pst[pb]], writes=[t_sq])
                    P.op("tensor", lambda e: e.matmul(out=ps[2][:, :], lhsT=onesb[:], rhs=sq[:], start=True, stop=True), reads=[t_sq], writes=[pst[2]])
                    P.op("scalar", lambda e: e.activation(out=rs[:], in_=ps[2][:, :], func=AF.Sqrt, bias=EPS, scale=1.0), reads=[pst[2]], writes=[t_rs])
                    P.op("vector", lambda e: e.reciprocal(out=rs[:], in_=rs[:]), writes=[t_rs])
                    dst = (kaT if isk else qaT)[hh]
                    dtok = (t_k if isk else t_q)[hh]
                    gsc = gk2t if isk else gq2t
                    P.op("vector", lambda e, dst=dst, pb=pb, gsc=gsc, nb=nb: e.scalar_tensor_tensor(
                        out=dst[:, nb * 512:(nb + 1) * 512], in0=ps[pb][:, :], scalar=gsc[:, 0:1], in1=rs[:], op0=ALU.mult, op1=ALU.mult),
                        reads=[pst[pb], t_rs], writes=[dtok])
                for tt in range(4):
                    pb = 3 + (tt % 2)
                    for k in range(16):
                        P.op("tensor", lambda e, hb=hb, tt=tt, k=k, pb=pb: e.matmul(out=ps[pb][:, 0:256], lhsT=hb[:, k, tt * 128:(tt + 1) * 128], rhs=wA[:, k, 512:768],
                                                                                  start=(k == 0), stop=(k == 15)), reads=[t_wA, hbtok], writes=[pst[pb]])
                    P.op("scalar", lambda e, tt=tt, pb=pb, nb=nb: e.activation(out=va[:, nb * 4 + tt, :, 0:128], in_=ps[pb][:, 0:256].rearrange("p (h d) -> p h d", h=2), func=AF.Copy),
                         reads=[pst[pb]], writes=[t_va])
            ptring = Ring(P, st, nc, "pt", 3, [128, 512], BF16)
            osb = sb("osb", [128, 2, 130])
            o1 = sb("o1", [128, 128])
            o2 = sb("o2", [128, 128])
            rec = sb("rec", [128, 4])
            mst = Ring(P, st, nc, "mst", 2, [128, 4, 128], BF16)
            t_fin = Tok("fin")
            junk2 = sb("junk2", [128, 128])
            for hh in range(2):
                farb = [rbb[:, 15 * 2 + hh:15 * 2 + hh + 1], rbb[:, 31 * 2 + hh:31 * 2 + hh + 1]]
                for qb in range(16):
                    for kt in range(64):
                        oi = kt - 4 * qb + 1
                        near = 0 <= oi < 6
                        for m in range(2):
                            sbk = 2 + ((kt * 2 + m) % 2)
                            P.op("tensor", lambda e, hh=hh, qb=qb, kt=kt, m=m, sbk=sbk: e.matmul(
                                out=ps[sbk][:, :], lhsT=kaT[hh][m * 64:(m + 1) * 64, kt * 128:(kt + 1) * 128],
                                rhs=qaT[hh][m * 64:(m + 1) * 64, qb * 512:(qb + 1) * 512], start=True, stop=True),
                                reads=[t_k[hh], t_q[hh]], writes=[pst[sbk]])
                            pt, pttok, _ = ptring.next()
                            if near:
                                P.op("scalar", lambda e, pt=pt, sbk=sbk: e.activation(out=pt[:], in_=ps[sbk][:, :], func=AF.Exp, scale=0.125), reads=[pst[sbk]], writes=[pttok])
                                P.op("vector", lambda e, pt=pt, oi=oi, hh=hh: e.tensor_tensor(out=pt[:], in0=pt[:], in1=EB[:, oi, hh, :], op=ALU.mult), reads=[t_EB], writes=[pttok])
                            else:
                                fb = farb[0] if oi < 0 else farb[1]
                                P.op("scalar", lambda e, pt=pt, sbk=sbk, fb=fb: e.activation(out=pt[:], in_=ps[sbk][:, :], func=AF.Exp, scale=0.125, bias=fb), reads=[pst[sbk]], writes=[pttok])
                            for s4 in range(4):
                                ob = 4 + 2 * m + s4 // 2
                                oc = (s4 % 2) * 256
                                P.op("tensor", lambda e, pt=pt, s4=s4, ob=ob, oc=oc, kt=kt, hh=hh: e.matmul(
                                    out=ps[ob][:, oc:oc + 130], lhsT=pt[:, s4 * 128:(s4 + 1) * 128], rhs=va[:, kt, hh, :],
                                    start=(kt == 0 and s4 % 2 == 0), stop=(kt == 63), skip_group_check=True),
                                    reads=[pttok, t_va], writes=[pst[ob]])
                    ms, mstok, mssem = mst.next()
                    for s4 in range(4):
                        oc = (s4 % 2) * 256
                        b1 = 4 + s4 // 2
                        b2 = 6 + s4 // 2
                        P.op("vector", lambda e, b1=b1, oc=oc: e.reciprocal(out=rec[:, 0:1], in_=ps[b1][:, oc + 128:oc + 129]), reads=[pst[b1]], writes=[t_fin])
                        P.op("vector", lambda e, b2=b2, oc=oc: e.reciprocal(out=rec[:, 1:2], in_=ps[b2][:, oc + 128:oc + 129]), reads=[pst[b2]], writes=[t_fin])
                        P.op("vector", lambda e: e.tensor_tensor(out=rec[:, 1:2], in0=rec[:, 1:2], in1=neglam[:], op=ALU.mult), writes=[t_fin])
                        P.op("vector", lambda e, b1=b1, oc=oc: e.tensor_scalar(out=o1[:], in0=ps[b1][:, oc:oc + 128], scalar1=rec[:, 0:1], scalar2=None, op0=ALU.mult), reads=[pst[b1]], writes=[t_fin])
                        P.op("vector", lambda e, b2=b2, oc=oc: e.scalar_tensor_tensor(out=o1[:], in0=ps[b2][:, oc:oc + 128], scalar=rec[:, 1:2], in1=o1[:], op0=ALU.mult, op1=ALU.add),
                             reads=[pst[b2]], writes=[t_fin])
                        P.op("scalar", lambda e: e.activation(out=junk2[:], in_=o1[:], func=AF.Square, accum_out=rec[:, 2:3]), writes=[t_fin])
                        P.op("vector", lambda e: e.tensor_scalar(out=rec[:, 2:3], in0=rec[:, 2:3], scalar1=1.0 / 128, scalar2=EPS, op0=ALU.mult, op1=ALU.add), writes=[t_fin])
                        P.op("scalar", lambda e: e.activation(out=rec[:, 2:3], in_=rec[:, 2:3], func=AF.Sqrt), writes=[t_fin])
                        P.op("vector", lambda e: e.reciprocal(out=rec[:, 3:4], in_=rec[:, 2:3]), writes=[t_fin])
                        P.op("vector", lambda e: e.tensor_scalar(out=o1[:], in0=o1[:], scalar1=rec[:, 3:4], scalar2=1.0 - lam_init, op0=ALU.mult, op1=ALU.mult), writes=[t_fin])
                        P.op("vector", lambda e, ms=ms, s4=s4: e.tensor_tensor(out=ms[:, s4, :], in0=o1[:], in1=gsubb[:], op=ALU.mult), reads=[t_fin], writes=[mstok])
                    P.op("sync", lambda e, ms=ms, qb=qb, hh=hh: e.dma_start(
                        out=mix[qb * 512:(qb + 1) * 512, hh * 128:(hh + 1) * 128].rearrange("(s p) d -> p s d", p=128), in_=ms[:]),
                        reads=[mstok], dma_sem=mssem)
            P.barrier()

        if dbg == "A":
            P.emit()
            return nc
        build_hgrn(nc, P, ps, pst, w_own, hT_d, t_hTd, of_d, mix, lbt, omlt, ghgb, mskf, mskb, identb)
        P.barrier()
        P.emit()
    return nc


EPS_AP = [None]


DBG_N = [0]


def build_hgrn(nc, P, ps, pst, w_own, hT_d, t_hTd, of_d, mix, lbt, omlt, ghgb, mskf, mskb, identb, dbg_n=0):
    with contextlib.ExitStack() as st:
        sb = lambda n, s, d=F32: st.enter_context(nc.sbuf_tensor(n, s, d))
        wH = sb("wH", [128, 16, 1280], BF16)
        t_wH = Tok("wH")
        with contextlib.ExitStack() as st2:
            wsr = Ring(P, st2, nc, "wsH", 2, [128, 16, 256], F32)
            for g in range(5):
                wt, wtok, wsem = wsr.next()
                P.op("sync", lambda e, wt=wt, g=g: e.dma_start(out=wt[:], in_=w_own[:, (3 + g) * 256:(4 + g) * 256].rearrange("(k p) n -> p k n", p=128)),
                     writes=[wtok], dma_sem=wsem)
                for hh in range(2):
                    P.op("vector", lambda e, wt=wt, g=g, hh=hh: e.tensor_copy(out=wH[:, :, hh * 640 + g * 128:hh * 640 + (g + 1) * 128], in_=wt[:, :, hh * 128:(hh + 1) * 128]),
                         reads=[wtok], writes=[t_wH])
            P.barrier()
        hbring = Ring(P, st, nc, "hbH", 2, [128, 16, 512], BF16)
        rmask = sb("rmask", [128, 512])
        cmlo = sb("cmlo", [128, 512])
        cmhi = sb("cmhi", [128, 512])
        t_rm = Tok("rm")
        P.op("vector", lambda e: e.memset(rmask[:], 1.0), writes=[t_rm])
        P.op("vector", lambda e: e.memset(rmask[:].rearrange("p (c t) -> p c t", t=64)[:, :, 0:1], 0.0), writes=[t_rm])
        P.op("vector", lambda e: e.memset(cmlo[:], 0.0), writes=[t_rm])
        P.op("vector", lambda e: e.memset(cmhi[:], 0.0), writes=[t_rm])
        P.op("vector", lambda e: e.memset(cmlo[:].rearrange("p (c t) -> p c t", t=128)[:, :, 0:64], 1.0), writes=[t_rm])
        P.op("vector", lambda e: e.memset(cmhi[:].rearrange("p (c t) -> p c t", t=128)[:, :, 64:128], 1.0), writes=[t_rm])
        H2 = range(2)
        Sf = [sb("Sf%d" % h, [128, 128]) for h in H2]
        Sb_ = [sb("Sb%d" % h, [128, 128], BF16) for h in H2]
        tmpS = [sb("tmpS%d" % h, [128, 128]) for h in H2]
        qs = [sb("qs%d" % h, [128, 512]) for h in H2]
        ff = [sb("ff%d" % h, [128, 512]) for h in H2]
        lg = [sb("lg%d" % h, [128, 512]) for h in H2]
        bb = [sb("bb%d" % h, [128, 512]) for h in H2]
        eb = [sb("eb%d" % h, [128, 512]) for h in H2]
        enb = [sb("enb%d" % h, [128, 512]) for h in H2]
        qfb = [sb("qfb%d" % h, [128, 512], BF16) for h in H2]
        qlo = [sb("qlo%d" % h, [128, 512], BF16) for h in H2]
        qhi = [sb("qhi%d" % h, [128, 512], BF16) for h in H2]
        kt_ = [sb("kt%d" % h, [128, 512], BF16) for h in H2]
        vt = [sb("vt%d" % h, [128, 4, 128], BF16) for h in H2]
        vlo = [sb("vlo%d" % h, [128, 4, 128], BF16) for h in H2]
        vhi = [sb("vhi%d" % h, [128, 4, 128], BF16) for h in H2]
        sg = [sb("sg%d" % h, [128, 4, 128]) for h in H2]
        kh2 = [sb("kh2%d" % h, [128, 128], BF16) for h in H2]
        scm = [sb("scm%d" % h, [128, 128], BF16) for h in H2]
        ob = [sb("ob%d" % h, [128, 4, 128]) for h in H2]
        ofl = [sb("ofl%d" % h, [128, 4, 128]) for h in H2]
        obb = [sb("obb%d" % h, [128, 4, 128], BF16) for h in H2]
        stat = [sb("stat%d" % h, [128, 4]) for h in H2]
        junk = [sb("junkh%d" % h, [128, 4, 128]) for h in H2]
        t_S = [Tok("S") for h in H2]
        tE = [Tok("tE") for h in H2]
        t_v = [Tok("v") for h in H2]
        t_sg = [Tok("sg") for h in H2]
        t_kh = [Tok("kh") for h in H2]
        t_scm = [Tok("scm") for h in H2]
        t_ob = [Tok("ob") for h in H2]
        t_ofl = [Tok("ofl") for h in H2]
        s_ob = [P.dsem("q_ob%d" % h) for h in H2]
        s_ofl = [P.dsem("q_ofl%d" % h) for h in H2]
        t_ofd = [[Tok("ofd") for _ in range(16)] for _ in H2]
        for hh in H2:
            P.op("vector", lambda e, hh=hh: e.memset(vlo[hh][:], 0.0), writes=[t_v[hh]])
            P.op("vector", lambda e, hh=hh: e.memset(vhi[hh][:], 0.0), writes=[t_v[hh]])
        for d in range(2):
            if dbg_n and d == 1:
                break
            for hh in H2:
                P.op("vector", lambda e, hh=hh: e.memset(Sf[hh][:], 0.0), writes=[t_S[hh]])
                P.op("vector", lambda e, hh=hh: e.memset(Sb_[hh][:], 0.0), writes=[t_S[hh]])
            blocks = range(16) if d == 0 else range(15, -1, -1)
            if dbg_n:
                blocks = range(1)
            mk = mskf if d == 0 else mskb
            for nb in blocks:
                hb, hbtok, hbsem = hbring.next()
                P.op("sync", lambda e, hb=hb, nb=nb: e.dma_start(out=hb[:], in_=hT_d[:, :, nb * 512:(nb + 1) * 512]), reads=[t_hTd[nb]], writes=[hbtok], dma_sem=hbsem)
                for hh in H2:
                    base = hh * 640
                    zc = base + (2 + d) * 128
                    li = d * 2 + hh
                    for k in range(16):
                        P.op("tensor", lambda e, hb=hb, k=k, base=base: e.matmul(out=ps[0][:, :], lhsT=wH[:, k, base:base + 128], rhs=hb[:, k, :], start=(k == 0), stop=(k == 15)),
                             reads=[t_wH, hbtok], writes=[pst[0]])
                    P.op("scalar", lambda e, hh=hh: e.activation(out=qs[hh][:], in_=ps[0][:, :], func=AF.Silu), reads=[pst[0]], writes=[tE[hh]])
                    for k in range(16):
                        P.op("tensor", lambda e, hb=hb, k=k, zc=zc: e.matmul(out=ps[1][:, :], lhsT=wH[:, k, zc:zc + 128], rhs=hb[:, k, :], start=(k == 0), stop=(k == 15)),
                             reads=[t_wH, hbtok], writes=[pst[1]])
                    P.op("scalar", lambda e, hh=hh: e.activation(out=ff[hh][:], in_=ps[1][:, :], func=AF.Sigmoid), reads=[pst[1]], writes=[tE[hh]])
                    P.op("vector", lambda e, hh=hh, li=li: e.tensor_scalar(out=ff[hh][:], in0=ff[hh][:], scalar1=omlt[:, li:li + 1], scalar2=lbt[:, li:li + 1], op0=ALU.mult, op1=ALU.add), writes=[tE[hh]])
                    P.op("scalar", lambda e, hh=hh: e.activation(out=lg[hh][:], in_=ff[hh][:], func=AF.Ln), writes=[tE[hh]])
                    P.op("vector", lambda e, hh=hh: e.tensor_scalar(out=ff[hh][:], in0=ff[hh][:], scalar1=-1.0, scalar2=1.0, op0=ALU.mult, op1=ALU.add), writes=[tE[hh]])
                    P.op("vector", lambda e, hh=hh: e.tensor_tensor_scan(out=bb[hh][:], data0=rmask[:], data1=lg[hh][:], initial=0.0, op0=ALU.mult, op1=ALU.add), reads=[t_rm], writes=[tE[hh]])
                    if d == 1:
                        b3 = bb[hh][:].rearrange("p (c t) -> p c t", t=64)
                        l3 = lg[hh][:].rearrange("p (c t) -> p c t", t=64)
                        P.op("vector", lambda e, hh=hh: e.tensor_tensor(out=lg[hh][:], in0=lg[hh][:], in1=bb[hh][:], op=ALU.subtract), writes=[tE[hh]])
                        P.op("vector", lambda e, hh=hh: e.tensor_copy(out=stat[hh][:, 0:4], in_=bb[hh][:].rearrange("p (c t) -> p c t", t=128)[:, :, 63]), writes=[tE[hh]])
                        P.op("vector", lambda e, hh=hh: e.tensor_copy(out=junk[hh][:, 0, 0:4], in_=bb[hh][:].rearrange("p (c t) -> p c t", t=128)[:, :, 127]), writes=[tE[hh]])
                        for c in range(8):
                            tot = stat[hh][:, c // 2:c // 2 + 1] if c % 2 == 0 else junk[hh][:, 0, c // 2:c // 2 + 1]
                            P.op("vector", lambda e, hh=hh, c=c, tot=tot: e.tensor_scalar(out=bb[hh][:, c * 64:(c + 1) * 64], in0=lg[hh][:, c * 64:(c + 1) * 64], scalar1=tot, scalar2=None, op0=ALU.add), writes=[tE[hh]])
                    P.op("scalar", lambda e, hh=hh: e.activation(out=eb[hh][:], in_=bb[hh][:], func=AF.Exp), writes=[tE[hh]])
                    P.op("scalar", lambda e, hh=hh: e.activation(out=enb[hh][:], in_=bb[hh][:], func=AF.Exp, scale=-1.0), writes=[tE[hh]])
                    P.op("vector", lambda e, hh=hh: e.tensor_tensor(out=qfb[hh][:], in0=qs[hh][:], in1=eb[hh][:], op=ALU.mult), writes=[tE[hh]])
                    P.op("vector", lambda e, hh=hh: e.tensor_tensor(out=qlo[hh][:], in0=qfb[hh][:], in1=cmlo[:], op=ALU.mult), writes=[tE[hh]])
                    P.op("vector", lambda e, hh=hh: e.tensor_tensor(out=qhi[hh][:], in0=qfb[hh][:], in1=cmhi[:], op=ALU.mult), writes=[tE[hh]])
                    P.op("vector", lambda e, hh=hh: e.tensor_tensor(out=kt_[hh][:], in0=ff[hh][:], in1=enb[hh][:], op=ALU.mult), writes=[tE[hh]])
                    for which in range(2 if d == 1 else 1):
                        cc = base + (1 if which == 0 else 4) * 128
                        pb = which
                        for tt in range(4):
                            for k in range(16):
                                P.op("tensor", lambda e, hb=hb, k=k, tt=tt, cc=cc, pb=pb: e.matmul(
                                    out=ps[pb][:, tt * 128:(tt + 1) * 128], lhsT=hb[:, k, tt * 128:(tt + 1) * 128], rhs=wH[:, k, cc:cc + 128],
                                    start=(k == 0), stop=(k == 15)), reads=[t_wH, hbtok], writes=[pst[pb]])
                        if which == 0:
                            src = lambda lo, hi: ps[0][lo:hi, :].rearrange("p (c d) -> p c d", c=4)
                            P.op("vector", lambda e, hh=hh, src=src: e.tensor_copy(out=vt[hh][:], in_=src(0, 128)), reads=[pst[0]], writes=[t_v[hh]])
                            P.op("vector", lambda e, hh=hh, src=src: e.tensor_copy(out=vlo[hh][0:64], in_=src(0, 64)), reads=[pst[0]], writes=[t_v[hh]])
                            P.op("scalar", lambda e, hh=hh, src=src: e.activation(out=vhi[hh][64:128], in_=src(64, 128), func=AF.Copy), reads=[pst[0]], writes=[t_v[hh]])
                        else:
                            P.op("scalar", lambda e, hh=hh: e.activation(out=sg[hh][:], in_=ps[1][:, :].rearrange("p (c d) -> p c d", c=4), func=AF.Silu),
                                 reads=[pst[1]], writes=[t_sg[hh]])
                    if d == 1:
                        P.op("sync", lambda e, hh=hh, nb=nb: e.dma_start(out=ofl[hh][:], in_=of_d[hh, nb * 512:(nb + 1) * 512, :].rearrange("(c p) d -> p c d", p=128)),
                             reads=[t_ofd[hh][nb]], writes=[t_ofl[hh]], dma_sem=s_ofl[hh])
                    pK, pO, pU = 2 + 3 * hh, 3 + 3 * hh, 4 + 3 * hh
                    tiles = range(4) if d == 0 else range(3, -1, -1)
                    for tt in tiles:
                        tsl = slice(tt * 128, (tt + 1) * 128)
                        P.op("tensor", lambda e, hh=hh, tsl=tsl, pK=pK: e.transpose(out=ps[pK][:].bitcast(BF16)[:, 0:128], in_=kt_[hh][:, tsl], identity=identb[:]),
                             reads=[tE[hh]], writes=[pst[pK]])
                        P.op("tensor", lambda e, hh=hh, tsl=tsl, pK=pK: e.matmul(out=ps[pK][:, 256:384], lhsT=kt_[hh][:, tsl], rhs=qfb[hh][:, tsl], start=True, stop=True, skip_group_check=True),
                             reads=[tE[hh]], writes=[pst[pK]])
                        P.op("scalar", lambda e, hh=hh, pK=pK: e.activation(out=kh2[hh][:], in_=ps[pK][:].bitcast(BF16)[:, 0:128], func=AF.Copy), reads=[pst[pK]], writes=[t_kh[hh]])
                        P.op("vector", lambda e, hh=hh, pK=pK, mk=mk: e.tensor_tensor(out=scm[hh][:], in0=ps[pK][:, 256:384], in1=mk[:], op=ALU.mult), reads=[pst[pK]], writes=[t_scm[hh]])
                        order = [(qlo, vlo, 2 * tt), (qhi, vhi, 2 * tt + 1)]
                        if d == 1:
                            order = order[::-1]
                        for oi, (qX, vX, c) in enumerate(order):
                            lastcol = c * 64 + (63 if d == 0 else 0)
                            P.op("tensor", lambda e, hh=hh, tsl=tsl, pO=pO, qX=qX, oi=oi: e.matmul(out=ps[pO][:, 0:128], lhsT=qX[hh][:, tsl], rhs=Sb_[hh][:], start=(oi == 0), stop=False, skip_group_check=True),
                                 reads=[tE[hh], t_S[hh]], writes=[pst[pO]])
                            if oi == 1:
                                P.op("tensor", lambda e, hh=hh, tt=tt, pO=pO: e.matmul(out=ps[pO][:, 0:128], lhsT=scm[hh][:], rhs=vt[hh][:, tt, :], start=False, stop=True, skip_group_check=True),
                                     reads=[t_scm[hh], t_v[hh]], writes=[pst[pO]])
                            P.op("tensor", lambda e, hh=hh, tt=tt, pU=pU, vX=vX: e.matmul(out=ps[pU][:, 0:128], lhsT=kh2[hh][:], rhs=vX[hh][:, tt, :], start=True, stop=True),
                                 reads=[t_kh[hh], t_v[hh]], writes=[pst[pU]])
                            P.op("vector", lambda e, hh=hh, pU=pU: e.tensor_tensor(out=tmpS[hh][:], in0=ps[pU][:, 0:128], in1=Sf[hh][:], op=ALU.add), reads=[pst[pU], t_S[hh]], writes=[t_S[hh]])
                            P.op("vector", lambda e, hh=hh, lastcol=lastcol: e.tensor_scalar(out=Sf[hh][:], in0=tmpS[hh][:], scalar1=eb[hh][:, lastcol:lastcol + 1], scalar2=None, op0=ALU.mult),
                                 reads=[tE[hh]], writes=[t_S[hh]])
                            P.op("vector", lambda e, hh=hh: e.tensor_copy(out=Sb_[hh][:], in_=Sf[hh][:]), writes=[t_S[hh]])
                        P.op("scalar", lambda e, hh=hh, tt=tt, pO=pO: e.activation(out=ob[hh][:, tt, :], in_=ps[pO][:, 0:128], func=AF.Copy), reads=[pst[pO]], writes=[t_ob[hh]])
                    if d == 0:
                        P.op("sync", lambda e, hh=hh, nb=nb: e.dma_start(out=of_d[hh, nb * 512:(nb + 1) * 512, :].rearrange("(c p) d -> p c d", p=128), in_=ob[hh][:]),
                             reads=[t_ob[hh]], writes=[t_ofd[hh][nb]], dma_sem=s_ob[hh])
                    else:
                        P.op("vector", lambda e, hh=hh: e.tensor_tensor(out=ob[hh][:], in0=ob[hh][:], in1=ofl[hh][:], op=ALU.add), reads=[t_ofl[hh]], writes=[t_ob[hh]])
                        P.op("vector", lambda e, hh=hh: e.tensor_tensor(out=junk[hh][:], in0=ob[hh][:], in1=ob[hh][:], op=ALU.mult), reads=[t_ob[hh], tE[hh]], writes=[t_sg[hh]])
                        P.op("vector", lambda e, hh=hh: e.tensor_reduce(out=stat[hh][:], in_=junk[hh][:], axis=AX.X, op=ALU.add), reads=[tE[hh]], writes=[t_sg[hh]])
                        P.op("vector", lambda e, hh=hh: e.tensor_scalar(out=stat[hh][:], in0=stat[hh][:], scalar1=1.0 / 128, scalar2=EPS, op0=ALU.mult, op1=ALU.add), writes=[t_sg[hh]])
                        P.op("scalar", lambda e, hh=hh: e.activation(out=stat[hh][:], in_=stat[hh][:], func=AF.Sqrt), writes=[t_sg[hh]])
                        P.op("vector", lambda e, hh=hh: e.reciprocal(out=stat[hh][:], in_=stat[hh][:]), writes=[t_sg[hh]])
                        for tt in range(4):
                            P.op("vector", lambda e, hh=hh, tt=tt: e.scalar_tensor_tensor(out=ob[hh][:, tt, :], in0=ob[hh][:, tt, :], scalar=stat[hh][:, tt:tt + 1], in1=sg[hh][:, tt, :], op0=ALU.mult, op1=ALU.mult),
                                 reads=[t_sg[hh]], writes=[t_ob[hh]])
                            P.op("vector", lambda e, hh=hh, tt=tt: e.tensor_tensor(out=obb[hh][:, tt, :], in0=ob[hh][:, tt, :], in1=ghgb[:], op=ALU.mult), writes=[t_ob[hh]])
                        P.op("sync", lambda e, hh=hh, nb=nb: e.dma_start(out=mix[nb * 512:(nb + 1) * 512, 256 + hh * 128:256 + (hh + 1) * 128].rearrange("(c p) d -> p c d", p=128), in_=obb[hh][:]),
                             reads=[t_ob[hh]], writes=[tE[hh]], dma_sem=s_ob[hh])
        P.barrier()


def l1_inputs(inputs, core):
    b, j = core // 4, core % 4
    f = lambda a: np.ascontiguousarray(a, dtype=np.float32)
    w_in = inputs["w_in"][0]
    offs = np.cumsum([0, 1024, 1024, 1024, 1024, 1024, 1024, 1024])
    cols = []
    for g in range(8):
        cols.append(w_in[:, offs[g] + j * 256: offs[g] + (j + 1) * 256])
    w_own = np.concatenate(cols, axis=1)
    lb = inputs["hgrn_lb_logits"]
    lbl = np.zeros((128, 2, 2, 2), np.float32)
    for d in range(2):
        for s_ in range(2):
            for hh in range(2):
                lbl[:, d, s_, hh] = lb[d, s_, (2 * j + hh) * 128:(2 * j + hh + 1) * 128]
    m = {
        "xb": f(inputs["x"][b]),
        "cT": f(inputs["c"][b].reshape(16, 128).T),
        "w_ada": f(inputs["w_ada"][0]),
        "b_adaT": f(inputs["b_ada"][0].reshape(96, 128).T),
        "g_mixT": f(inputs["g_mix"][0].reshape(16, 128).T),
        "w_own": f(w_own),
        "gq2": f(np.tile(inputs["g_q"][0], 2).reshape(128, 1)),
        "gk2": f(np.tile(inputs["g_k"][0], 2).reshape(128, 1)),
        "lamv": f(np.concatenate([inputs["lam_q1"][0], inputs["lam_k1"][0], inputs["lam_q2"][0], inputs["lam_k2"][0]]).reshape(1, 256)),
        "rb_own": f(inputs["rel_bias"][:, 2 * j:2 * j + 2]),
        "lbl": f(lbl.reshape(128, 8)),
        "gsub": f(inputs["g_sub"][0].reshape(1, 128)),
        "ghg": f(inputs["g_hgrn"][0].reshape(1, 128)),
    }
    m.update(_consts())
    return m


BIG = 1.0e4


def build_l2a():
    nc = bass.Bass("TRN2", target_bir_lowering=False)
    din = lambda n, s, d=F32: nc.dram_tensor(n, s, d, kind="ExternalInput").ap()
    xo = din("xo", [2048, D])
    cat = din("cat", [2048, D], BF16)
    w_out = din("w_out", [D, D])
    rows = din("rows", [6, D])
    w_router = din("w_router", [D, NE])
    rbias = din("rbias", [1, NE])
    wsg = din("wsg", [D, FF])
    wsu = din("wsu", [D, FF])
    wsd = din("wsd", [FF, D])
    ident = din("ident", [128, 128])
    x1s_o = nc.dram_tensor("x1s", [2048, D], F32, kind="ExternalOutput").ap()
    h2_o = nc.dram_tensor("h2", [2048, D], BF16, kind="ExternalOutput").ap()
    gates_o = nc.dram_tensor("gates", [2048, NE], F32, kind="ExternalOutput").ap()
    with contextlib.ExitStack() as st:
        P = Prog(nc, st)
        ps = [st.enter_context(nc.psum_tensor("ps%d" % i, [128, 512], F32)) for i in range(8)]
        pst = [Tok("ps%d" % i) for i in range(8)]
        sb = lambda n, s, d=F32: st.enter_context(nc.sbuf_tensor(n, s, d))
        identf = sb("identf", [128, 128])
        identb = sb("identb", [128, 128], BF16)
        gt1b = sb("gt1b", [128, D], BF16)
        A2b = sb("A2b", [128, D])
        sh2b = sb("sh2b", [128, D])
        gt2b = sb("gt2b", [128, D], BF16)
        rbb = sb("rbb", [128, NE])
        wo = sb("wo", [128, 16, D], BF16)
        wr = sb("wr", [128, 16, NE])
        wgb = sb("wgb", [128, 16, FF], BF16)
        wub = sb("wub", [128, 16, FF], BF16)
        wdb = sb("wdb", [128, 4, D], BF16)
        t_c = Tok("c")
        cs = P.dsem("q_c")
        ld = lambda dst, src: P.op("sync", lambda e: e.dma_start(out=dst, in_=src), writes=[t_c], dma_sem=cs)
        ld(identf[:], ident[:, :])
        ld(A2b[:], rows[1:2, :].partition_broadcast(128))
        ld(sh2b[:], rows[2:3, :].partition_broadcast(128))
        ld(rbb[:], rbias[0:1, :].partition_broadcast(128))
        ld(wr[:], w_router.rearrange("(k p) n -> p k n", p=128))
        t_wo = Tok("wo")
        with contextlib.ExitStack() as st2:
            st3 = contextlib.ExitStack()
            gfb = st3.enter_context(nc.sbuf_tensor("gfb", [128, D], F32))
            gtmp = st3.enter_context(nc.sbuf_tensor("gtmp", [128, 2, D], F32))
            ld(gfb[:], rows[4:5, :].partition_broadcast(128))
            ld(gtmp[:, 0, :], rows[0:1, :].partition_broadcast(128))
            ld(gtmp[:, 1, :], rows[3:4, :].partition_broadcast(128))
            P.barrier()
            P.op("vector", lambda e: e.tensor_copy(out=gt1b[:], in_=gtmp[:, 0, :]), writes=[t_c])
            P.op("vector", lambda e: e.tensor_copy(out=gt2b[:], in_=gtmp[:, 1, :]), writes=[t_c])
            P.op("vector", lambda e: e.tensor_copy(out=identb[:], in_=identf[:]), writes=[t_c])
            P.op("vector", lambda e: e.scalar_tensor_tensor(out=A2b[:], in0=A2b[:], scalar=1.0, in1=gfb[:], op0=ALU.add, op1=ALU.mult), writes=[t_c])
            P.barrier()
            st3.close()
            wsr = Ring(P, st2, nc, "wso", 2, [128, 16, 256], F32)
            for g in range(8):
                wt, wtok, wsem = wsr.next()
                P.op("sync", lambda e, wt=wt, g=g: e.dma_start(out=wt[:], in_=w_out[:, g * 256:(g + 1) * 256].rearrange("(k p) n -> p k n", p=128)), writes=[wtok], dma_sem=wsem)
                P.op("vector", lambda e, wt=wt, g=g: e.tensor_copy(out=wo[:, :, g * 256:(g + 1) * 256], in_=wt[:]), reads=[wtok], writes=[t_wo])
            for wi, (src, dst) in enumerate([(wsg, wgb), (wsu, wub)]):
                for g in range(2):
                    wt, wtok, wsem = wsr.next()
                    P.op("sync", lambda e, wt=wt, g=g, src=src: e.dma_start(out=wt[:], in_=src[:, g * 256:(g + 1) * 256].rearrange("(k p) n -> p k n", p=128)), writes=[wtok], dma_sem=wsem)
                    P.op("vector", lambda e, wt=wt, g=g, dst=dst: e.tensor_copy(out=dst[:, :, g * 256:(g + 1) * 256], in_=wt[:]), reads=[wtok], writes=[t_wo])
            for fc in range(4):
                wt, wtok, wsem = wsr.next()
                P.op("sync", lambda e, wt=wt, fc=fc: e.dma_start(out=wt[:].rearrange("p k n -> p (k n)")[:, 0:D], in_=wsd[fc * 128:(fc + 1) * 128, :]), writes=[wtok], dma_sem=wsem)
                P.op("vector", lambda e, wt=wt, fc=fc: e.tensor_copy(out=wdb[:, fc, :], in_=wt[:].rearrange("p k n -> p (k n)")[:, 0:D]), reads=[wtok], writes=[t_wo])
            P.barrier()
        cbr = Ring(P, st, nc, "cb", 1, [128, D], BF16)
        ctr = Ring(P, st, nc, "ct", 1, [128, 16, 128], BF16)
        x1r = Ring(P, st, nc, "x1", 1, [128, D], F32)
        h2r = Ring(P, st, nc, "h2f", 1, [128, D], F32)
        h2Tr = Ring(P, st, nc, "h2T", 1, [128, 16, 128], F32)
        gtr = Ring(P, st, nc, "gt", 2, [128, NE], F32)
        hTr = Ring(P, st, nc, "hTs", 1, [128, 4, 128], BF16)
        st_ = sb("st_", [128, 8])
        scs = sb("scs", [128, NE])
        bis = sb("bis", [128, NE])
        mskd = sb("mskd", [128, NE])
        m8 = sb("m8", [128, 8, 8])
        gs = sb("gs", [128, 8])
        gm8 = sb("gm8", [128, 8])
        pen = sb("pen", [128, 8])
        t8 = sb("t8", [128, 8])
        sgl = sb("sgl", [128, 512])
        t_r = Tok("route")
        t_junk = Tok("junk")
        t_sgl = Tok("sgl")
        for i in range(16):
            rsl = slice(i * 128, (i + 1) * 128)
            cb_, cbtok, cbsem = cbr.next()
            h2b, h2btok, h2bsem = cb_, cbtok, cbsem
            junk = cb_
            cT, cTtok, _ = ctr.next()
            h2Tb, h2Tbtok = cT, cTtok
            x1, x1tok, x1sem = x1r.next()
            P.op("sync", lambda e, x1=x1, rsl=rsl: e.dma_start(out=x1[:], in_=xo[rsl, :]), writes=[x1tok], dma_sem=x1sem)
            h2, h2tok, _ = h2r.next()
            h2T, h2Ttok, _ = h2Tr.next()
            gt, gttok, gtsem = gtr.next()
            hT, hTtok, _ = hTr.next()
            P.op("sync", lambda e, cb_=cb_, rsl=rsl: e.dma_start(out=cb_[:], in_=cat[rsl, :]), writes=[cbtok], dma_sem=cbsem)
            for half in range(2):
                pb = half
                for c8 in range(8):
                    c = half * 8 + c8
                    P.op("tensor", lambda e, cb_=cb_, c=c, c8=c8, pb=pb: e.transpose(out=ps[pb][:].bitcast(BF16)[:, c8 * 128:(c8 + 1) * 128], in_=cb_[:, c * 128:(c + 1) * 128], identity=identb[:]),
                         reads=[cbtok], writes=[pst[pb]])
                P.op("scalar", lambda e, cT=cT, half=half, pb=pb: e.activation(out=cT[:, half * 8:(half + 1) * 8, :], in_=ps[pb][:].bitcast(BF16).rearrange("p (c t) -> p c t", c=8), func=AF.Copy),
                     reads=[pst[pb]], writes=[cTtok])
            for cbk in range(4):
                pb = 2 + cbk
                csl = slice(cbk * 512, (cbk + 1) * 512)
                for k in range(16):
                    P.op("tensor", lambda e, cT=cT, k=k, csl=csl, pb=pb: e.matmul(out=ps[pb][:, :], lhsT=cT[:, k, :], rhs=wo[:, k, csl], start=(k == 0), stop=(k == 15)),
                         reads=[cTtok, t_wo], writes=[pst[pb]])
                P.op("vector", lambda e, h2=h2, pb=pb, csl=csl: e.tensor_tensor(out=h2[:, csl], in0=ps[pb][:, :], in1=gt1b[:, csl], op=ALU.mult), reads=[pst[pb]], writes=[h2tok])
                P.op("gpsimd", lambda e, x1=x1, h2=h2, csl=csl: e.tensor_tensor(out=x1[:, csl], in0=x1[:, csl], in1=h2[:, csl], op=ALU.add), reads=[h2tok], writes=[x1tok])
            P.op("scalar", lambda e, x1=x1: e.activation(out=junk[:], in_=x1[:], func=AF.Square, accum_out=st_[:, 0:1]), reads=[x1tok], writes=[cbtok, t_r])
            P.op("vector", lambda e: e.tensor_scalar(out=st_[:, 0:1], in0=st_[:, 0:1], scalar1=1.0 / D, scalar2=EPS, op0=ALU.mult, op1=ALU.add), writes=[t_r])
            P.op("scalar", lambda e: e.activation(out=st_[:, 0:1], in_=st_[:, 0:1], func=AF.Sqrt), writes=[t_r])
            P.op("vector", lambda e: e.reciprocal(out=st_[:, 1:2], in_=st_[:, 0:1]), writes=[t_r])
            P.op("vector", lambda e, x1=x1, h2=h2: e.scalar_tensor_tensor(out=h2[:], in0=x1[:], scalar=st_[:, 1:2], in1=A2b[:], op0=ALU.mult, op1=ALU.mult), reads=[x1tok, t_r], writes=[h2tok])
            P.op("gpsimd", lambda e, h2=h2: e.tensor_tensor(out=h2[:], in0=h2[:], in1=sh2b[:], op=ALU.add), writes=[h2tok])
            P.op("scalar", lambda e, h2=h2, h2b=h2b: e.activation(out=h2b[:], in_=h2[:], func=AF.Copy), reads=[h2tok], writes=[h2btok])
            P.op("sync", lambda e, h2b=h2b, rsl=rsl: e.dma_start(out=h2_o[rsl, :], in_=h2b[:]), reads=[h2btok], dma_sem=h2bsem)
            for q4 in range(4):
                pb = 2 + q4
                for c4 in range(4):
                    c = q4 * 4 + c4
                    P.op("tensor", lambda e, h2=h2, c=c, c4=c4, pb=pb: e.transpose(out=ps[pb][:, c4 * 128:(c4 + 1) * 128], in_=h2[:, c * 128:(c + 1) * 128], identity=identf[:]),
                         reads=[h2tok], writes=[pst[pb]])
                eng = "vector" if q4 % 2 == 0 else "scalar"
                if eng == "vector":
                    P.op("vector", lambda e, h2T=h2T, q4=q4, pb=pb: e.tensor_copy(out=h2T[:, q4 * 4:(q4 + 1) * 4, :], in_=ps[pb][:, :].rearrange("p (c t) -> p c t", c=4)), reads=[pst[pb]], writes=[h2Ttok])
                else:
                    P.op("scalar", lambda e, h2T=h2T, q4=q4, pb=pb: e.activation(out=h2T[:, q4 * 4:(q4 + 1) * 4, :], in_=ps[pb][:, :].rearrange("p (c t) -> p c t", c=4), func=AF.Copy), reads=[pst[pb]], writes=[h2Ttok])
            P.op("gpsimd", lambda e, h2T=h2T, h2Tb=h2Tb: e.tensor_copy(out=h2Tb[:], in_=h2T[:]), reads=[h2Ttok], writes=[h2Tbtok])
            for k in range(16):
                P.op("tensor", lambda e, h2T=h2T, k=k: e.matmul(out=ps[6][:, 0:NE], lhsT=h2T[:, k, :], rhs=wr[:, k, :], start=(k == 0), stop=(k == 15)), reads=[h2Ttok], writes=[pst[6]])
            P.op("scalar", lambda e: e.activation(out=scs[:], in_=ps[6][:, 0:NE], func=AF.Sigmoid), reads=[pst[6]], writes=[t_r])
            P.op("vector", lambda e: e.tensor_tensor(out=bis[:], in0=scs[:], in1=rbb[:], op=ALU.add), writes=[t_r])
            for g in range(8):
                P.op("vector", lambda e, g=g: e.max(out=m8[:, g, :], in_=bis[:, g * 32:(g + 1) * 32]), writes=[t_r])
            P.op("vector", lambda e: e.tensor_tensor(out=gs[:], in0=m8[:, :, 0], in1=m8[:, :, 1], op=ALU.add), writes=[t_r])
            P.op("vector", lambda e: e.max(out=gm8[:], in_=gs[:]), writes=[t_r])
            P.op("vector", lambda e: e.tensor_scalar(out=pen[:], in0=gs[:], scalar1=gm8[:, 3:4], scalar2=None, op0=ALU.is_ge), writes=[t_r])
            P.op("vector", lambda e: e.tensor_scalar(out=pen[:], in0=pen[:], scalar1=BIG, scalar2=-BIG, op0=ALU.mult, op1=ALU.add), writes=[t_r])
            for g in range(8):
                P.op("vector", lambda e, g=g: e.tensor_scalar(out=mskd[:, g * 32:(g + 1) * 32], in0=bis[:, g * 32:(g + 1) * 32], scalar1=pen[:, g:g + 1], scalar2=None, op0=ALU.add), writes=[t_r])
            P.op("vector", lambda e: e.max(out=t8[:], in_=mskd[:]), writes=[t_r])
            P.op("vector", lambda e: e.tensor_scalar(out=mskd[:], in0=mskd[:], scalar1=t8[:, 7:8], scalar2=None, op0=ALU.is_ge), writes=[t_r])
            P.op("vector", lambda e: e.tensor_tensor(out=mskd[:], in0=mskd[:], in1=scs[:], op=ALU.mult), writes=[t_r])
            P.op("vector", lambda e: e.tensor_reduce(out=st_[:, 2:3], in_=mskd[:], axis=AX.X, op=ALU.add), writes=[t_r])
            P.op("vector", lambda e: e.reciprocal(out=st_[:, 3:4], in_=st_[:, 2:3]), writes=[t_r])
            P.op("vector", lambda e, gt=gt: e.tensor_scalar(out=gt[:], in0=mskd[:], scalar1=st_[:, 3:4], scalar2=2.5, op0=ALU.mult, op1=ALU.mult), reads=[t_r], writes=[gttok])
            P.op("sync", lambda e, gt=gt, rsl=rsl: e.dma_start(out=gates_o[rsl, :], in_=gt[:]), reads=[gttok], dma_sem=gtsem)
            for wi, (wsrc, pb) in enumerate([(wgb, 6), (wub, 7)]):
                for ft in range(4):
                    for k in range(16):
                        P.op("tensor", lambda e, h2Tb=h2Tb, wsrc=wsrc, ft=ft, k=k, pb=pb: e.matmul(out=ps[pb][:, ft * 128:(ft + 1) * 128], lhsT=wsrc[:, k, ft * 128:(ft + 1) * 128], rhs=h2Tb[:, k, :],
                                                                                                 start=(k == 0), stop=(k == 15)), reads=[h2Tbtok, t_wo], writes=[pst[pb]])
            P.op("scalar", lambda e: e.activation(out=sgl[:], in_=ps[6][:, :], func=AF.Silu), reads=[pst[6]], writes=[t_sgl])
            P.op("vector", lambda e, hT=hT: e.tensor_tensor(out=hT[:].rearrange("p f t -> p (f t)"), in0=sgl[:], in1=ps[7][:, :], op=ALU.mult), reads=[pst[7], t_sgl], writes=[hTtok])
            for cbk in range(4):
                pb = 2 + cbk
                csl = slice(cbk * 512, (cbk + 1) * 512)
                for fc in range(4):
                    P.op("tensor", lambda e, hT=hT, fc=fc, csl=csl, pb=pb: e.matmul(out=ps[pb][:, :], lhsT=hT[:, fc, :], rhs=wdb[:, fc, csl], start=(fc == 0), stop=(fc == 3)),
                         reads=[hTtok, t_wo], writes=[pst[pb]])
                P.op("vector", lambda e, h2=h2, pb=pb, csl=csl: e.tensor_tensor(out=h2[:, csl], in0=ps[pb][:, :], in1=gt2b[:, csl], op=ALU.mult), reads=[pst[pb]], writes=[h2tok])
                P.op("gpsimd", lambda e, x1=x1, h2=h2, csl=csl: e.tensor_tensor(out=x1[:, csl], in0=x1[:, csl], in1=h2[:, csl], op=ALU.add), reads=[h2tok], writes=[x1tok])
            P.op("sync", lambda e, x1=x1, rsl=rsl: e.dma_start(out=x1s_o[rsl, :], in_=x1[:]), reads=[x1tok], dma_sem=x1sem)
        P.barrier()
        P.emit()
    return nc


NTOK = 2 * S
NEL = 32


def build_l2b(nblk=NTOK // 512, nel=NEL):
    nc = bass.Bass("TRN2", target_bir_lowering=False)
    din = lambda n, s, d=F32: nc.dram_tensor(n, s, d, kind="ExternalInput").ap()
    h2a = din("h2a", [NTOK, D], BF16)
    gto = din("gto", [NTOK, NEL])
    wg = din("wg", [NEL, D, FF])
    wu = din("wu", [NEL, D, FF])
    wd = din("wd", [NEL, FF, D])
    ident = din("ident", [128, 128])
    part = nc.dram_tensor("part", [NTOK, D], F32, kind="ExternalOutput").ap()
    wgb_d = nc.dram_tensor("wgb_d", [NEL, 128, 16, FF], BF16).ap()
    wub_d = nc.dram_tensor("wub_d", [NEL, 128, 16, FF], BF16).ap()
    wdb_d = nc.dram_tensor("wdb_d", [NEL, 128, 4, D], BF16).ap()
    with contextlib.ExitStack() as st:
        P = Prog(nc, st)
        ps = [st.enter_context(nc.psum_tensor("ps%d" % i, [128, 512], F32)) for i in range(8)]
        pst = [Tok("ps%d" % i) for i in range(8)]
        sb = lambda n, s, d=F32: st.enter_context(nc.sbuf_tensor(n, s, d))
        identf = sb("identf", [128, 128])
        identb = sb("identb", [128, 128], BF16)
        t_c = Tok("c")
        P.op("sync", lambda e: e.dma_start(out=identf[:], in_=ident[:, :]), writes=[t_c], dma_sem=P.dsem("q_c"))
        P.op("vector", lambda e: e.tensor_copy(out=identb[:], in_=identf[:]), reads=[t_c], writes=[t_c])
        t_wd = [[Tok("wd") for _ in range(3)] for _ in range(nel)]
        with contextlib.ExitStack() as st2:
            stg = Ring(P, st2, nc, "stg", 3, [128, 8192], F32)
            cst = Ring(P, st2, nc, "cst", 3, [128, 8192], BF16)
            ci = 0
            for e_ in range(nel):
                for mi in range(3):
                    sg_, sgtok, sgsem = stg.next()
                    cb_, cbtok, cbsem = cst.next()
                    if mi < 2:
                        src = (wg, wu)[mi][e_].rearrange("(k p) f -> p k f", p=128)
                        dst = (wgb_d, wub_d)[mi][e_]
                        view = lambda t: t[:].rearrange("p (k f) -> p k f", k=16)
                    else:
                        src = wd[e_].rearrange("(k p) n -> p k n", p=128)
                        dst = wdb_d[e_]
                        view = lambda t: t[:].rearrange("p (k n) -> p k n", k=4)
                    P.op("sync", lambda e, sg_=sg_, src=src, view=view: e.dma_start(out=view(sg_), in_=src), writes=[sgtok], dma_sem=sgsem)
                    ceng = ("vector", "gpsimd", "scalar")[ci % 3]
                    ci += 1
                    if ceng == "scalar":
                        P.op("scalar", lambda e, sg_=sg_, cb_=cb_: e.activation(out=cb_[:], in_=sg_[:], func=AF.Copy), reads=[sgtok], writes=[cbtok])
                    else:
                        P.op(ceng, lambda e, sg_=sg_, cb_=cb_: e.tensor_copy(out=cb_[:], in_=sg_[:]), reads=[sgtok], writes=[cbtok])
                    P.op("sync", lambda e, cb_=cb_, dst=dst, view=view: e.dma_start(out=dst, in_=view(cb_)), reads=[cbtok], writes=[t_wd[e_][mi]], dma_sem=cbsem)
            P.barrier()
        h2r = Ring(P, st, nc, "h2t", 1, [128, 4, D], BF16)
        h2Tr = Ring(P, st, nc, "h2T", 2, [128, 16, 512], BF16)
        gr = Ring(P, st, nc, "gr", 2, [128, 4, NEL], F32)
        accr = Ring(P, st, nc, "acc", 1, [128, 4, D], F32)
        wgr = Ring(P, st, nc, "wgr", 2, [128, 16, FF], BF16)
        wur = Ring(P, st, nc, "wur", 2, [128, 16, FF], BF16)
        wdr = Ring(P, st, nc, "wdr", 2, [128, 4, D], BF16)
        hTr = Ring(P, st, nc, "hT", 2, [128, 4, 512], BF16)
        sglr = Ring(P, st, nc, "sgl", 2, [128, 512], F32)
        for nb in range(nblk):
            h2t, h2tok, h2sem = h2r.next()
            h2T, h2Ttok, _ = h2Tr.next()
            g_, gtok, gsem = gr.next()
            acc, acctok, accsem = accr.next()
            rows = slice(nb * 512, (nb + 1) * 512)
            P.op("sync", lambda e, h2t=h2t, rows=rows: e.dma_start(out=h2t[:], in_=h2a[rows, :].rearrange("(t p) d -> p t d", p=128)), writes=[h2tok], dma_sem=h2sem)
            P.op("sync", lambda e, g_=g_, rows=rows: e.dma_start(out=g_[:], in_=gto[rows, :].rearrange("(t p) n -> p t n", p=128)), writes=[gtok], dma_sem=gsem)
            P.op("gpsimd", lambda e, acc=acc: e.memset(acc[:], 0.0), writes=[acctok])
            for tt in range(4):
                for half in range(2):
                    pb = half
                    for c8 in range(8):
                        c = half * 8 + c8
                        P.op("tensor", lambda e, h2t=h2t, tt=tt, c=c, c8=c8, pb=pb: e.transpose(out=ps[pb][:].bitcast(BF16)[:, c8 * 128:(c8 + 1) * 128], in_=h2t[:, tt, c * 128:(c + 1) * 128], identity=identb[:]),
                             reads=[h2tok], writes=[pst[pb]])
                    P.op("scalar", lambda e, h2T=h2T, tt=tt, half=half, pb=pb: e.activation(out=h2T[:, half * 8:(half + 1) * 8, tt * 128:(tt + 1) * 128], in_=ps[pb][:].bitcast(BF16).rearrange("p (c t) -> p c t", c=8), func=AF.Copy),
                         reads=[pst[pb]], writes=[h2Ttok])
            for e_ in range(nel):
                wgt, wgtok, wgsem = wgr.next()
                wut, wutok, wusem = wur.next()
                wdt, wdtok, wdsem = wdr.next()
                P.op("sync", lambda e, wgt=wgt, e_=e_: e.dma_start(out=wgt[:], in_=wgb_d[e_]), reads=[t_wd[e_][0]], writes=[wgtok], dma_sem=wgsem)
                P.op("sync", lambda e, wut=wut, e_=e_: e.dma_start(out=wut[:], in_=wub_d[e_]), reads=[t_wd[e_][1]], writes=[wutok], dma_sem=wusem)
                P.op("sync", lambda e, wdt=wdt, e_=e_: e.dma_start(out=wdt[:], in_=wdb_d[e_]), reads=[t_wd[e_][2]], writes=[wdtok], dma_sem=wdsem)
                hT, hTtok, _ = hTr.next()
                for ft in range(4):
                    pg, pu = 2 + (ft % 2) * 2, 3 + (ft % 2) * 2
                    fsl = slice(ft * 128, (ft + 1) * 128)
                    for k in range(16):
                        P.op("tensor", lambda e, wgt=wgt, h2T=h2T, k=k, fsl=fsl, pg=pg: e.matmul(out=ps[pg][:, :], lhsT=wgt[:, k, fsl], rhs=h2T[:, k, :], start=(k == 0), stop=(k == 15)),
                             reads=[wgtok, h2Ttok], writes=[pst[pg]])
                    for k in range(16):
                        P.op("tensor", lambda e, wut=wut, h2T=h2T, k=k, fsl=fsl, pu=pu: e.matmul(out=ps[pu][:, :], lhsT=wut[:, k, fsl], rhs=h2T[:, k, :], start=(k == 0), stop=(k == 15)),
                             reads=[wutok, h2Ttok], writes=[pst[pu]])
                    sgl, sgltok, _ = sglr.next()
                    P.op("scalar", lambda e, sgl=sgl, pg=pg: e.activation(out=sgl[:], in_=ps[pg][:, :], func=AF.Silu), reads=[pst[pg]], writes=[sgltok])
                    P.op("vector", lambda e, hT=hT, ft=ft, sgl=sgl, pu=pu: e.tensor_tensor(out=hT[:, ft, :], in0=sgl[:], in1=ps[pu][:, :], op=ALU.mult), reads=[pst[pu], sgltok], writes=[hTtok])
                for tt in range(4):
                    for cbk in range(4):
                        pb = 6 + ((tt * 4 + cbk) % 2)
                        csl = slice(cbk * 512, (cbk + 1) * 512)
                        for fc in range(4):
                            P.op("tensor", lambda e, hT=hT, wdt=wdt, tt=tt, fc=fc, csl=csl, pb=pb: e.matmul(out=ps[pb][:, :], lhsT=hT[:, fc, tt * 128:(tt + 1) * 128], rhs=wdt[:, fc, csl], start=(fc == 0), stop=(fc == 3)),
                                 reads=[hTtok, wdtok], writes=[pst[pb]])
                        P.op("vector", lambda e, acc=acc, g_=g_, tt=tt, csl=csl, pb=pb, e_=e_: e.scalar_tensor_tensor(out=acc[:, tt, csl], in0=ps[pb][:, :], scalar=g_[:, tt, e_:e_ + 1], in1=acc[:, tt, csl], op0=ALU.mult, op1=ALU.add),
                             reads=[pst[pb], gtok], writes=[acctok])
            P.op("sync", lambda e, acc=acc, rows=rows: e.dma_start(out=part[rows, :].rearrange("(t p) d -> p t d", p=128), in_=acc[:]), reads=[acctok], dma_sem=accsem)
        P.barrier()
        P.emit()
    return nc


def build_l3():
    nc = bass.Bass("TRN2", target_bir_lowering=False)
    din = lambda n, s, d=F32: nc.dram_tensor(n, s, d, kind="ExternalInput").ap()
    x1s = din("x1s", [2048, D])
    parts = din("parts", [8, 2048, D])
    gt2 = din("gt2", [1, D])
    out = nc.dram_tensor("out", [2048, D], F32, kind="ExternalOutput").ap()
    with contextlib.ExitStack() as st:
        P = Prog(nc, st)
        sb = lambda n, s, d=F32: st.enter_context(nc.sbuf_tensor(n, s, d))
        gt2b = sb("gt2b", [128, D])
        t_c = Tok("c")
        P.op("sync", lambda e: e.dma_start(out=gt2b[:], in_=gt2[0:1, :].partition_broadcast(128)), writes=[t_c], dma_sem=P.dsem("q_c"))
        pr = Ring(P, st, nc, "pr", 4, [128, D], F32)
        xr = Ring(P, st, nc, "xr", 2, [128, D], F32)
        ar = Ring(P, st, nc, "ar", 2, [128, D], F32)
        for i in range(16):
            rsl = slice(i * 128, (i + 1) * 128)
            xt, xtok, xsem = xr.next()
            acc, acctok, accsem = ar.next()
            P.op("sync", lambda e, xt=xt, rsl=rsl: e.dma_start(out=xt[:], in_=x1s[rsl, :]), writes=[xtok], dma_sem=xsem)
            for c in range(8):
                pt, ptok, psem = pr.next()
                P.op("sync", lambda e, pt=pt, c=c, rsl=rsl: e.dma_start(out=pt[:], in_=parts[c, rsl, :]), writes=[ptok], dma_sem=psem)
                eng = "vector" if c % 2 == 0 else "gpsimd"
                if c == 0:
                    P.op("vector", lambda e, acc=acc, pt=pt: e.tensor_copy(out=acc[:], in_=pt[:]), reads=[ptok], writes=[acctok])
                else:
                    P.op(eng, lambda e, acc=acc, pt=pt: e.tensor_tensor(out=acc[:], in0=acc[:], in1=pt[:], op=ALU.add), reads=[ptok], writes=[acctok])
            P.op("vector", lambda e, acc=acc: e.tensor_tensor(out=acc[:], in0=acc[:], in1=gt2b[:], op=ALU.mult), reads=[t_c], writes=[acctok])
            P.op("gpsimd", lambda e, acc=acc, xt=xt: e.tensor_tensor(out=acc[:], in0=acc[:], in1=xt[:], op=ALU.add), reads=[xtok], writes=[acctok])
            P.op("sync", lambda e, acc=acc, rsl=rsl: e.dma_start(out=out[rsl, :], in_=acc[:]), reads=[acctok], dma_sem=accsem)
        P.barrier()
        P.emit()
    return nc


def _run(nc, maps):
    return run_bass_kernel_spmd(nc, maps, core_ids=list(range(len(maps)))).results


def run_l1(inputs):
    r1 = _run(build_l1(), [l1_inputs(inputs, c) for c in range(8)])
    cat = np.zeros((2, S, D), r1[0]["mix"].dtype)
    for c in range(8):
        b, j = c // 4, c % 4
        m = r1[c]["mix"]
        cat[b, :, j * 256:(j + 1) * 256] = m[:, 0:256]
        cat[b, :, 1024 + j * 256:1024 + (j + 1) * 256] = m[:, 256:512]
    mods = [r1[0]["modrow"].reshape(-1), r1[4]["modrow"].reshape(-1)]
    return cat, mods


def run_l2a(inputs, cat, mods):
    f = lambda a: np.ascontiguousarray(a, dtype=np.float32)
    ident = np.eye(128, dtype=np.float32)
    maps = []
    for c in range(8):
        b, r = c // 4, c % 4
        mod = mods[b]
        rows = np.stack([mod[2 * D:3 * D], mod[4 * D:5 * D], mod[3 * D:4 * D], mod[5 * D:6 * D], inputs["g_ffn"][0], np.zeros(D, np.float32)])
        maps.append({
            "xo": f(inputs["x"][b, r * 2048:(r + 1) * 2048]), "cat": np.ascontiguousarray(cat[b, r * 2048:(r + 1) * 2048]),
            "w_out": f(inputs["w_out"][0]), "rows": f(rows), "w_router": f(inputs["w_router"][0]),
            "rbias": f(inputs["router_bias"][0].reshape(1, NE)), "wsg": f(inputs["w_sh_gate"][0]), "wsu": f(inputs["w_sh_up"][0]),
            "wsd": f(inputs["w_sh_down"][0]), "ident": ident,
        })
    r = _run(build_l2a(), maps)
    x1s = np.concatenate([r[c]["x1s"] for c in range(8)], 0)
    h2 = np.concatenate([r[c]["h2"] for c in range(8)], 0)
    gates = np.concatenate([r[c]["gates"] for c in range(8)], 0)
    return x1s, h2, gates


def run_l2b(inputs, h2, gates):
    f = lambda a: np.ascontiguousarray(a, dtype=np.float32)
    ident = np.eye(128, dtype=np.float32)
    maps = []
    for c in range(8):
        es = slice(c * NEL, (c + 1) * NEL)
        maps.append({"h2a": h2, "gto": f(gates[:, es]), "wg": f(inputs["w_exp_gate"][0, es]), "wu": f(inputs["w_exp_up"][0, es]),
                     "wd": f(inputs["w_exp_down"][0, es]), "ident": ident})
    r = _run(build_l2b(), maps)
    return [r[c]["part"] for c in range(8)]


def run_l3(x1s, parts, mods):
    maps = []
    for c in range(8):
        b = c // 4
        rs = slice(c * 2048, (c + 1) * 2048)
        maps.append({"x1s": np.ascontiguousarray(x1s[rs]), "parts": np.ascontiguousarray(np.stack([p[rs] for p in parts])),
                     "gt2": np.ascontiguousarray(mods[b][5 * D:6 * D].reshape(1, D))})
    r = _run(build_l3(), maps)
    return np.concatenate([r[c]["out"] for c in range(8)], 0).reshape(2, S, D)


def kernel(**inputs):
    cat, mods = run_l1(inputs)
    x1s, h2, gates = run_l2a(inputs, cat, mods)
    parts = run_l2b(inputs, h2, gates)
    return run_l3(x1s, parts, mods)
```

```python
import contextlib
import math
import numpy as np
import concourse.bass as bass
import concourse.mybir as mybir
from concourse.bass_utils import run_bass_kernel_spmd

F32 = mybir.dt.float32
BF16 = mybir.dt.bfloat16
I32 = mybir.dt.int32
U32 = mybir.dt.uint32
AF = mybir.ActivationFunctionType
ALU = mybir.AluOpType
AX = mybir.AxisListType

D = 2048
S = 8192
EPS = 1e-6
NE = 256
FF = 512
CAP = 256
NSL = CAP // 128
ENGS = ("tensor", "vector", "scalar", "gpsimd", "sync")


class Tok:
    __slots__ = ("name", "last_w", "readers")

    def __init__(self, name=""):
        self.name = name
        self.last_w = None
        self.readers = []


class Op:
    __slots__ = ("eng", "fn", "deps", "dma_sem", "dma_val", "sig", "needs_sig", "idx")

    def __init__(self, eng, fn):
        self.eng = eng
        self.fn = fn
        self.deps = []
        self.dma_sem = None
        self.dma_val = 0
        self.sig = 0
        self.needs_sig = False


class Prog:
    def __init__(self, nc, stack):
        self.nc = nc
        self.stack = stack
        self.ops = []
        self.sems = {e: stack.enter_context(nc.semaphore("s_" + e)) for e in ENGS}
        self.dma_sem_count = {}
        self.dma_since_barrier = []
        self.nsem = 0

    def dsem(self, name):
        s = self.stack.enter_context(self.nc.semaphore(name))
        self.dma_sem_count[id(s)] = 0
        self.nsem += 1
        return s

    def op(self, eng, fn, reads=(), writes=(), dma_sem=None, extra_deps=()):
        o = Op(eng, fn)
        deps = []
        for r in reads:
            if r.last_w is not None:
                deps.append(r.last_w)
            r.readers.append(o)
        for w in writes:
            if w.last_w is not None:
                deps.append(w.last_w)
            deps.extend(w.readers)
            w.last_w = o
            w.readers = []
        deps.extend(extra_deps)
        seen = set()
        dd = []
        for d in deps:
            if d is o or id(d) in seen:
                continue
            seen.add(id(d))
            dd.append(d)
        best = {}
        keep = []
        for d in dd:
            if d.dma_sem is not None:
                keep.append(d)
            elif d.eng not in best or best[d.eng].idx < d.idx:
                best[d.eng] = d
        o.deps = keep + list(best.values())
        o.idx = len(self.ops)
        if dma_sem is not None:
            o.dma_sem = dma_sem
            self.dma_sem_count[id(dma_sem)] += 16
            o.dma_val = self.dma_sem_count[id(dma_sem)]
            self.dma_since_barrier.append(o)
        self.ops.append(o)
        return o

    def barrier(self):
        lasts = []
        for e in ENGS:
            for o in reversed(self.ops):
                if o.eng == e and o.dma_sem is None and o.fn is not None:
                    lasts.append(o)
                    break
        dmas = list(self.dma_since_barrier)
        self.dma_since_barrier = []
        for e in ENGS:
            self.op(e, None, extra_deps=lasts + dmas)

    def emit(self):
        nc = self.nc
        for o in self.ops:
            for d in o.deps:
                if d.dma_sem is None:
                    if d.eng == "tensor" and o.eng == "tensor":
                        continue
                    d.needs_sig = True
        counters = {e: 0 for e in ENGS}
        per_eng = {e: [] for e in ENGS}
        for o in self.ops:
            if o.dma_sem is None and o.needs_sig:
                counters[o.eng] += 1
                o.sig = counters[o.eng]
            per_eng[o.eng].append(o)
        sems = self.sems

        def run_engine(ename, eng):
            waited = {}
            for o in per_eng[ename]:
                for d in o.deps:
                    if d.dma_sem is not None:
                        key, sem, val = id(d.dma_sem), d.dma_sem, d.dma_val
                    else:
                        if d.eng == "tensor" and ename == "tensor":
                            continue
                        key, sem, val = d.eng, sems[d.eng], d.sig
                    if waited.get(key, 0) >= val:
                        continue
                    waited[key] = val
                    eng.wait_ge(sem, val)
                if o.fn is None:
                    continue
                inst = o.fn(eng)
                if o.dma_sem is not None:
                    inst.then_inc(o.dma_sem, 16)
                elif o.needs_sig:
                    inst.then_inc(sems[ename], 1)

        with nc.Block() as block:
            @block.tensor
            def _(e):
                run_engine("tensor", e)

            @block.vector
            def _(e):
                run_engine("vector", e)

            @block.scalar
            def _(e):
                run_engine("scalar", e)

            @block.gpsimd
            def _(e):
                run_engine("gpsimd", e)

            @block.sync
            def _(e):
                run_engine("sync", e)


class Ring:
    def __init__(self, P, st, nc, name, n, shape, dtype):
        self.t = [st.enter_context(nc.sbuf_tensor("%s%d" % (name, i), shape, dtype)) for i in range(n)]
        self.tok = [Tok("%s%d" % (name, i)) for i in range(n)]
        self.sem = [P.dsem("q_%s%d" % (name, i)) for i in range(n)]
        self.n = n
        self.i = -1

    def next(self):
        self.i = (self.i + 1) % self.n
        return self.t[self.i], self.tok[self.i], self.sem[self.i]


def _t5_bucket_np(rel):
    half, me = 16, 8
    ret = np.where(rel > 0, half, 0)
    n = np.abs(rel)
    nf = np.maximum(n, 1).astype(np.float32)
    large = me + (np.log(nf / np.float32(me)) / np.float32(math.log(128 / 8)) * np.float32(half - me)).astype(np.int32)
    large = np.minimum(large, half - 1)
    return ret + np.where(n < me, n, large)


def _consts():
    c = {}
    c["ident"] = np.eye(128, dtype=np.float32)
    c["antiid"] = np.eye(128, dtype=np.float32)[::-1].copy()
    ob = np.zeros((128, 128), np.float32)
    ob[:64, :64] = 1.0 / 64
    ob[64:, 64:] = 1.0 / 64
    c["onesblk"] = ob
    m = np.arange(1280)
    b = _t5_bucket_np(639 - m)
    oh = np.zeros((32, 1280), np.float32)
    oh[b, m] = 1.0
    c["ohg"] = oh
    s = np.arange(128)
    same = (s[:, None] // 64) == (s[None, :] // 64)
    c["mask_f"] = (same & (s[:, None] <= s[None, :])).astype(np.float32)
    c["mask_b"] = (same & (s[:, None] >= s[None, :])).astype(np.float32)
    return c


def build_l1(dbg=False):
    nc = bass.Bass("TRN2", target_bir_lowering=False)
    din = lambda n, s, d=F32: nc.dram_tensor(n, s, d, kind="ExternalInput").ap()
    xb = din("xb", [S, D] if dbg != "H" else [128, 128])
    cT = din("cT", [128, 16])
    w_ada = din("w_ada", [D, 6 * D] if dbg != "H" else [128, 128])
    b_adaT = din("b_adaT", [128, 96])
    g_mixT = din("g_mixT", [128, 16])
    w_own = din("w_own", [D, 2048])
    gq2 = din("gq2", [128, 1])
    gk2 = din("gk2", [128, 1])
    lamv = din("lamv", [1, 256])
    rb_own = din("rb_own", [32, 2])
    lbl = din("lbl", [128, 8])
    gsub = din("gsub", [1, 128])
    ghg = din("ghg", [1, 128])
    ident = din("ident", [128, 128])
    antiid = din("antiid", [128, 128])
    onesblk = din("onesblk", [128, 128])
    ohg = din("ohg", [32, 1280])
    mask_f = din("mask_f", [128, 128])
    mask_b = din("mask_b", [128, 128])
    mix = nc.dram_tensor("mix", [S, 512], BF16, kind="ExternalOutput").ap()
    modrow = nc.dram_tensor("modrow", [96, 128], F32, kind="ExternalOutput").ap()
    hT_d = nc.dram_tensor("hT_d", [128, 16, S], BF16).ap()
    G_d = nc.dram_tensor("G_d", [2, 1280], F32).ap()
    of_d = nc.dram_tensor("of_d", [2, S, 128], F32).ap()

    with contextlib.ExitStack() as st0:
        P = Prog(nc, st0)
        ps = [st0.enter_context(nc.psum_tensor("ps%d" % i, [128, 512], F32)) for i in range(8)]
        pst = [Tok("ps%d" % i) for i in range(8)]
        sb0 = lambda n, s, d=F32: st0.enter_context(nc.sbuf_tensor(n, s, d))
        identf = sb0("identf", [128, 128])
        identb = sb0("identb", [128, 128], BF16)
        antif = sb0("antif", [128, 128])
        onesb = sb0("onesb", [128, 128])
        modT = sb0("modT", [128, 96])
        A1 = sb0("A1", [128, 16])
        gq2t = sb0("gq2t", [128, 1])
        gk2t = sb0("gk2t", [128, 1])
        neglam = sb0("neglam", [128, 1])
        rbb = sb0("rbb", [128, 64])
        lbt = sb0("lbt", [128, 4])
        omlt = sb0("omlt", [128, 4])
        gsubb = sb0("gsubb", [128, 128])
        ghgb = sb0("ghgb", [128, 128])
        mskf = sb0("mskf", [128, 128])
        mskb = sb0("mskb", [128, 128])
        t_const = Tok("const")
        cs = P.dsem("q_const")
        ld = lambda dst, src: P.op("sync", lambda e: e.dma_start(out=dst, in_=src), writes=[t_const], dma_sem=cs)
        ld(identf[:], ident[:, :])
        ld(antif[:], antiid[:, :])
        ld(onesb[:], onesblk[:, :])
        ld(gq2t[:], gq2[:, :])
        ld(gk2t[:], gk2[:, :])
        ld(rbb[:], rb_own.rearrange("b h -> (b h)").rearrange("(o n) -> o n", o=1).partition_broadcast(128))
        ld(gsubb[:], gsub[0:1, :].partition_broadcast(128))
        ld(ghgb[:], ghg[0:1, :].partition_broadcast(128))
        ld(mskf[:], mask_f[:, :])
        ld(mskb[:], mask_b[:, :])
        P.barrier()
        P.op("vector", lambda e: e.tensor_copy(out=identb[:], in_=identf[:]), writes=[t_const])

        if dbg == "H":
            P.op("vector", lambda e: e.memset(lbt[:], 0.5), writes=[t_const])
            P.op("vector", lambda e: e.memset(omlt[:], 0.5), writes=[t_const])
            t_hTd = [Tok("hTd%d" % i) for i in range(16)]
            build_hgrn(nc, P, ps, pst, w_own, hT_d, t_hTd, of_d, mix, lbt, omlt, ghgb, mskf, mskb, identb, dbg_n=DBG_N[0])
            P.barrier()
            P.emit()
            return nc
        with contextlib.ExitStack() as st:
            sb = lambda n, s, d=F32: st.enter_context(nc.sbuf_tensor(n, s, d))
            ct = sb("ct", [128, 16])
            sct = sb("sct", [128, 16])
            bat = sb("bat", [128, 96])
            gmt = sb("gmt", [128, 16])
            lamt = sb("lamt", [128, 256])
            lamp = sb("lamp", [128, 128])
            lams = sb("lams", [128, 2])
            lbl_t = sb("lbl_t", [128, 8])
            modS = sb("modS", [96, 128])
            t0 = Tok("p0")
            s0 = P.dsem("q_p0")
            ld0 = lambda dst, src: P.op("sync", lambda e: e.dma_start(out=dst, in_=src), writes=[t0], dma_sem=s0)
            ld0(ct[:], cT[:, :])
            ld0(bat[:], b_adaT[:, :])
            ld0(gmt[:], g_mixT[:, :])
            ld0(lamt[:], lamv[0:1, :].partition_broadcast(128))
            ld0(lbl_t[:], lbl[:, :])
            P.barrier()
            t_sct = Tok("sct")
            P.op("scalar", lambda e: e.activation(out=sct[:], in_=ct[:], func=AF.Silu), writes=[t_sct])
            t_lam = Tok("lam")
            lam_init = 0.8 - 0.6 * math.exp(0.0)
            P.op("vector", lambda e: e.tensor_tensor(out=lamp[:, 0:64], in0=lamt[:, 0:64], in1=lamt[:, 64:128], op=ALU.mult), writes=[t_lam])
            P.op("vector", lambda e: e.tensor_tensor(out=lamp[:, 64:128], in0=lamt[:, 128:192], in1=lamt[:, 192:256], op=ALU.mult), writes=[t_lam])
            P.op("vector", lambda e: e.tensor_reduce(out=lams[:], in_=lamp[:].rearrange("p (a b) -> p a b", a=2), axis=AX.X, op=ALU.add), writes=[t_lam])
            P.op("scalar", lambda e: e.activation(out=lams[:], in_=lams[:], func=AF.Exp), writes=[t_lam])
            P.op("vector", lambda e: e.tensor_tensor(out=neglam[:], in0=lams[:, 1:2], in1=lams[:, 0:1], op=ALU.subtract), writes=[t_lam])
            P.op("vector", lambda e: e.tensor_scalar(out=neglam[:], in0=neglam[:], scalar1=-lam_init, scalar2=None, op0=ALU.add), writes=[t_lam])
            t_lb = Tok("lb")
            lv = lbl_t[:].rearrange("p (d s h) -> p d s h", d=2, s=2)
            P.op("vector", lambda e: e.tensor_tensor(out=lbt[:].rearrange("p (d h) -> p d h", d=2), in0=lv[:, :, 0, :], in1=lv[:, :, 1, :], op=ALU.subtract), writes=[t_lb])
            P.op("scalar", lambda e: e.activation(out=lbt[:], in_=lbt[:], func=AF.Sigmoid), writes=[t_lb])
            P.op("vector", lambda e: e.tensor_scalar(out=omlt[:], in0=lbt[:], scalar1=-1.0, scalar2=1.0, op0=ALU.mult, op1=ALU.add), writes=[t_lb])

            wring = Ring(P, st, nc, "wa", 2, [128, 16, 512], F32)
            t_mod = pst[0]
            for cb in range(24):
                wt, wtok, wsem = wring.next()
                P.op("sync", lambda e, wt=wt, cb=cb: e.dma_start(out=wt[:], in_=w_ada[:, cb * 512:(cb + 1) * 512].rearrange("(k p) n -> p k n", p=128)),
                     writes=[wtok], dma_sem=wsem)
                for t in range(4):
                    col = cb * 4 + t
                    for k in range(16):
                        P.op("tensor", lambda e, wt=wt, t=t, k=k, col=col: e.matmul(
                            out=ps[0][:, col:col + 1], lhsT=wt[:, k, t * 128:(t + 1) * 128], rhs=sct[:, k:k + 1],
                            start=(k == 0), stop=(k == 15)), reads=[wtok, t_sct], writes=[t_mod])
            t_modT = Tok("modT")
            P.op("vector", lambda e: e.tensor_tensor(out=modT[:], in0=ps[0][:, 0:96], in1=bat[:], op=ALU.add), reads=[t_mod], writes=[t_modT])
            P.op("vector", lambda e: e.scalar_tensor_tensor(out=A1[:], in0=modT[:, 16:32], scalar=1.0, in1=gmt[:], op0=ALU.add, op1=ALU.mult),
                 reads=[t_modT], writes=[t_modT])
            P.op("tensor", lambda e: e.transpose(out=ps[1][0:96, 0:128], in_=modT[:, 0:96], identity=identf[:]), reads=[t_modT], writes=[pst[1]])
            t_modS = Tok("modS")
            P.op("vector", lambda e: e.tensor_copy(out=modS[:], in_=ps[1][0:96, 0:128]), reads=[pst[1]], writes=[t_modS])
            P.op("sync", lambda e: e.dma_start(out=modrow[:, :], in_=modS[:]), reads=[t_modS], dma_sem=P.dsem("q_modrow"))
            P.barrier()
        sh1 = modT[:, 0:16]

        t_hTd = [Tok("hTd%d" % i) for i in range(16)]
        with contextlib.ExitStack() as st:
            xring = Ring(P, st, nc, "xr", 3, [128, D], F32)
            xnring = Ring(P, st, nc, "xn", 2, [128, D], BF16)
            hbring = Ring(P, st, nc, "hb", 2, [128, 16, 512], BF16)
            junk = st.enter_context(nc.sbuf_tensor("junk", [128, D], BF16))
            ssr = st.enter_context(nc.sbuf_tensor("ssr", [128, 64], F32))
            t_junk = Tok("junk")
            t_ss = Tok("ss")
            for nb in range(16):
                hb, hbtok, hbsem = hbring.next()
                for tt in range(4):
                    i = nb * 4 + tt
                    xt, xtok, xsem = xring.next()
                    xn, xntok, _ = xnring.next()
                    P.op("sync", lambda e, xt=xt, i=i: e.dma_start(out=xt[:], in_=xb[i * 128:(i + 1) * 128, :]), writes=[xtok], dma_sem=xsem)
                    P.op("scalar", lambda e, xt=xt, i=i: e.activation(out=junk[:], in_=xt[:], func=AF.Square, accum_out=ssr[:, i:i + 1]),
                         reads=[xtok], writes=[t_junk, t_ss])
                    P.op("vector", lambda e, i=i: e.tensor_scalar(out=ssr[:, i:i + 1], in0=ssr[:, i:i + 1], scalar1=1.0 / D, scalar2=EPS, op0=ALU.mult, op1=ALU.add), writes=[t_ss])
                    P.op("scalar", lambda e, i=i: e.activation(out=ssr[:, i:i + 1], in_=ssr[:, i:i + 1], func=AF.Sqrt), writes=[t_ss])
                    P.op("vector", lambda e, i=i: e.reciprocal(out=ssr[:, i:i + 1], in_=ssr[:, i:i + 1]), writes=[t_ss])
                    P.op("vector", lambda e, xt=xt, xn=xn, i=i: e.tensor_scalar(out=xn[:], in0=xt[:], scalar1=ssr[:, i:i + 1], scalar2=None, op0=ALU.mult),
                         reads=[xtok, t_ss], writes=[xntok])
                    for half in range(2):
                        pb = 2 + half
                        for c8 in range(8):
                            c = half * 8 + c8
                            P.op("tensor", lambda e, xn=xn, c=c, c8=c8, pb=pb: e.transpose(
                                out=ps[pb][:].bitcast(BF16)[:, c8 * 128:(c8 + 1) * 128], in_=xn[:, c * 128:(c + 1) * 128], identity=identb[:]),
                                reads=[xntok], writes=[pst[pb]])
                        for c8 in range(8):
                            c = half * 8 + c8
                            src = lambda pb=pb, c8=c8: ps[pb][:].bitcast(BF16)[:, c8 * 128:(c8 + 1) * 128]
                            if c % 2 == 0:
                                P.op("vector", lambda e, hb=hb, c=c, tt=tt, src=src: e.tensor_scalar(
                                    out=hb[:, c, tt * 128:(tt + 1) * 128], in0=src(), scalar1=A1[:, c:c + 1], scalar2=modT[:, c:c + 1], op0=ALU.mult, op1=ALU.add),
                                    reads=[pst[pb]], writes=[hbtok])
                            else:
                                P.op("scalar", lambda e, hb=hb, c=c, tt=tt, src=src: e.activation(
                                    out=hb[:, c, tt * 128:(tt + 1) * 128], in_=src(), func=AF.Identity, scale=A1[:, c:c + 1], bias=modT[:, c:c + 1]),
                                    reads=[pst[pb]], writes=[hbtok])
                P.op("sync", lambda e, hb=hb, nb=nb: e.dma_start(out=hT_d[:, :, nb * 512:(nb + 1) * 512], in_=hb[:]), reads=[hbtok], writes=[t_hTd[nb]], dma_sem=hbsem)
            P.barrier()

        if dbg == "1a":
            P.emit()
            return nc
        with contextlib.ExitStack() as st:
            sb = lambda n, s, d=F32: st.enter_context(nc.sbuf_tensor(n, s, d))
            wA = sb("wA", [128, 16, 768], BF16)
            qaT = [sb("qaT%d" % h, [128, S], BF16) for h in range(2)]
            kaT = [sb("kaT%d" % h, [128, S], BF16) for h in range(2)]
            va = sb("va", [128, 64, 2, 130], BF16)
            EB = sb("EB", [128, 6, 2, 512], BF16)
            t_wA = Tok("wA")
            t_q = [Tok("q0"), Tok("q1")]
            t_k = [Tok("k0"), Tok("k1")]
            t_va = Tok("va")
            t_EB = Tok("EB")
            P.op("vector", lambda e: e.memset(va[:, :, :, 128:130], 1.0), writes=[t_va])
            with contextlib.ExitStack() as st2:
                wsr = Ring(P, st2, nc, "wsA", 2, [128, 16, 256], F32)
                for g in range(3):
                    wt, wtok, wsem = wsr.next()
                    P.op("sync", lambda e, wt=wt, g=g: e.dma_start(out=wt[:], in_=w_own[:, g * 256:(g + 1) * 256].rearrange("(k p) n -> p k n", p=128)),
                         writes=[wtok], dma_sem=wsem)
                    P.op("vector", lambda e, wt=wt, g=g: e.tensor_copy(out=wA[:, :, g * 256:(g + 1) * 256], in_=wt[:]), reads=[wtok], writes=[t_wA])
                ohs = st2.enter_context(nc.sbuf_tensor("ohs", [32, 1280], F32))
                rbs = st2.enter_context(nc.sbuf_tensor("rbs", [32, 2], F32))
                Gs = st2.enter_context(nc.sbuf_tensor("Gs", [2, 1280], F32))
                Hk = st2.enter_context(nc.sbuf_tensor("Hk", [128, 512], F32))
                t_oh = Tok("oh")
                s_oh = P.dsem("q_oh")
                P.op("sync", lambda e: e.dma_start(out=ohs[:], in_=ohg[:, :]), writes=[t_oh], dma_sem=s_oh)
                s_rbs = P.dsem("q_rbs")
                t_rbs = Tok("rbs")
                P.op("sync", lambda e: e.dma_start(out=rbs[:], in_=rb_own[:, :]), writes=[t_rbs], dma_sem=s_rbs)
                t_Gs = Tok("Gs")
                for q3 in range(3):
                    w_ = 512 if q3 < 2 else 256
                    P.op("tensor", lambda e, q3=q3, w_=w_: e.matmul(out=ps[0][0:2, 0:w_], lhsT=rbs[:, :], rhs=ohs[:, q3 * 512:q3 * 512 + w_], start=True, stop=True),
                         reads=[t_oh, t_rbs], writes=[pst[0]])
                    P.op("vector", lambda e, q3=q3, w_=w_: e.tensor_copy(out=Gs[:, q3 * 512:q3 * 512 + w_], in_=ps[0][0:2, 0:w_]), reads=[pst[0]], writes=[t_Gs])
                t_Gd = Tok("Gd")
                s_G = P.dsem("q_G")
                P.op("sync", lambda e: e.dma_start(out=G_d[:, :], in_=Gs[:]), reads=[t_Gs], writes=[t_Gd], dma_sem=s_G)
                t_Hk = Tok("Hk")
                s_Hk = P.dsem("q_Hk")
                for oi in range(6):
                    base = 512 - (oi - 1) * 128
                    for hh in range(2):
                        src = bass.AP(tensor=G_d.tensor, offset=G_d[hh:hh + 1, base:base + 1].offset, ap=[[1, 128], [1, 512]])
                        P.op("sync", lambda e, src=src: e.dma_start(out=Hk[:], in_=src), reads=[t_Gd], writes=[t_Hk], dma_sem=s_Hk)
                        P.op("tensor", lambda e: e.matmul(out=ps[0][:, :], lhsT=antif[:], rhs=Hk[:], start=True, stop=True), reads=[t_Hk], writes=[pst[0]])
                        P.op("scalar", lambda e, oi=oi, hh=hh: e.activation(out=EB[:, oi, hh, :], in_=ps[0][:, :], func=AF.Exp), reads=[pst[0]], writes=[t_EB])
                P.barrier()
            hbring = Ring(P, st, nc, "hbA", 2, [128, 16, 512], BF16)
            sq = sb("sq", [128, 512])
            rs = sb("rs", [128, 512])
            t_sq, t_rs = Tok("sq"), Tok("rs")
            for nb in range(16):
                hb, hbtok, hbsem = hbring.next()
                P.op("sync", lambda e, hb=hb, nb=nb: e.dma_start(out=hb[:], in_=hT_d[:, :, nb * 512:(nb + 1) * 512]), reads=[t_hTd[nb]], writes=[hbtok], dma_sem=hbsem)
                for ci in range(4):
                    isk, hh = ci // 2, ci % 2
                    pb = ci % 2
                    for k in range(16):
                        P.op("tensor", lambda e, hb=hb, ci=ci, k=k, pb=pb: e.matmul(out=ps[pb][:, :], lhsT=wA[:, k, ci * 128:(ci + 1) * 128], rhs=hb[:, k, :],
                                                                                  start=(k == 0), stop=(k == 15)), reads=[t_wA, hbtok], writes=[pst[pb]])
                    P.op("scalar", lambda e, pb=pb: e.activation(out=sq[:], in_=ps[pb][:, :], func=AF.Square), reads=[pst[pb]], writes=[t_sq])
                    P.op("tensor", lambda e: e.matmul(out=ps[2][:, :], lhsT=onesb[:], rhs=sq[:], start=True, stop=True), reads=[t_sq], writes=[pst[2]])
                    P.op("scalar", lambda e: e.activation(out=rs[:], in_=ps[2][:, :], func=AF.Sqrt, bias=EPS, scale=1.0), reads=[pst[2]], writes=[t_rs])
                    P.op("vector", lambda e: e.reciprocal(out=rs[:], in_=rs[:]), writes=[t_rs])
                    dst = (kaT if isk else qaT)[hh]
                    dtok = (t_k if isk else t_q)[hh]
                    gsc = gk2t if isk else gq2t
                    P.op("vector", lambda e, dst=dst, pb=pb, gsc=gsc, nb=nb: e.scalar_tensor_tensor(
                        out=dst[:, nb * 512:(nb + 1) * 512], in0=ps[pb][:, :], scalar=gsc[:, 0:1], in1=rs[:], op0=ALU.mult, op1=ALU.mult),
                        reads=[pst[pb], t_rs], writes=[dtok])
                for tt in range(4):
                    pb = 3 + (tt % 2)
                    for k in range(16):
                        P.op("tensor", lambda e, hb=hb, tt=tt, k=k, pb=pb: e.matmul(out=ps[pb][:, 0:256], lhsT=hb[:, k, tt * 128:(tt + 1) * 128], rhs=wA[:, k, 512:768],
                                                                                  start=(k == 0), stop=(k == 15)), reads=[t_wA, hbtok], writes=[pst[pb]])
                    P.op("scalar", lambda e, tt=tt, pb=pb, nb=nb: e.activation(out=va[:, nb * 4 + tt, :, 0:128], in_=ps[pb][:, 0:256].rearrange("p (h d) -> p h d", h=2), func=AF.Copy),
                         reads=[pst[pb]], writes=[t_va])
            ptring = Ring(P, st, nc, "pt", 3, [128, 512], BF16)
            osb = sb("osb", [128, 2, 130])
            o1 = sb("o1", [128, 128])
            o2 = sb("o2", [128, 128])
            rec = sb("rec", [128, 4])
            mst = Ring(P, st, nc, "mst", 2, [128, 4, 128], BF16)
            t_fin = Tok("fin")
            junk2 = sb("junk2", [128, 128])
            for hh in range(2):
                farb = [rbb[:, 15 * 2 + hh:15 * 2 + hh + 1], rbb[:, 31 * 2 + hh:31 * 2 + hh + 1]]
                for qb in range(16):
                    for kt in range(64):
                        oi = kt - 4 * qb + 1
                        near = 0 <= oi < 6
                        for m in range(2):
                            sbk = 2 + ((kt * 2 + m) % 2)
                            P.op("tensor", lambda e, hh=hh, qb=qb, kt=kt, m=m, sbk=sbk: e.matmul(
                                out=ps[sbk][:, :], lhsT=kaT[hh][m * 64:(m + 1) * 64, kt * 128:(kt + 1) * 128],
                                rhs=qaT[hh][m * 64:(m + 1) * 64, qb * 512:(qb + 1) * 512], start=True, stop=True),
                                reads=[t_k[hh], t_q[hh]], writes=[pst[sbk]])
                            pt, pttok, _ = ptring.next()
                            if near:
                                P.op("scalar", lambda e, pt=pt, sbk=sbk: e.activation(out=pt[:], in_=ps[sbk][:, :], func=AF.Exp, scale=0.125), reads=[pst[sbk]], writes=[pttok])
                                P.op("vector", lambda e, pt=pt, oi=oi, hh=hh: e.tensor_tensor(out=pt[:], in0=pt[:], in1=EB[:, oi, hh, :], op=ALU.mult), reads=[t_EB], writes=[pttok])
                            else:
                                fb = farb[0] if oi < 0 else farb[1]
                                P.op("scalar", lambda e, pt=pt, sbk=sbk, fb=fb: e.activation(out=pt[:], in_=ps[sbk][:, :], func=AF.Exp, scale=0.125, bias=fb), reads=[pst[sbk]], writes=[pttok])
                            for s4 in range(4):
                                ob = 4 + 2 * m + s4 // 2
                                oc = (s4 % 2) * 256
                                P.op("tensor", lambda e, pt=pt, s4=s4, ob=ob, oc=oc, kt=kt, hh=hh: e.matmul(
                                    out=ps[ob][:, oc:oc + 130], lhsT=pt[:, s4 * 128:(s4 + 1) * 128], rhs=va[:, kt, hh, :],
                                    start=(kt == 0 and s4 % 2 == 0), stop=(kt == 63), skip_group_check=True),
                                    reads=[pttok, t_va], writes=[pst[ob]])
                    ms, mstok, mssem = mst.next()
                    for s4 in range(4):
                        oc = (s4 % 2) * 256
                        b1 = 4 + s4 // 2
                        b2 = 6 + s4 // 2
                        P.op("vector", lambda e, b1=b1, oc=oc: e.reciprocal(out=rec[:, 0:1], in_=ps[b1][:, oc + 128:oc + 129]), reads=[pst[b1]], writes=[t_fin])
                        P.op("vector", lambda e, b2=b2, oc=oc: e.reciprocal(out=rec[:, 1:2], in_=ps[b2][:, oc + 128:oc + 129]), reads=[pst[b2]], writes=[t_fin])
                        P.op("vector", lambda e: e.tensor_tensor(out=rec[:, 1:2], in0=rec[:, 1:2], in1=neglam[:], op=ALU.mult), writes=[t_fin])
                        P.op("vector", lambda e, b1=b1, oc=oc: e.tensor_scalar(out=o1[:], in0=ps[b1][:, oc:oc + 128], scalar1=rec[:, 0:1], scalar2=None, op0=ALU.mult), reads=[pst[b1]], writes=[t_fin])
                        P.op("vector", lambda e, b2=b2, oc=oc: e.scalar_tensor_tensor(out=o1[:], in0=ps[b2][:, oc:oc + 128], scalar=rec[:, 1:2], in1=o1[:], op0=ALU.mult, op1=ALU.add),
                             reads=[pst[b2]], writes=[t_fin])
                        P.op("scalar", lambda e: e.activation(out=junk2[:], in_=o1[:], func=AF.Square, accum_out=rec[:, 2:3]), writes=[t_fin])
                        P.op("vector", lambda e: e.tensor_scalar(out=rec[:, 2:3], in0=rec[:, 2:3], scalar1=1.0 / 128, scalar2=EPS, op0=ALU.mult, op1=ALU.add), writes=[t_fin])
                        P.op("scalar", lambda e: e.activation(out=rec[:, 2:3], in_=rec[:, 2:3], func=AF.Sqrt), writes=[t_fin])
                        P.op("vector", lambda e: e.reciprocal(out=rec[:, 3:4], in_=rec[:, 2:3]), writes=[t_fin])
                        P.op("vector", lambda e: e.tensor_scalar(out=o1[:], in0=o1[:], scalar1=rec[:, 3:4], scalar2=1.0 - lam_init, op0=ALU.mult, op1=ALU.mult), writes=[t_fin])
                        P.op("vector", lambda e, ms=ms, s4=s4: e.tensor_tensor(out=ms[:, s4, :], in0=o1[:], in1=gsubb[:], op=ALU.mult), reads=[t_fin], writes=[mstok])
                    P.op("sync", lambda e, ms=ms, qb=qb, hh=hh: e.dma_start(
                        out=mix[qb * 512:(qb + 1) * 512, hh * 128:(hh + 1) * 128].rearrange("(s p) d -> p s d", p=128), in_=ms[:]),
                        reads=[mstok], dma_sem=mssem)
            P.barrier()

        if dbg == "A":
            P.emit()
            return nc
        build_hgrn(nc, P, ps, pst, w_own, hT_d, t_hTd, of_d, mix, lbt, omlt, ghgb, mskf, mskb, identb)
        P.barrier()
        P.emit()
    return nc


EPS_AP = [None]


DBG_N = [0]


def build_hgrn(nc, P, ps, pst, w_own, hT_d, t_hTd, of_d, mix, lbt, omlt, ghgb, mskf, mskb, identb, dbg_n=0):
    with contextlib.ExitStack() as st:
        sb = lambda n, s, d=F32: st.enter_context(nc.sbuf_tensor(n, s, d))
        wH = sb("wH", [128, 16, 1280], BF16)
        t_wH = Tok("wH")
        with contextlib.ExitStack() as st2:
            wsr = Ring(P, st2, nc, "wsH", 2, [128, 16, 256], F32)
            for g in range(5):
                wt, wtok, wsem = wsr.next()
                P.op("sync", lambda e, wt=wt, g=g: e.dma_start(out=wt[:], in_=w_own[:, (3 + g) * 256:(4 + g) * 256].rearrange("(k p) n -> p k n", p=128)),
                     writes=[wtok], dma_sem=wsem)
                for hh in range(2):
                    P.op("vector", lambda e, wt=wt, g=g, hh=hh: e.tensor_copy(out=wH[:, :, hh * 640 + g * 128:hh * 640 + (g + 1) * 128], in_=wt[:, :, hh * 128:(hh + 1) * 128]),
                         reads=[wtok], writes=[t_wH])
            P.barrier()
        hbring = Ring(P, st, nc, "hbH", 2, [128, 16, 512], BF16)
        rmask = sb("rmask", [128, 512])
        cmlo = sb("cmlo", [128, 512])
        cmhi = sb("cmhi", [128, 512])
        t_rm = Tok("rm")
        P.op("vector", lambda e: e.memset(rmask[:], 1.0), writes=[t_rm])
        P.op("vector", lambda e: e.memset(rmask[:].rearrange("p (c t) -> p c t", t=64)[:, :, 0:1], 0.0), writes=[t_rm])
        P.op("vector", lambda e: e.memset(cmlo[:], 0.0), writes=[t_rm])
        P.op("vector", lambda e: e.memset(cmhi[:], 0.0), writes=[t_rm])
        P.op("vector", lambda e: e.memset(cmlo[:].rearrange("p (c t) -> p c t", t=128)[:, :, 0:64], 1.0), writes=[t_rm])
        P.op("vector", lambda e: e.memset(cmhi[:].rearrange("p (c t) -> p c t", t=128)[:, :, 64:128], 1.0), writes=[t_rm])
        H2 = range(2)
        Sf = [sb("Sf%d" % h, [128, 128]) for h in H2]
        Sb_ = [sb("Sb%d" % h, [128, 128], BF16) for h in H2]
        tmpS = [sb("tmpS%d" % h, [128, 128]) for h in H2]
        qs = [sb("qs%d" % h, [128, 512]) for h in H2]
        ff = [sb("ff%d" % h, [128, 512]) for h in H2]
        lg = [sb("lg%d" % h, [128, 512]) for h in H2]
        bb = [sb("bb%d" % h, [128, 512]) for h in H2]
        eb = [sb("eb%d" % h, [128, 512]) for h in H2]
        enb = [sb("enb%d" % h, [128, 512]) for h in H2]
        qfb = [sb("qfb%d" % h, [128, 512], BF16) for h in H2]
        qlo = [sb("qlo%d" % h, [128, 512], BF16) for h in H2]
        qhi = [sb("qhi%d" % h, [128, 512], BF16) for h in H2]
        kt_ = [sb("kt%d" % h, [128, 512], BF16) for h in H2]
        vt = [sb("vt%d" % h, [128, 4, 128], BF16) for h in H2]
        vlo = [sb("vlo%d" % h, [128, 4, 128], BF16) for h in H2]
        vhi = [sb("vhi%d" % h, [128, 4, 128], BF16) for h in H2]
        sg = [sb("sg%d" % h, [128, 4, 128]) for h in H2]
        kh2 = [sb("kh2%d" % h, [128, 128], BF16) for h in H2]
        scm = [sb("scm%d" % h, [128, 128], BF16) for h in H2]
        ob = [sb("ob%d" % h, [128, 4, 128]) for h in H2]
        ofl = [sb("ofl%d" % h, [128, 4, 128]) for h in H2]
        obb = [sb("obb%d" % h, [128, 4, 128], BF16) for h in H2]
        stat = [sb("stat%d" % h, [128, 4]) for h in H2]
        junk = [sb("junkh%d" % h, [128, 4, 128]) for h in H2]
        t_S = [Tok("S") for h in H2]
        tE = [Tok("tE") for h in H2]
        t_v = [Tok("v") for h in H2]
        t_sg = [Tok("sg") for h in H2]
        t_kh = [Tok("kh") for h in H2]
        t_scm = [Tok("scm") for h in H2]
        t_ob = [Tok("ob") for h in H2]
        t_ofl = [Tok("ofl") for h in H2]
        s_ob = [P.dsem("q_ob%d" % h) for h in H2]
        s_ofl = [P.dsem("q_ofl%d" % h) for h in H2]
        t_ofd = [[Tok("ofd") for _ in range(16)] for _ in H2]
        for hh in H2:
            P.op("vector", lambda e, hh=hh: e.memset(vlo[hh][:], 0.0), writes=[t_v[hh]])
            P.op("vector", lambda e, hh=hh: e.memset(vhi[hh][:], 0.0), writes=[t_v[hh]])
        for d in range(2):
            if dbg_n and d == 1:
                break
            for hh in H2:
                P.op("vector", lambda e, hh=hh: e.memset(Sf[hh][:], 0.0), writes=[t_S[hh]])
                P.op("vector", lambda e, hh=hh: e.memset(Sb_[hh][:], 0.0), writes=[t_S[hh]])
            blocks = range(16) if d == 0 else range(15, -1, -1)
            if dbg_n:
                blocks = range(1)
            mk = mskf if d == 0 else mskb
            for nb in blocks:
                hb, hbtok, hbsem = hbring.next()
                P.op("sync", lambda e, hb=hb, nb=nb: e.dma_start(out=hb[:], in_=hT_d[:, :, nb * 512:(nb + 1) * 512]), reads=[t_hTd[nb]], writes=[hbtok], dma_sem=hbsem)
                for hh in H2:
                    base = hh * 640
                    zc = base + (2 + d) * 128
                    li = d * 2 + hh
                    for k in range(16):
                        P.op("tensor", lambda e, hb=hb, k=k, base=base: e.matmul(out=ps[0][:, :], lhsT=wH[:, k, base:base + 128], rhs=hb[:, k, :], start=(k == 0), stop=(k == 15)),
                             reads=[t_wH, hbtok], writes=[pst[0]])
                    P.op("scalar", lambda e, hh=hh: e.activation(out=qs[hh][:], in_=ps[0][:, :], func=AF.Silu), reads=[pst[0]], writes=[tE[hh]])
                    for k in range(16):
                        P.op("tensor", lambda e, hb=hb, k=k, zc=zc: e.matmul(out=ps[1][:, :], lhsT=wH[:, k, zc:zc + 128], rhs=hb[:, k, :], start=(k == 0), stop=(k == 15)),
                             reads=[t_wH, hbtok], writes=[pst[1]])
                    P.op("scalar", lambda e, hh=hh: e.activation(out=ff[hh][:], in_=ps[1][:, :], func=AF.Sigmoid), reads=[pst[1]], writes=[tE[hh]])
                    P.op("vector", lambda e, hh=hh, li=li: e.tensor_scalar(out=ff[hh][:], in0=ff[hh][:], scalar1=omlt[:, li:li + 1], scalar2=lbt[:, li:li + 1], op0=ALU.mult, op1=ALU.add), writes=[tE[hh]])
                    P.op("scalar", lambda e, hh=hh: e.activation(out=lg[hh][:], in_=ff[hh][:], func=AF.Ln), writes=[tE[hh]])
                    P.op("vector", lambda e, hh=hh: e.tensor_scalar(out=ff[hh][:], in0=ff[hh][:], scalar1=-1.0, scalar2=1.0, op0=ALU.mult, op1=ALU.add), writes=[tE[hh]])
                    P.op("vector", lambda e, hh=hh: e.tensor_tensor_scan(out=bb[hh][:], data0=rmask[:], data1=lg[hh][:], initial=0.0, op0=ALU.mult, op1=ALU.add), reads=[t_rm], writes=[tE[hh]])
                    if d == 1:
                        b3 = bb[hh][:].rearrange("p (c t) -> p c t", t=64)
                        l3 = lg[hh][:].rearrange("p (c t) -> p c t", t=64)
                        P.op("vector", lambda e, hh=hh: e.tensor_tensor(out=lg[hh][:], in0=lg[hh][:], in1=bb[hh][:], op=ALU.subtract), writes=[tE[hh]])
                        P.op("vector", lambda e, hh=hh: e.tensor_copy(out=stat[hh][:, 0:4], in_=bb[hh][:].rearrange("p (c t) -> p c t", t=128)[:, :, 63]), writes=[tE[hh]])
                        P.op("vector", lambda e, hh=hh: e.tensor_copy(out=junk[hh][:, 0, 0:4], in_=bb[hh][:].rearrange("p (c t) -> p c t", t=128)[:, :, 127]), writes=[tE[hh]])
                        for c in range(8):
                            tot = stat[hh][:, c // 2:c // 2 + 1] if c % 2 == 0 else junk[hh][:, 0, c // 2:c // 2 + 1]
                            P.op("vector", lambda e, hh=hh, c=c, tot=tot: e.tensor_scalar(out=bb[hh][:, c * 64:(c + 1) * 64], in0=lg[hh][:, c * 64:(c + 1) * 64], scalar1=tot, scalar2=None, op0=ALU.add), writes=[tE[hh]])
                    P.op("scalar", lambda e, hh=hh: e.activation(out=eb[hh][:], in_=bb[hh][:], func=AF.Exp), writes=[tE[hh]])
                    P.op("scalar", lambda e, hh=hh: e.activation(out=enb[hh][:], in_=bb[hh][:], func=AF.Exp, scale=-1.0), writes=[tE[hh]])
                    P.op("vector", lambda e, hh=hh: e.tensor_tensor(out=qfb[hh][:], in0=qs[hh][:], in1=eb[hh][:], op=ALU.mult), writes=[tE[hh]])
                    P.op("vector", lambda e, hh=hh: e.tensor_tensor(out=qlo[hh][:], in0=qfb[hh][:], in1=cmlo[:], op=ALU.mult), writes=[tE[hh]])
                    P.op("vector", lambda e, hh=hh: e.tensor_tensor(out=qhi[hh][:], in0=qfb[hh][:], in1=cmhi[:], op=ALU.mult), writes=[tE[hh]])
                    P.op("vector", lambda e, hh=hh: e.tensor_tensor(out=kt_[hh][:], in0=ff[hh][:], in1=enb[hh][:], op=ALU.mult), writes=[tE[hh]])
                    for which in range(2 if d == 1 else 1):
                        cc = base + (1 if which == 0 else 4) * 128
                        pb = which
                        for tt in range(4):
                            for k in range(16):
                                P.op("tensor", lambda e, hb=hb, k=k, tt=tt, cc=cc, pb=pb: e.matmul(
                                    out=ps[pb][:, tt * 128:(tt + 1) * 128], lhsT=hb[:, k, tt * 128:(tt + 1) * 128], rhs=wH[:, k, cc:cc + 128],
                                    start=(k == 0), stop=(k == 15)), reads=[t_wH, hbtok], writes=[pst[pb]])
                        if which == 0:
                            src = lambda lo, hi: ps[0][lo:hi, :].rearrange("p (c d) -> p c d", c=4)
                            P.op("vector", lambda e, hh=hh, src=src: e.tensor_copy(out=vt[hh][:], in_=src(0, 128)), reads=[pst[0]], writes=[t_v[hh]])
                            P.op("vector", lambda e, hh=hh, src=src: e.tensor_copy(out=vlo[hh][0:64], in_=src(0, 64)), reads=[pst[0]], writes=[t_v[hh]])
                            P.op("scalar", lambda e, hh=hh, src=src: e.activation(out=vhi[hh][64:128], in_=src(64, 128), func=AF.Copy), reads=[pst[0]], writes=[t_v[hh]])
                        else:
                            P.op("scalar", lambda e, hh=hh: e.activation(out=sg[hh][:], in_=ps[1][:, :].rearrange("p (c d) -> p c d", c=4), func=AF.Silu),
                                 reads=[pst[1]], writes=[t_sg[hh]])
                    if d == 1:
                        P.op("sync", lambda e, hh=hh, nb=nb: e.dma_start(out=ofl[hh][:], in_=of_d[hh, nb * 512:(nb + 1) * 512, :].rearrange("(c p) d -> p c d", p=128)),
                             reads=[t_ofd[hh][nb]], writes=[t_ofl[hh]], dma_sem=s_ofl[hh])
                    pK, pO, pU = 2 + 3 * hh, 3 + 3 * hh, 4 + 3 * hh
                    tiles = range(4) if d == 0 else range(3, -1, -1)
                    for tt in tiles:
                        tsl = slice(tt * 128, (tt + 1) * 128)
                        P.op("tensor", lambda e, hh=hh, tsl=tsl, pK=pK: e.transpose(out=ps[pK][:].bitcast(BF16)[:, 0:128], in_=kt_[hh][:, tsl], identity=identb[:]),
                             reads=[tE[hh]], writes=[pst[pK]])
                        P.op("tensor", lambda e, hh=hh, tsl=tsl, pK=pK: e.matmul(out=ps[pK][:, 256:384], lhsT=kt_[hh][:, tsl], rhs=qfb[hh][:, tsl], start=True, stop=True, skip_group_check=True),
                             reads=[tE[hh]], writes=[pst[pK]])
                        P.op("scalar", lambda e, hh=hh, pK=pK: e.activation(out=kh2[hh][:], in_=ps[pK][:].bitcast(BF16)[:, 0:128], func=AF.Copy), reads=[pst[pK]], writes=[t_kh[hh]])
                        P.op("vector", lambda e, hh=hh, pK=pK, mk=mk: e.tensor_tensor(out=scm[hh][:], in0=ps[pK][:, 256:384], in1=mk[:], op=ALU.mult), reads=[pst[pK]], writes=[t_scm[hh]])
                        order = [(qlo, vlo, 2 * tt), (qhi, vhi, 2 * tt + 1)]
                        if d == 1:
                            order = order[::-1]
                        for oi, (qX, vX, c) in enumerate(order):
                            lastcol = c * 64 + (63 if d == 0 else 0)
                            P.op("tensor", lambda e, hh=hh, tsl=tsl, pO=pO, qX=qX, oi=oi: e.matmul(out=ps[pO][:, 0:128], lhsT=qX[hh][:, tsl], rhs=Sb_[hh][:], start=(oi == 0), stop=False, skip_group_check=True),
                                 reads=[tE[hh], t_S[hh]], writes=[pst[pO]])
                            if oi == 1:
                                P.op("tensor", lambda e, hh=hh, tt=tt, pO=pO: e.matmul(out=ps[pO][:, 0:128], lhsT=scm[hh][:], rhs=vt[hh][:, tt, :], start=False, stop=True, skip_group_check=True),
                                     reads=[t_scm[hh], t_v[hh]], writes=[pst[pO]])
                            P.op("tensor", lambda e, hh=hh, tt=tt, pU=pU, vX=vX: e.matmul(out=ps[pU][:, 0:128], lhsT=kh2[hh][:], rhs=vX[hh][:, tt, :], start=True, stop=True),
                                 reads=[t_kh[hh], t_v[hh]], writes=[pst[pU]])
                            P.op("vector", lambda e, hh=hh, pU=pU: e.tensor_tensor(out=tmpS[hh][:], in0=ps[pU][:, 0:128], in1=Sf[hh][:], op=ALU.add), reads=[pst[pU], t_S[hh]], writes=[t_S[hh]])
                            P.op("vector", lambda e, hh=hh, lastcol=lastcol: e.tensor_scalar(out=Sf[hh][:], in0=tmpS[hh][:], scalar1=eb[hh][:, lastcol:lastcol + 1], scalar2=None, op0=ALU.mult),
                                 reads=[tE[hh]], writes=[t_S[hh]])
                            P.op("vector", lambda e, hh=hh: e.tensor_copy(out=Sb_[hh][:], in_=Sf[hh][:]), writes=[t_S[hh]])
                        P.op("scalar", lambda e, hh=hh, tt=tt, pO=pO: e.activation(out=ob[hh][:, tt, :], in_=ps[pO][:, 0:128], func=AF.Copy), reads=[pst[pO]], writes=[t_ob[hh]])
                    if d == 0:
                        P.op("sync", lambda e, hh=hh, nb=nb: e.dma_start(out=of_d[hh, nb * 512:(nb + 1) * 512, :].rearrange("(c p) d -> p c d", p=128), in_=ob[hh][:]),
                             reads=[t_ob[hh]], writes=[t_ofd[hh][nb]], dma_sem=s_ob[hh])
                    else:
                        P.op("vector", lambda e, hh=hh: e.tensor_tensor(out=ob[hh][:], in0=ob[hh][:], in1=ofl[hh][:], op=ALU.add), reads=[t_ofl[hh]], writes=[t_ob[hh]])
                        P.op("vector", lambda e, hh=hh: e.tensor_tensor(out=junk[hh][:], in0=ob[hh][:], in1=ob[hh][:], op=ALU.mult), reads=[t_ob[hh], tE[hh]], writes=[t_sg[hh]])
                        P.op("vector", lambda e, hh=hh: e.tensor_reduce(out=stat[hh][:], in_=junk[hh][:], axis=AX.X, op=ALU.add), reads=[tE[hh]], writes=[t_sg[hh]])
                        P.op("vector", lambda e, hh=hh: e.tensor_scalar(out=stat[hh][:], in0=stat[hh][:], scalar1=1.0 / 128, scalar2=EPS, op0=ALU.mult, op1=ALU.add), writes=[t_sg[hh]])
                        P.op("scalar", lambda e, hh=hh: e.activation(out=stat[hh][:], in_=stat[hh][:], func=AF.Sqrt), writes=[t_sg[hh]])
                        P.op("vector", lambda e, hh=hh: e.reciprocal(out=stat[hh][:], in_=stat[hh][:]), writes=[t_sg[hh]])
                        for tt in range(4):
                            P.op("vector", lambda e, hh=hh, tt=tt: e.scalar_tensor_tensor(out=ob[hh][:, tt, :], in0=ob[hh][:, tt, :], scalar=stat[hh][:, tt:tt + 1], in1=sg[hh][:, tt, :], op0=ALU.mult, op1=ALU.mult),
                                 reads=[t_sg[hh]], writes=[t_ob[hh]])
                            P.op("vector", lambda e, hh=hh, tt=tt: e.tensor_tensor(out=obb[hh][:, tt, :], in0=ob[hh][:, tt, :], in1=ghgb[:], op=ALU.mult), writes=[t_ob[hh]])
                        P.op("sync", lambda e, hh=hh, nb=nb: e.dma_start(out=mix[nb * 512:(nb + 1) * 512, 256 + hh * 128:256 + (hh + 1) * 128].rearrange("(c p) d -> p c d", p=128), in_=obb[hh][:]),
                             reads=[t_ob[hh]], writes=[tE[hh]], dma_sem=s_ob[hh])
        P.barrier()


def l1_inputs(inputs, core):
    b, j = core // 4, core % 4
    f = lambda a: np.ascontiguousarray(a, dtype=np.float32)
    w_in = inputs["w_in"][0]
    offs = np.cumsum([0, 1024, 1024, 1024, 1024, 1024, 1024, 1024])
    cols = []
    for g in range(8):
        cols.append(w_in[:, offs[g] + j * 256: offs[g] + (j + 1) * 256])
    w_own = np.concatenate(cols, axis=1)
    lb = inputs["hgrn_lb_logits"]
    lbl = np.zeros((128, 2, 2, 2), np.float32)
    for d in range(2):
        for s_ in range(2):
            for hh in range(2):
                lbl[:, d, s_, hh] = lb[d, s_, (2 * j + hh) * 128:(2 * j + hh + 1) * 128]
    m = {
        "xb": f(inputs["x"][b]),
        "cT": f(inputs["c"][b].reshape(16, 128).T),
        "w_ada": f(inputs["w_ada"][0]),
        "b_adaT": f(inputs["b_ada"][0].reshape(96, 128).T),
        "g_mixT": f(inputs["g_mix"][0].reshape(16, 128).T),
        "w_own": f(w_own),
        "gq2": f(np.tile(inputs["g_q"][0], 2).reshape(128, 1)),
        "gk2": f(np.tile(inputs["g_k"][0], 2).reshape(128, 1)),
        "lamv": f(np.concatenate([inputs["lam_q1"][0], inputs["lam_k1"][0], inputs["lam_q2"][0], inputs["lam_k2"][0]]).reshape(1, 256)),
        "rb_own": f(inputs["rel_bias"][:, 2 * j:2 * j + 2]),
        "lbl": f(lbl.reshape(128, 8)),
        "gsub": f(inputs["g_sub"][0].reshape(1, 128)),
        "ghg": f(inputs["g_hgrn"][0].reshape(1, 128)),
    }
    m.update(_consts())
    return m


BIG = 1.0e4
CAPC = 192
ESLOTS = 8 * CAPC


def build_l2a():
    nc = bass.Bass("TRN2", target_bir_lowering=False)
    din = lambda n, s, d=F32: nc.dram_tensor(n, s, d, kind="ExternalInput").ap()
    xo = din("xo", [2048, D])
    cat = din("cat", [2048, D], BF16)
    w_out = din("w_out", [D, D])
    rows = din("rows", [6, D])
    w_router = din("w_router", [D, NE])
    rbias = din("rbias", [1, NE])
    wsg = din("wsg", [D, FF])
    wsu = din("wsu", [D, FF])
    wsd = din("wsd", [FF, D])
    ident = din("ident", [128, 128])
    tris = din("tris", [128, 128])
    iotae = din("iotae", [1, NE])
    srcoff = din("srcoff", [128, 1])
    destk_o = nc.dram_tensor("destk", [2048, 8], F32, kind="ExternalOutput").ap()
    gk_o = nc.dram_tensor("gk", [2048, 8], F32, kind="ExternalOutput").ap()
    x1s_o = nc.dram_tensor("x1s", [2048, D], F32, kind="ExternalOutput").ap()
    h2_o = nc.dram_tensor("h2", [2048, D], BF16, kind="ExternalOutput").ap()
    gates_o = nc.dram_tensor("gates", [2048, NE], F32, kind="ExternalOutput").ap()
    with contextlib.ExitStack() as st:
        P = Prog(nc, st)
        ps = [st.enter_context(nc.psum_tensor("ps%d" % i, [128, 512], F32)) for i in range(8)]
        pst = [Tok("ps%d" % i) for i in range(8)]
        sb = lambda n, s, d=F32: st.enter_context(nc.sbuf_tensor(n, s, d))
        identf = sb("identf", [128, 128])
        identb = sb("identb", [128, 128], BF16)
        gt1b = sb("gt1b", [128, D], BF16)
        A2b = sb("A2b", [128, D])
        sh2b = sb("sh2b", [128, D])
        gt2b = sb("gt2b", [128, D], BF16)
        rbb = sb("rbb", [128, NE])
        iob = sb("iob", [128, NE])
        trib = sb("trib", [128, 128], BF16)
        onesb_ = sb("onesb_", [128, 128], BF16)
        selsum = sb("selsum", [128, NE], BF16)
        selb = sb("selb", [128, NE], BF16)
        srct = sb("srct", [128, 1])
        idx8 = sb("idx8", [128, 8], U32)
        idxf = sb("idxf", [128, 8])
        posd = sb("posd", [128, NE])
        jk = sb("jk", [128, NE])
        dk_ = sb("dk_", [128, 16, 8])
        gk_ = sb("gk_", [128, 16, 8])
        pk_ = sb("pk_", [128, 8])
        wo = sb("wo", [128, 16, D], BF16)
        wr = sb("wr", [128, 16, NE])
        wgb = sb("wgb", [128, 16, FF], BF16)
        wub = sb("wub", [128, 16, FF], BF16)
        wdb = sb("wdb", [128, 4, D], BF16)
        t_c = Tok("c")
        cs = P.dsem("q_c")
        ld = lambda dst, src: P.op("sync", lambda e: e.dma_start(out=dst, in_=src), writes=[t_c], dma_sem=cs)
        ld(identf[:], ident[:, :])
        ld(A2b[:], rows[1:2, :].partition_broadcast(128))
        ld(sh2b[:], rows[2:3, :].partition_broadcast(128))
        ld(rbb[:], rbias[0:1, :].partition_broadcast(128))
        ld(iob[:], iotae[0:1, :].partition_broadcast(128))
        ld(srct[:], srcoff[:, :])
        ld(wr[:], w_router.rearrange("(k p) n -> p k n", p=128))
        t_wo = Tok("wo")
        with contextlib.ExitStack() as st2:
            st3 = contextlib.ExitStack()
            gfb = st3.enter_context(nc.sbuf_tensor("gfb", [128, D], F32))
            gtmp = st3.enter_context(nc.sbuf_tensor("gtmp", [128, 2, D], F32))
            tristg = st3.enter_context(nc.sbuf_tensor("tristg", [128, 128], F32))
            ld(tristg[:], tris[:, :])
            ld(gfb[:], rows[4:5, :].partition_broadcast(128))
            ld(gtmp[:, 0, :], rows[0:1, :].partition_broadcast(128))
            ld(gtmp[:, 1, :], rows[3:4, :].partition_broadcast(128))
            P.barrier()
            P.op("vector", lambda e: e.tensor_copy(out=trib[:], in_=tristg[:]), writes=[t_c])
            P.op("vector", lambda e: e.memset(onesb_[:], 1.0), writes=[t_c])
            P.op("vector", lambda e: e.memset(selsum[:], 0.0), writes=[t_c])
            P.barrier()
            ld(gtmp[:, 0, :], rows[0:1, :].partition_broadcast(128))
            P.barrier()
            P.op("vector", lambda e: e.tensor_copy(out=gt1b[:], in_=gtmp[:, 0, :]), writes=[t_c])
            P.op("vector", lambda e: e.tensor_copy(out=gt2b[:], in_=gtmp[:, 1, :]), writes=[t_c])
            P.op("vector", lambda e: e.tensor_copy(out=identb[:], in_=identf[:]), writes=[t_c])
            P.op("vector", lambda e: e.scalar_tensor_tensor(out=A2b[:], in0=A2b[:], scalar=1.0, in1=gfb[:], op0=ALU.add, op1=ALU.mult), writes=[t_c])
            P.barrier()
            st3.close()
            wsr = Ring(P, st2, nc, "wso", 2, [128, 16, 256], F32)
            for g in range(8):
                wt, wtok, wsem = wsr.next()
                P.op("sync", lambda e, wt=wt, g=g: e.dma_start(out=wt[:], in_=w_out[:, g * 256:(g + 1) * 256].rearrange("(k p) n -> p k n", p=128)), writes=[wtok], dma_sem=wsem)
                P.op("vector", lambda e, wt=wt, g=g: e.tensor_copy(out=wo[:, :, g * 256:(g + 1) * 256], in_=wt[:]), reads=[wtok], writes=[t_wo])
            for wi, (src, dst) in enumerate([(wsg, wgb), (wsu, wub)]):
                for g in range(2):
                    wt, wtok, wsem = wsr.next()
                    P.op("sync", lambda e, wt=wt, g=g, src=src: e.dma_start(out=wt[:], in_=src[:, g * 256:(g + 1) * 256].rearrange("(k p) n -> p k n", p=128)), writes=[wtok], dma_sem=wsem)
                    P.op("vector", lambda e, wt=wt, g=g, dst=dst: e.tensor_copy(out=dst[:, :, g * 256:(g + 1) * 256], in_=wt[:]), reads=[wtok], writes=[t_wo])
            for fc in range(4):
                wt, wtok, wsem = wsr.next()
                P.op("sync", lambda e, wt=wt, fc=fc: e.dma_start(out=wt[:].rearrange("p k n -> p (k n)")[:, 0:D], in_=wsd[fc * 128:(fc + 1) * 128, :]), writes=[wtok], dma_sem=wsem)
                P.op("vector", lambda e, wt=wt, fc=fc: e.tensor_copy(out=wdb[:, fc, :], in_=wt[:].rearrange("p k n -> p (k n)")[:, 0:D]), reads=[wtok], writes=[t_wo])
            P.barrier()
        cbr = Ring(P, st, nc, "cb", 1, [128, D], BF16)
        ctr = Ring(P, st, nc, "ct", 1, [128, 16, 128], BF16)
        x1r = Ring(P, st, nc, "x1", 1, [128, D], F32)
        h2r = Ring(P, st, nc, "h2f", 1, [128, D], F32)
        h2Tr = Ring(P, st, nc, "h2T", 1, [128, 16, 128], F32)
        gtr = Ring(P, st, nc, "gt", 2, [128, NE], F32)
        hTr = Ring(P, st, nc, "hTs", 1, [128, 4, 128], BF16)
        st_ = sb("st_", [128, 8])
        scs = sb("scs", [128, NE])
        bis = sb("bis", [128, NE])
        mskd = sb("mskd", [128, NE])
        m8 = sb("m8", [128, 8, 8])
        gs = sb("gs", [128, 8])
        gm8 = sb("gm8", [128, 8])
        pen = sb("pen", [128, 8])
        t8 = sb("t8", [128, 8])
        sgl = sb("sgl", [128, 512])
        t_r = Tok("route")
        t_junk = Tok("junk")
        t_sgl = Tok("sgl")
        for i in range(16):
            rsl = slice(i * 128, (i + 1) * 128)
            cb_, cbtok, cbsem = cbr.next()
            h2b, h2btok, h2bsem = cb_, cbtok, cbsem
            junk = cb_
            cT, cTtok, _ = ctr.next()
            h2Tb, h2Tbtok = cT, cTtok
            x1, x1tok, x1sem = x1r.next()
            P.op("sync", lambda e, x1=x1, rsl=rsl: e.dma_start(out=x1[:], in_=xo[rsl, :]), writes=[x1tok], dma_sem=x1sem)
            h2, h2tok, _ = h2r.next()
            h2T, h2Ttok, _ = h2Tr.next()
            gt, gttok, gtsem = gtr.next()
            hT, hTtok, _ = hTr.next()
            P.op("sync", lambda e, cb_=cb_, rsl=rsl: e.dma_start(out=cb_[:], in_=cat[rsl, :]), writes=[cbtok], dma_sem=cbsem)
            for half in range(2):
                pb = half
                for c8 in range(8):
                    c = half * 8 + c8
                    P.op("tensor", lambda e, cb_=cb_, c=c, c8=c8, pb=pb: e.transpose(out=ps[pb][:].bitcast(BF16)[:, c8 * 128:(c8 + 1) * 128], in_=cb_[:, c * 128:(c + 1) * 128], identity=identb[:]),
                         reads=[cbtok], writes=[pst[pb]])
                P.op("scalar", lambda e, cT=cT, half=half, pb=pb: e.activation(out=cT[:, half * 8:(half + 1) * 8, :], in_=ps[pb][:].bitcast(BF16).rearrange("p (c t) -> p c t", c=8), func=AF.Copy),
                     reads=[pst[pb]], writes=[cTtok])
            for cbk in range(4):
                pb = 2 + cbk
                csl = slice(cbk * 512, (cbk + 1) * 512)
                for k in range(16):
                    P.op("tensor", lambda e, cT=cT, k=k, csl=csl, pb=pb: e.matmul(out=ps[pb][:, :], lhsT=cT[:, k, :], rhs=wo[:, k, csl], start=(k == 0), stop=(k == 15)),
                         reads=[cTtok, t_wo], writes=[pst[pb]])
                P.op("vector", lambda e, h2=h2, pb=pb, csl=csl: e.tensor_tensor(out=h2[:, csl], in0=ps[pb][:, :], in1=gt1b[:, csl], op=ALU.mult), reads=[pst[pb]], writes=[h2tok])
                P.op("gpsimd", lambda e, x1=x1, h2=h2, csl=csl: e.tensor_tensor(out=x1[:, csl], in0=x1[:, csl], in1=h2[:, csl], op=ALU.add), reads=[h2tok], writes=[x1tok])
            P.op("scalar", lambda e, x1=x1: e.activation(out=junk[:], in_=x1[:], func=AF.Square, accum_out=st_[:, 0:1]), reads=[x1tok], writes=[cbtok, t_r])
            P.op("vector", lambda e: e.tensor_scalar(out=st_[:, 0:1], in0=st_[:, 0:1], scalar1=1.0 / D, scalar2=EPS, op0=ALU.mult, op1=ALU.add), writes=[t_r])
            P.op("scalar", lambda e: e.activation(out=st_[:, 0:1], in_=st_[:, 0:1], func=AF.Sqrt), writes=[t_r])
            P.op("vector", lambda e: e.reciprocal(out=st_[:, 1:2], in_=st_[:, 0:1]), writes=[t_r])
            P.op("vector", lambda e, x1=x1, h2=h2: e.scalar_tensor_tensor(out=h2[:], in0=x1[:], scalar=st_[:, 1:2], in1=A2b[:], op0=ALU.mult, op1=ALU.mult), reads=[x1tok, t_r], writes=[h2tok])
            P.op("gpsimd", lambda e, h2=h2: e.tensor_tensor(out=h2[:], in0=h2[:], in1=sh2b[:], op=ALU.add), writes=[h2tok])
            P.op("scalar", lambda e, h2=h2, h2b=h2b: e.activation(out=h2b[:], in_=h2[:], func=AF.Copy), reads=[h2tok], writes=[h2btok])
            P.op("sync", lambda e, h2b=h2b, rsl=rsl: e.dma_start(out=h2_o[rsl, :], in_=h2b[:]), reads=[h2btok], dma_sem=h2bsem)
            for q4 in range(4):
                pb = 2 + q4
                for c4 in range(4):
                    c = q4 * 4 + c4
                    P.op("tensor", lambda e, h2=h2, c=c, c4=c4, pb=pb: e.transpose(out=ps[pb][:, c4 * 128:(c4 + 1) * 128], in_=h2[:, c * 128:(c + 1) * 128], identity=identf[:]),
                         reads=[h2tok], writes=[pst[pb]])
                eng = "vector" if q4 % 2 == 0 else "scalar"
                if eng == "vector":
                    P.op("vector", lambda e, h2T=h2T, q4=q4, pb=pb: e.tensor_copy(out=h2T[:, q4 * 4:(q4 + 1) * 4, :], in_=ps[pb][:, :].rearrange("p (c t) -> p c t", c=4)), reads=[pst[pb]], writes=[h2Ttok])
                else:
                    P.op("scalar", lambda e, h2T=h2T, q4=q4, pb=pb: e.activation(out=h2T[:, q4 * 4:(q4 + 1) * 4, :], in_=ps[pb][:, :].rearrange("p (c t) -> p c t", c=4), func=AF.Copy), reads=[pst[pb]], writes=[h2Ttok])
            P.op("gpsimd", lambda e, h2T=h2T, h2Tb=h2Tb: e.tensor_copy(out=h2Tb[:], in_=h2T[:]), reads=[h2Ttok], writes=[h2Tbtok])
            for k in range(16):
                P.op("tensor", lambda e, h2T=h2T, k=k: e.matmul(out=ps[6][:, 0:NE], lhsT=h2T[:, k, :], rhs=wr[:, k, :], start=(k == 0), stop=(k == 15)), reads=[h2Ttok], writes=[pst[6]])
            P.op("scalar", lambda e: e.activation(out=scs[:], in_=ps[6][:, 0:NE], func=AF.Sigmoid), reads=[pst[6]], writes=[t_r])
            P.op("vector", lambda e: e.tensor_tensor(out=bis[:], in0=scs[:], in1=rbb[:], op=ALU.add), writes=[t_r])
            for g in range(8):
                P.op("vector", lambda e, g=g: e.max(out=m8[:, g, :], in_=bis[:, g * 32:(g + 1) * 32]), writes=[t_r])
            P.op("vector", lambda e: e.tensor_tensor(out=gs[:], in0=m8[:, :, 0], in1=m8[:, :, 1], op=ALU.add), writes=[t_r])
            P.op("vector", lambda e: e.max(out=gm8[:], in_=gs[:]), writes=[t_r])
            P.op("vector", lambda e: e.tensor_scalar(out=pen[:], in0=gs[:], scalar1=gm8[:, 3:4], scalar2=None, op0=ALU.is_ge), writes=[t_r])
            P.op("vector", lambda e: e.tensor_scalar(out=pen[:], in0=pen[:], scalar1=BIG, scalar2=-BIG, op0=ALU.mult, op1=ALU.add), writes=[t_r])
            for g in range(8):
                P.op("vector", lambda e, g=g: e.tensor_scalar(out=mskd[:, g * 32:(g + 1) * 32], in0=bis[:, g * 32:(g + 1) * 32], scalar1=pen[:, g:g + 1], scalar2=None, op0=ALU.add), writes=[t_r])
            P.op("vector", lambda e: e.max(out=t8[:], in_=mskd[:]), writes=[t_r])
            P.op("vector", lambda e: e.max_index(out=idx8[:], in_max=t8[:], in_values=mskd[:]), writes=[t_r])
            P.op("vector", lambda e: e.tensor_copy(out=idxf[:], in_=idx8[:]), writes=[t_r])
            P.op("vector", lambda e: e.tensor_scalar(out=mskd[:], in0=mskd[:], scalar1=t8[:, 7:8], scalar2=None, op0=ALU.is_ge), writes=[t_r])
            P.op("vector", lambda e: e.tensor_copy(out=selb[:], in_=mskd[:]), writes=[t_r])
            P.op("tensor", lambda e: e.matmul(out=ps[7][:, 0:NE], lhsT=trib[:], rhs=selb[:], start=True, stop=False), reads=[t_r], writes=[pst[7]])
            P.op("tensor", lambda e: e.matmul(out=ps[7][:, 0:NE], lhsT=onesb_[:], rhs=selsum[:], start=False, stop=True), reads=[t_r], writes=[pst[7]])
            P.op("vector", lambda e: e.tensor_copy(out=posd[:], in_=ps[7][:, 0:NE]), reads=[pst[7]], writes=[t_r])
            P.op("vector", lambda e: e.tensor_tensor(out=selsum[:], in0=selsum[:], in1=selb[:], op=ALU.add), writes=[t_r])
            P.op("vector", lambda e: e.tensor_tensor(out=mskd[:], in0=mskd[:], in1=scs[:], op=ALU.mult), writes=[t_r])
            P.op("vector", lambda e: e.tensor_reduce(out=st_[:, 2:3], in_=mskd[:], axis=AX.X, op=ALU.add), writes=[t_r])
            P.op("vector", lambda e: e.reciprocal(out=st_[:, 3:4], in_=st_[:, 2:3]), writes=[t_r])
            P.op("vector", lambda e, gt=gt: e.tensor_scalar(out=gt[:], in0=mskd[:], scalar1=st_[:, 3:4], scalar2=2.5, op0=ALU.mult, op1=ALU.mult), reads=[t_r], writes=[gttok])
            P.op("sync", lambda e, gt=gt, rsl=rsl: e.dma_start(out=gates_o[rsl, :], in_=gt[:]), reads=[gttok], dma_sem=gtsem)
            for k in range(8):
                P.op("vector", lambda e, gt=gt, k=k, i=i: e.scalar_tensor_tensor(out=jk[:], in0=iob[:], scalar=idxf[:, k:k + 1], in1=gt[:], op0=ALU.is_equal, op1=ALU.mult, accum_out=gk_[:, i, k:k + 1]),
                     reads=[gttok], writes=[t_r])
                P.op("vector", lambda e, k=k: e.scalar_tensor_tensor(out=jk[:], in0=iob[:], scalar=idxf[:, k:k + 1], in1=posd[:], op0=ALU.is_equal, op1=ALU.mult, accum_out=pk_[:, k:k + 1]), writes=[t_r])
            P.op("vector", lambda e, i=i: e.scalar_tensor_tensor(out=dk_[:, i, :], in0=idxf[:], scalar=float(ESLOTS), in1=pk_[:], op0=ALU.mult, op1=ALU.add), writes=[t_r])
            P.op("vector", lambda e, i=i: e.tensor_scalar(out=dk_[:, i, :], in0=dk_[:, i, :], scalar1=srct[:, 0:1], scalar2=None, op0=ALU.add), writes=[t_r])
            P.op("vector", lambda e: e.tensor_scalar(out=pk_[:], in0=pk_[:], scalar1=float(CAPC), scalar2=-2.0e6, op0=ALU.is_ge, op1=ALU.mult), writes=[t_r])
            P.op("vector", lambda e, i=i: e.tensor_tensor(out=dk_[:, i, :], in0=dk_[:, i, :], in1=pk_[:], op=ALU.add), writes=[t_r])
            for wi, (wsrc, pb) in enumerate([(wgb, 6), (wub, 7)]):
                for ft in range(4):
                    for k in range(16):
                        P.op("tensor", lambda e, h2Tb=h2Tb, wsrc=wsrc, ft=ft, k=k, pb=pb: e.matmul(out=ps[pb][:, ft * 128:(ft + 1) * 128], lhsT=wsrc[:, k, ft * 128:(ft + 1) * 128], rhs=h2Tb[:, k, :],
                                                                                                 start=(k == 0), stop=(k == 15)), reads=[h2Tbtok, t_wo], writes=[pst[pb]])
            P.op("scalar", lambda e: e.activation(out=sgl[:], in_=ps[6][:, :], func=AF.Silu), reads=[pst[6]], writes=[t_sgl])
            P.op("vector", lambda e, hT=hT: e.tensor_tensor(out=hT[:].rearrange("p f t -> p (f t)"), in0=sgl[:], in1=ps[7][:, :], op=ALU.mult), reads=[pst[7], t_sgl], writes=[hTtok])
            for cbk in range(4):
                pb = 2 + cbk
                csl = slice(cbk * 512, (cbk + 1) * 512)
                for fc in range(4):
                    P.op("tensor", lambda e, hT=hT, fc=fc, csl=csl, pb=pb: e.matmul(out=ps[pb][:, :], lhsT=hT[:, fc, :], rhs=wdb[:, fc, csl], start=(fc == 0), stop=(fc == 3)),
                         reads=[hTtok, t_wo], writes=[pst[pb]])
                P.op("vector", lambda e, h2=h2, pb=pb, csl=csl: e.tensor_tensor(out=h2[:, csl], in0=ps[pb][:, :], in1=gt2b[:, csl], op=ALU.mult), reads=[pst[pb]], writes=[h2tok])
                P.op("gpsimd", lambda e, x1=x1, h2=h2, csl=csl: e.tensor_tensor(out=x1[:, csl], in0=x1[:, csl], in1=h2[:, csl], op=ALU.add), reads=[h2tok], writes=[x1tok])
            P.op("sync", lambda e, x1=x1, rsl=rsl: e.dma_start(out=x1s_o[rsl, :], in_=x1[:]), reads=[x1tok], dma_sem=x1sem)
        sK = P.dsem("q_dk")
        P.op("sync", lambda e: e.dma_start(out=destk_o.rearrange("(i p) k -> p i k", p=128), in_=dk_[:]), reads=[t_r], dma_sem=sK)
        P.op("sync", lambda e: e.dma_start(out=gk_o.rearrange("(i p) k -> p i k", p=128), in_=gk_[:]), reads=[t_r], dma_sem=sK)
        P.barrier()
        P.emit()
    return nc


NTOK = 2 * S
NEL = 32


def build_l2b_dense(nblk=NTOK // 512, nel=NEL):
    nc = bass.Bass("TRN2", target_bir_lowering=False)
    din = lambda n, s, d=F32: nc.dram_tensor(n, s, d, kind="ExternalInput").ap()
    h2a = din("h2a", [NTOK, D], BF16)
    gto = din("gto", [NTOK, NEL])
    wg = din("wg", [NEL, D, FF])
    wu = din("wu", [NEL, D, FF])
    wd = din("wd", [NEL, FF, D])
    ident = din("ident", [128, 128])
    part = nc.dram_tensor("part", [NTOK, D], F32, kind="ExternalOutput").ap()
    wgb_d = nc.dram_tensor("wgb_d", [NEL, 128, 16, FF], BF16).ap()
    wub_d = nc.dram_tensor("wub_d", [NEL, 128, 16, FF], BF16).ap()
    wdb_d = nc.dram_tensor("wdb_d", [NEL, 128, 4, D], BF16).ap()
    with contextlib.ExitStack() as st:
        P = Prog(nc, st)
        ps = [st.enter_context(nc.psum_tensor("ps%d" % i, [128, 512], F32)) for i in range(8)]
        pst = [Tok("ps%d" % i) for i in range(8)]
        sb = lambda n, s, d=F32: st.enter_context(nc.sbuf_tensor(n, s, d))
        identf = sb("identf", [128, 128])
        identb = sb("identb", [128, 128], BF16)
        t_c = Tok("c")
        P.op("sync", lambda e: e.dma_start(out=identf[:], in_=ident[:, :]), writes=[t_c], dma_sem=P.dsem("q_c"))
        P.op("vector", lambda e: e.tensor_copy(out=identb[:], in_=identf[:]), reads=[t_c], writes=[t_c])
        t_wd = [[Tok("wd") for _ in range(3)] for _ in range(nel)]
        with contextlib.ExitStack() as st2:
            stg = Ring(P, st2, nc, "stg", 3, [128, 8192], F32)
            cst = Ring(P, st2, nc, "cst", 3, [128, 8192], BF16)
            ci = 0
            for e_ in range(nel):
                for mi in range(3):
                    sg_, sgtok, sgsem = stg.next()
                    cb_, cbtok, cbsem = cst.next()
                    if mi < 2:
                        src = (wg, wu)[mi][e_].rearrange("(k p) f -> p k f", p=128)
                        dst = (wgb_d, wub_d)[mi][e_]
                        view = lambda t: t[:].rearrange("p (k f) -> p k f", k=16)
                    else:
                        src = wd[e_].rearrange("(k p) n -> p k n", p=128)
                        dst = wdb_d[e_]
                        view = lambda t: t[:].rearrange("p (k n) -> p k n", k=4)
                    P.op("sync", lambda e, sg_=sg_, src=src, view=view: e.dma_start(out=view(sg_), in_=src), writes=[sgtok], dma_sem=sgsem)
                    ceng = ("vector", "gpsimd", "scalar")[ci % 3]
                    ci += 1
                    if ceng == "scalar":
                        P.op("scalar", lambda e, sg_=sg_, cb_=cb_: e.activation(out=cb_[:], in_=sg_[:], func=AF.Copy), reads=[sgtok], writes=[cbtok])
                    else:
                        P.op(ceng, lambda e, sg_=sg_, cb_=cb_: e.tensor_copy(out=cb_[:], in_=sg_[:]), reads=[sgtok], writes=[cbtok])
                    P.op("sync", lambda e, cb_=cb_, dst=dst, view=view: e.dma_start(out=dst, in_=view(cb_)), reads=[cbtok], writes=[t_wd[e_][mi]], dma_sem=cbsem)
            P.barrier()
        h2r = Ring(P, st, nc, "h2t", 1, [128, 4, D], BF16)
        h2Tr = Ring(P, st, nc, "h2T", 2, [128, 16, 512], BF16)
        gr = Ring(P, st, nc, "gr", 2, [128, 4, NEL], F32)
        accr = Ring(P, st, nc, "acc", 1, [128, 4, D], F32)
        wgr = Ring(P, st, nc, "wgr", 2, [128, 16, FF], BF16)
        wur = Ring(P, st, nc, "wur", 2, [128, 16, FF], BF16)
        wdr = Ring(P, st, nc, "wdr", 2, [128, 4, D], BF16)
        hTr = Ring(P, st, nc, "hT", 2, [128, 4, 512], BF16)
        sglr = Ring(P, st, nc, "sgl", 2, [128, 512], F32)
        for nb in range(nblk):
            h2t, h2tok, h2sem = h2r.next()
            h2T, h2Ttok, _ = h2Tr.next()
            g_, gtok, gsem = gr.next()
            acc, acctok, accsem = accr.next()
            rows = slice(nb * 512, (nb + 1) * 512)
            P.op("sync", lambda e, h2t=h2t, rows=rows: e.dma_start(out=h2t[:], in_=h2a[rows, :].rearrange("(t p) d -> p t d", p=128)), writes=[h2tok], dma_sem=h2sem)
            P.op("sync", lambda e, g_=g_, rows=rows: e.dma_start(out=g_[:], in_=gto[rows, :].rearrange("(t p) n -> p t n", p=128)), writes=[gtok], dma_sem=gsem)
            P.op("gpsimd", lambda e, acc=acc: e.memset(acc[:], 0.0), writes=[acctok])
            for tt in range(4):
                for half in range(2):
                    pb = half
                    for c8 in range(8):
                        c = half * 8 + c8
                        P.op("tensor", lambda e, h2t=h2t, tt=tt, c=c, c8=c8, pb=pb: e.transpose(out=ps[pb][:].bitcast(BF16)[:, c8 * 128:(c8 + 1) * 128], in_=h2t[:, tt, c * 128:(c + 1) * 128], identity=identb[:]),
                             reads=[h2tok], writes=[pst[pb]])
                    P.op("scalar", lambda e, h2T=h2T, tt=tt, half=half, pb=pb: e.activation(out=h2T[:, half * 8:(half + 1) * 8, tt * 128:(tt + 1) * 128], in_=ps[pb][:].bitcast(BF16).rearrange("p (c t) -> p c t", c=8), func=AF.Copy),
                         reads=[pst[pb]], writes=[h2Ttok])
            for e_ in range(nel):
                wgt, wgtok, wgsem = wgr.next()
                wut, wutok, wusem = wur.next()
                wdt, wdtok, wdsem = wdr.next()
                P.op("sync", lambda e, wgt=wgt, e_=e_: e.dma_start(out=wgt[:], in_=wgb_d[e_]), reads=[t_wd[e_][0]], writes=[wgtok], dma_sem=wgsem)
                P.op("sync", lambda e, wut=wut, e_=e_: e.dma_start(out=wut[:], in_=wub_d[e_]), reads=[t_wd[e_][1]], writes=[wutok], dma_sem=wusem)
                P.op("sync", lambda e, wdt=wdt, e_=e_: e.dma_start(out=wdt[:], in_=wdb_d[e_]), reads=[t_wd[e_][2]], writes=[wdtok], dma_sem=wdsem)
                hT, hTtok, _ = hTr.next()
                for ft in range(4):
                    pg, pu = 2 + (ft % 2) * 2, 3 + (ft % 2) * 2
                    fsl = slice(ft * 128, (ft + 1) * 128)
                    for k in range(16):
                        P.op("tensor", lambda e, wgt=wgt, h2T=h2T, k=k, fsl=fsl, pg=pg: e.matmul(out=ps[pg][:, :], lhsT=wgt[:, k, fsl], rhs=h2T[:, k, :], start=(k == 0), stop=(k == 15)),
                             reads=[wgtok, h2Ttok], writes=[pst[pg]])
                    for k in range(16):
                        P.op("tensor", lambda e, wut=wut, h2T=h2T, k=k, fsl=fsl, pu=pu: e.matmul(out=ps[pu][:, :], lhsT=wut[:, k, fsl], rhs=h2T[:, k, :], start=(k == 0), stop=(k == 15)),
                             reads=[wutok, h2Ttok], writes=[pst[pu]])
                    sgl, sgltok, _ = sglr.next()
                    P.op("scalar", lambda e, sgl=sgl, pg=pg: e.activation(out=sgl[:], in_=ps[pg][:, :], func=AF.Silu), reads=[pst[pg]], writes=[sgltok])
                    P.op("vector", lambda e, hT=hT, ft=ft, sgl=sgl, pu=pu: e.tensor_tensor(out=hT[:, ft, :], in0=sgl[:], in1=ps[pu][:, :], op=ALU.mult), reads=[pst[pu], sgltok], writes=[hTtok])
                for tt in range(4):
                    for cbk in range(4):
                        pb = 6 + ((tt * 4 + cbk) % 2)
                        csl = slice(cbk * 512, (cbk + 1) * 512)
                        for fc in range(4):
                            P.op("tensor", lambda e, hT=hT, wdt=wdt, tt=tt, fc=fc, csl=csl, pb=pb: e.matmul(out=ps[pb][:, :], lhsT=hT[:, fc, tt * 128:(tt + 1) * 128], rhs=wdt[:, fc, csl], start=(fc == 0), stop=(fc == 3)),
                                 reads=[hTtok, wdtok], writes=[pst[pb]])
                        P.op("vector", lambda e, acc=acc, g_=g_, tt=tt, csl=csl, pb=pb, e_=e_: e.scalar_tensor_tensor(out=acc[:, tt, csl], in0=ps[pb][:, :], scalar=g_[:, tt, e_:e_ + 1], in1=acc[:, tt, csl], op0=ALU.mult, op1=ALU.add),
                             reads=[pst[pb], gtok], writes=[acctok])
            P.op("sync", lambda e, acc=acc, rows=rows: e.dma_start(out=part[rows, :].rearrange("(t p) d -> p t d", p=128), in_=acc[:]), reads=[acctok], dma_sem=accsem)
        P.barrier()
        P.emit()
    return nc


NOWN = NEL * ESLOTS
ZROW = NOWN + 128


def build_l2b(ntile=NTOK // 128, nel=NEL):
    nc = bass.Bass("TRN2", target_bir_lowering=False)
    din = lambda n, s, d=F32: nc.dram_tensor(n, s, d, kind="ExternalInput").ap()
    h2a = din("h2a", [NTOK, D], BF16)
    destk = din("destk", [NTOK, 8])
    gkin = din("gkin", [NTOK, 8])
    lohi = din("lohi", [128, 4])
    wg = din("wg", [NEL, D, FF])
    wu = din("wu", [NEL, D, FF])
    wd = din("wd", [NEL, FF, D])
    ident = din("ident", [128, 128])
    part = nc.dram_tensor("part", [NTOK, D], F32, kind="ExternalOutput").ap()
    wgb_d = nc.dram_tensor("wgb_d", [NEL, 128, 16, FF], BF16).ap()
    wub_d = nc.dram_tensor("wub_d", [NEL, 128, 16, FF], BF16).ap()
    wdb_d = nc.dram_tensor("wdb_d", [NEL, 128, 4, D], BF16).ap()
    xe_d = nc.dram_tensor("xe_d", [NOWN + 256, D], BF16).ap()
    y_d = nc.dram_tensor("y_d", [NOWN + 256, D], BF16).ap()
    NT = NTOK // 128
    with contextlib.ExitStack() as st:
        P = Prog(nc, st)
        ps = [st.enter_context(nc.psum_tensor("ps%d" % i, [128, 512], F32)) for i in range(8)]
        pst = [Tok("ps%d" % i) for i in range(8)]
        sb = lambda n, s, d=F32: st.enter_context(nc.sbuf_tensor(n, s, d))
        identf = sb("identf", [128, 128])
        identb = sb("identb", [128, 128], BF16)
        lh = sb("lh", [128, 4])
        sidx = sb("sidx", [128, NT * 8], I32)
        gidx = sb("gidx", [128, NT * 8], I32)
        wk = sb("wk", [128, NT * 8])
        t_c = Tok("c")
        cs = P.dsem("q_c")
        P.op("sync", lambda e: e.dma_start(out=identf[:], in_=ident[:, :]), writes=[t_c], dma_sem=cs)
        P.op("sync", lambda e: e.dma_start(out=lh[:], in_=lohi[:, :]), writes=[t_c], dma_sem=cs)
        t_idx = Tok("idx")
        with contextlib.ExitStack() as st2:
            dall = st2.enter_context(nc.sbuf_tensor("dall", [128, NT * 8], F32))
            own = st2.enter_context(nc.sbuf_tensor("own", [128, NT * 8], F32))
            tmp = st2.enter_context(nc.sbuf_tensor("tmpi", [128, NT * 8], F32))
            P.op("sync", lambda e: e.dma_start(out=dall[:].rearrange("p (i k) -> p i k", k=8), in_=destk.rearrange("(i p) k -> p i k", p=128)), writes=[t_c], dma_sem=cs)
            P.op("sync", lambda e: e.dma_start(out=wk[:].rearrange("p (i k) -> p i k", k=8), in_=gkin.rearrange("(i p) k -> p i k", p=128)), writes=[t_c], dma_sem=cs)
            P.barrier()
            P.op("vector", lambda e: e.tensor_copy(out=identb[:], in_=identf[:]), writes=[t_idx])
            V = lambda fn: P.op("vector", fn, writes=[t_idx])
            V(lambda e: e.tensor_scalar(out=own[:], in0=dall[:], scalar1=lh[:, 0:1], scalar2=None, op0=ALU.is_ge))
            V(lambda e: e.tensor_scalar(out=tmp[:], in0=dall[:], scalar1=lh[:, 1:2], scalar2=None, op0=ALU.is_lt))
            V(lambda e: e.tensor_tensor(out=own[:], in0=own[:], in1=tmp[:], op=ALU.mult))
            V(lambda e: e.tensor_tensor(out=wk[:], in0=wk[:], in1=own[:], op=ALU.mult))
            V(lambda e: e.tensor_scalar(out=dall[:], in0=dall[:], scalar1=lh[:, 0:1], scalar2=None, op0=ALU.subtract))
            V(lambda e: e.tensor_scalar(out=tmp[:], in0=dall[:], scalar1=lh[:, 2:3], scalar2=None, op0=ALU.subtract))
            V(lambda e: e.tensor_tensor(out=tmp[:], in0=tmp[:], in1=own[:], op=ALU.mult))
            V(lambda e: e.tensor_scalar(out=tmp[:], in0=tmp[:], scalar1=lh[:, 2:3], scalar2=None, op0=ALU.add))
            V(lambda e: e.tensor_copy(out=sidx[:], in_=tmp[:]))
            V(lambda e: e.tensor_scalar(out=tmp[:], in0=dall[:], scalar1=lh[:, 3:4], scalar2=None, op0=ALU.subtract))
            V(lambda e: e.tensor_tensor(out=tmp[:], in0=tmp[:], in1=own[:], op=ALU.mult))
            V(lambda e: e.tensor_scalar(out=tmp[:], in0=tmp[:], scalar1=lh[:, 3:4], scalar2=None, op0=ALU.add))
            V(lambda e: e.tensor_copy(out=gidx[:], in_=tmp[:]))
            P.barrier()
        t_wd = [[Tok("wd") for _ in range(3)] for _ in range(nel)]
        with contextlib.ExitStack() as st2:
            stg = Ring(P, st2, nc, "stg", 3, [128, 8192], F32)
            cst = Ring(P, st2, nc, "cst", 3, [128, 8192], BF16)
            hr = Ring(P, st2, nc, "hr", 3, [128, D], BF16)
            zt = st2.enter_context(nc.sbuf_tensor("zt", [128, D], BF16))
            P.op("vector", lambda e: e.memset(zt[:], 0.0), writes=[t_idx])
            P.op("sync", lambda e: e.dma_start(out=y_d[ZROW:ZROW + 128, :], in_=zt[:]), reads=[t_idx], dma_sem=P.dsem("q_z"))
            sc_sem = [P.dsem("q_sc%d" % i) for i in range(3)]
            ci = 0
            for i in range(ntile):
                ht, htok, hsem = hr.next()
                P.op("sync", lambda e, ht=ht, i=i: e.dma_start(out=ht[:], in_=h2a[i * 128:(i + 1) * 128, :]), writes=[htok], dma_sem=hsem)
                for k in range(8):
                    col = i * 8 + k
                    P.op("gpsimd", lambda e, ht=ht, col=col: e.indirect_dma_start(out=xe_d[:, :], out_offset=bass.IndirectOffsetOnAxis(ap=sidx[:, col:col + 1], axis=0), in_=ht[:], in_offset=None),
                         reads=[htok, t_idx], dma_sem=sc_sem[hr.i])
            for e_ in range(nel):
                for mi in range(3):
                    sg_, sgtok, sgsem = stg.next()
                    cb_, cbtok, cbsem = cst.next()
                    if mi < 2:
                        src = (wg, wu)[mi][e_].rearrange("(k p) f -> p k f", p=128)
                        dst = (wgb_d, wub_d)[mi][e_]
                        view = lambda t: t[:].rearrange("p (k f) -> p k f", k=16)
                    else:
                        src = wd[e_].rearrange("(k p) n -> p k n", p=128)
                        dst = wdb_d[e_]
                        view = lambda t: t[:].rearrange("p (k n) -> p k n", k=4)
                    P.op("sync", lambda e, sg_=sg_, src=src, view=view: e.dma_start(out=view(sg_), in_=src), writes=[sgtok], dma_sem=sgsem)
                    ceng = ("vector", "scalar")[ci % 2]
                    ci += 1
                    if ceng == "scalar":
                        P.op("scalar", lambda e, sg_=sg_, cb_=cb_: e.activation(out=cb_[:], in_=sg_[:], func=AF.Copy), reads=[sgtok], writes=[cbtok])
                    else:
                        P.op(ceng, lambda e, sg_=sg_, cb_=cb_: e.tensor_copy(out=cb_[:], in_=sg_[:]), reads=[sgtok], writes=[cbtok])
                    P.op("sync", lambda e, cb_=cb_, dst=dst, view=view: e.dma_start(out=dst, in_=view(cb_)), reads=[cbtok], writes=[t_wd[e_][mi]], dma_sem=cbsem)
            P.barrier()
        with contextlib.ExitStack() as st2:
            h2r = Ring(P, st2, nc, "h2t", 1, [128, 4, D], BF16)
            h2Tr = Ring(P, st2, nc, "h2T", 1, [128, 16, 512], BF16)
            yr = Ring(P, st2, nc, "yt", 1, [128, 4, D], BF16)
            wgr = Ring(P, st2, nc, "wgr", 2, [128, 16, FF], BF16)
            wur = Ring(P, st2, nc, "wur", 2, [128, 16, FF], BF16)
            wdr = Ring(P, st2, nc, "wdr", 2, [128, 4, D], BF16)
            hTr = Ring(P, st2, nc, "hT", 2, [128, 4, 512], BF16)
            sglr = Ring(P, st2, nc, "sgl", 2, [128, 512], F32)
            nsb = ESLOTS // 512
            for e_ in range(nel):
                wgt, wgtok, wgsem = wgr.next()
                wut, wutok, wusem = wur.next()
                wdt, wdtok, wdsem = wdr.next()
                P.op("sync", lambda e, wgt=wgt, e_=e_: e.dma_start(out=wgt[:], in_=wgb_d[e_]), reads=[t_wd[e_][0]], writes=[wgtok], dma_sem=wgsem)
                P.op("sync", lambda e, wut=wut, e_=e_: e.dma_start(out=wut[:], in_=wub_d[e_]), reads=[t_wd[e_][1]], writes=[wutok], dma_sem=wusem)
                P.op("sync", lambda e, wdt=wdt, e_=e_: e.dma_start(out=wdt[:], in_=wdb_d[e_]), reads=[t_wd[e_][2]], writes=[wdtok], dma_sem=wdsem)
                for sb_ in range(nsb):
                    r0 = e_ * ESLOTS + sb_ * 512
                    h2t, h2tok, h2sem = h2r.next()
                    h2T, h2Ttok, _ = h2Tr.next()
                    yt, ytok, ysem = yr.next()
                    P.op("sync", lambda e, h2t=h2t, r0=r0: e.dma_start(out=h2t[:], in_=xe_d[r0:r0 + 512, :].rearrange("(t p) d -> p t d", p=128)), writes=[h2tok], dma_sem=h2sem)
                    for tt in range(4):
                        for half in range(2):
                            pb = half
                            for c8 in range(8):
                                c = half * 8 + c8
                                P.op("tensor", lambda e, h2t=h2t, tt=tt, c=c, c8=c8, pb=pb: e.transpose(out=ps[pb][:].bitcast(BF16)[:, c8 * 128:(c8 + 1) * 128], in_=h2t[:, tt, c * 128:(c + 1) * 128], identity=identb[:]),
                                     reads=[h2tok], writes=[pst[pb]])
                            P.op("scalar", lambda e, h2T=h2T, tt=tt, half=half, pb=pb: e.activation(out=h2T[:, half * 8:(half + 1) * 8, tt * 128:(tt + 1) * 128], in_=ps[pb][:].bitcast(BF16).rearrange("p (c t) -> p c t", c=8), func=AF.Copy),
                                 reads=[pst[pb]], writes=[h2Ttok])
                    hT, hTtok, _ = hTr.next()
                    for ft in range(4):
                        pg, pu = 2 + (ft % 2) * 2, 3 + (ft % 2) * 2
                        fsl = slice(ft * 128, (ft + 1) * 128)
                        for k in range(16):
                            P.op("tensor", lambda e, wgt=wgt, h2T=h2T, k=k, fsl=fsl, pg=pg: e.matmul(out=ps[pg][:, :], lhsT=wgt[:, k, fsl], rhs=h2T[:, k, :], start=(k == 0), stop=(k == 15)),
                                 reads=[wgtok, h2Ttok], writes=[pst[pg]])
                        for k in range(16):
                            P.op("tensor", lambda e, wut=wut, h2T=h2T, k=k, fsl=fsl, pu=pu: e.matmul(out=ps[pu][:, :], lhsT=wut[:, k, fsl], rhs=h2T[:, k, :], start=(k == 0), stop=(k == 15)),
                                 reads=[wutok, h2Ttok], writes=[pst[pu]])
                        sgl, sgltok, _ = sglr.next()
                        P.op("scalar", lambda e, sgl=sgl, pg=pg: e.activation(out=sgl[:], in_=ps[pg][:, :], func=AF.Silu), reads=[pst[pg]], writes=[sgltok])
                        P.op("vector", lambda e, hT=hT, ft=ft, sgl=sgl, pu=pu: e.tensor_tensor(out=hT[:, ft, :], in0=sgl[:], in1=ps[pu][:, :], op=ALU.mult), reads=[pst[pu], sgltok], writes=[hTtok])
                    for tt in range(4):
                        for cbk in range(4):
                            pb = 6 + ((tt * 4 + cbk) % 2)
                            csl = slice(cbk * 512, (cbk + 1) * 512)
                            for fc in range(4):
                                P.op("tensor", lambda e, hT=hT, wdt=wdt, tt=tt, fc=fc, csl=csl, pb=pb: e.matmul(out=ps[pb][:, :], lhsT=hT[:, fc, tt * 128:(tt + 1) * 128], rhs=wdt[:, fc, csl], start=(fc == 0), stop=(fc == 3)),
                                     reads=[hTtok, wdtok], writes=[pst[pb]])
                            if (tt * 4 + cbk) % 2 == 0:
                                P.op("vector", lambda e, yt=yt, tt=tt, csl=csl, pb=pb: e.tensor_copy(out=yt[:, tt, csl], in_=ps[pb][:, :]), reads=[pst[pb]], writes=[ytok])
                            else:
                                P.op("scalar", lambda e, yt=yt, tt=tt, csl=csl, pb=pb: e.activation(out=yt[:, tt, csl], in_=ps[pb][:, :], func=AF.Copy), reads=[pst[pb]], writes=[ytok])
                    P.op("sync", lambda e, yt=yt, r0=r0: e.dma_start(out=y_d[r0:r0 + 512, :].rearrange("(t p) d -> p t d", p=128), in_=yt[:]), reads=[ytok], dma_sem=ysem)
            P.barrier()
        with contextlib.ExitStack() as st2:
            gr_ = Ring(P, st2, nc, "gy", 6, [128, D], BF16)
            ar = Ring(P, st2, nc, "acc", 2, [128, D], F32)
            for i in range(ntile):
                acc, acctok, accsem = ar.next()
                for k in range(8):
                    col = i * 8 + k
                    yk, yktok, yksem = gr_.next()
                    P.op("gpsimd", lambda e, yk=yk, col=col: e.indirect_dma_start(out=yk[:], out_offset=None, in_=y_d[:, :], in_offset=bass.IndirectOffsetOnAxis(ap=gidx[:, col:col + 1], axis=0)),
                         writes=[yktok], dma_sem=yksem)
                    if k == 0:
                        P.op("vector", lambda e, acc=acc, yk=yk, col=col: e.tensor_scalar(out=acc[:], in0=yk[:], scalar1=wk[:, col:col + 1], scalar2=None, op0=ALU.mult), reads=[yktok], writes=[acctok])
                    else:
                        P.op("vector", lambda e, acc=acc, yk=yk, col=col: e.scalar_tensor_tensor(out=acc[:], in0=yk[:], scalar=wk[:, col:col + 1], in1=acc[:], op0=ALU.mult, op1=ALU.add), reads=[yktok], writes=[acctok])
                P.op("sync", lambda e, acc=acc, i=i: e.dma_start(out=part[i * 128:(i + 1) * 128, :], in_=acc[:]), reads=[acctok], dma_sem=accsem)
            P.barrier()
        P.emit()
    return nc


def build_l3():
    nc = bass.Bass("TRN2", target_bir_lowering=False)
    din = lambda n, s, d=F32: nc.dram_tensor(n, s, d, kind="ExternalInput").ap()
    x1s = din("x1s", [2048, D])
    parts = din("parts", [8, 2048, D])
    gt2 = din("gt2", [1, D])
    out = nc.dram_tensor("out", [2048, D], F32, kind="ExternalOutput").ap()
    with contextlib.ExitStack() as st:
        P = Prog(nc, st)
        sb = lambda n, s, d=F32: st.enter_context(nc.sbuf_tensor(n, s, d))
        gt2b = sb("gt2b", [128, D])
        t_c = Tok("c")
        P.op("sync", lambda e: e.dma_start(out=gt2b[:], in_=gt2[0:1, :].partition_broadcast(128)), writes=[t_c], dma_sem=P.dsem("q_c"))
        pr = Ring(P, st, nc, "pr", 4, [128, D], F32)
        xr = Ring(P, st, nc, "xr", 2, [128, D], F32)
        ar = Ring(P, st, nc, "ar", 2, [128, D], F32)
        for i in range(16):
            rsl = slice(i * 128, (i + 1) * 128)
            xt, xtok, xsem = xr.next()
            acc, acctok, accsem = ar.next()
            P.op("sync", lambda e, xt=xt, rsl=rsl: e.dma_start(out=xt[:], in_=x1s[rsl, :]), writes=[xtok], dma_sem=xsem)
            for c in range(8):
                pt, ptok, psem = pr.next()
                P.op("sync", lambda e, pt=pt, c=c, rsl=rsl: e.dma_start(out=pt[:], in_=parts[c, rsl, :]), writes=[ptok], dma_sem=psem)
                eng = "vector" if c % 2 == 0 else "gpsimd"
                if c == 0:
                    P.op("vector", lambda e, acc=acc, pt=pt: e.tensor_copy(out=acc[:], in_=pt[:]), reads=[ptok], writes=[acctok])
                else:
                    P.op(eng, lambda e, acc=acc, pt=pt: e.tensor_tensor(out=acc[:], in0=acc[:], in1=pt[:], op=ALU.add), reads=[ptok], writes=[acctok])
            P.op("vector", lambda e, acc=acc: e.tensor_tensor(out=acc[:], in0=acc[:], in1=gt2b[:], op=ALU.mult), reads=[t_c], writes=[acctok])
            P.op("gpsimd", lambda e, acc=acc, xt=xt: e.tensor_tensor(out=acc[:], in0=acc[:], in1=xt[:], op=ALU.add), reads=[xtok], writes=[acctok])
            P.op("sync", lambda e, acc=acc, rsl=rsl: e.dma_start(out=out[rsl, :], in_=acc[:]), reads=[acctok], dma_sem=accsem)
        P.barrier()
        P.emit()
    return nc


def _run(nc, maps):
    return run_bass_kernel_spmd(nc, maps, core_ids=list(range(len(maps)))).results


def run_l1(inputs):
    r1 = _run(build_l1(), [l1_inputs(inputs, c) for c in range(8)])
    cat = np.zeros((2, S, D), r1[0]["mix"].dtype)
    for c in range(8):
        b, j = c // 4, c % 4
        m = r1[c]["mix"]
        cat[b, :, j * 256:(j + 1) * 256] = m[:, 0:256]
        cat[b, :, 1024 + j * 256:1024 + (j + 1) * 256] = m[:, 256:512]
    mods = [r1[0]["modrow"].reshape(-1), r1[4]["modrow"].reshape(-1)]
    return cat, mods


def run_l2a(inputs, cat, mods):
    f = lambda a: np.ascontiguousarray(a, dtype=np.float32)
    ident = np.eye(128, dtype=np.float32)
    maps = []
    for c in range(8):
        b, r = c // 4, c % 4
        mod = mods[b]
        rows = np.stack([mod[2 * D:3 * D], mod[4 * D:5 * D], mod[3 * D:4 * D], mod[5 * D:6 * D], inputs["g_ffn"][0], np.zeros(D, np.float32)])
        maps.append({
            "xo": f(inputs["x"][b, r * 2048:(r + 1) * 2048]), "cat": np.ascontiguousarray(cat[b, r * 2048:(r + 1) * 2048]),
            "w_out": f(inputs["w_out"][0]), "rows": f(rows), "w_router": f(inputs["w_router"][0]),
            "rbias": f(inputs["router_bias"][0].reshape(1, NE)), "wsg": f(inputs["w_sh_gate"][0]), "wsu": f(inputs["w_sh_up"][0]),
            "wsd": f(inputs["w_sh_down"][0]), "ident": ident,
            "tris": np.triu(np.ones((128, 128), np.float32), 1), "iotae": np.arange(NE, dtype=np.float32).reshape(1, NE),
            "srcoff": np.full((128, 1), c * CAPC, np.float32),
        })
    r = _run(build_l2a(), maps)
    x1s = np.concatenate([r[c]["x1s"] for c in range(8)], 0)
    h2 = np.concatenate([r[c]["h2"] for c in range(8)], 0)
    gates = np.concatenate([r[c]["gates"] for c in range(8)], 0)
    destk = np.concatenate([r[c]["destk"] for c in range(8)], 0)
    gk = np.concatenate([r[c]["gk"] for c in range(8)], 0)
    return x1s, h2, gates, destk, gk


def l2b_maps(inputs, h2, destk, gk, cores=range(8)):
    f = lambda a: np.ascontiguousarray(a, dtype=np.float32)
    ident = np.eye(128, dtype=np.float32)
    maps = []
    for c in cores:
        es = slice(c * NEL, (c + 1) * NEL)
        lohi = np.zeros((128, 4), np.float32)
        lohi[:, 0] = c * NOWN
        lohi[:, 1] = (c + 1) * NOWN
        lohi[:, 2] = NOWN + np.arange(128)
        lohi[:, 3] = ZROW
        maps.append({"h2a": h2, "destk": f(destk), "gkin": f(gk), "lohi": lohi, "wg": f(inputs["w_exp_gate"][0, es]), "wu": f(inputs["w_exp_up"][0, es]),
                     "wd": f(inputs["w_exp_down"][0, es]), "ident": ident})
    return maps


def run_l2b(inputs, h2, destk, gk):
    r = _run(build_l2b(), l2b_maps(inputs, h2, destk, gk))
    return [r[c]["part"] for c in range(8)]


def run_l3(x1s, parts, mods):
    maps = []
    for c in range(8):
        b = c // 4
        rs = slice(c * 2048, (c + 1) * 2048)
        maps.append({"x1s": np.ascontiguousarray(x1s[rs]), "parts": np.ascontiguousarray(np.stack([p[rs] for p in parts])),
                     "gt2": np.ascontiguousarray(mods[b][5 * D:6 * D].reshape(1, D))})
    r = _run(build_l3(), maps)
    return np.concatenate([r[c]["out"] for c in range(8)], 0).reshape(2, S, D)


def kernel(**inputs):
    cat, mods = run_l1(inputs)
    x1s, h2, gates, destk, gk = run_l2a(inputs, cat, mods)
    parts = run_l2b(inputs, h2, destk, gk)
    return run_l3(x1s, parts, mods)
```

```python
import contextlib
import math
import numpy as np
import concourse.bass as bass
import concourse.mybir as mybir
from concourse.bass_utils import run_bass_kernel_spmd

F32 = mybir.dt.float32
BF16 = mybir.dt.bfloat16
I32 = mybir.dt.int32
U32 = mybir.dt.uint32
AF = mybir.ActivationFunctionType
ALU = mybir.AluOpType
AX = mybir.AxisListType

D = 2048
S = 8192
EPS = 1e-6
NE = 256
FF = 512
CAP = 256
NSL = CAP // 128
ENGS = ("tensor", "vector", "scalar", "gpsimd", "sync")


class Tok:
    __slots__ = ("name", "last_w", "readers")

    def __init__(self, name=""):
        self.name = name
        self.last_w = None
        self.readers = []


class Op:
    __slots__ = ("eng", "fn", "deps", "dma_sem", "dma_val", "sig", "needs_sig", "idx")

    def __init__(self, eng, fn):
        self.eng = eng
        self.fn = fn
        self.deps = []
        self.dma_sem = None
        self.dma_val = 0
        self.sig = 0
        self.needs_sig = False


class Prog:
    def __init__(self, nc, stack):
        self.nc = nc
        self.stack = stack
        self.ops = []
        self.sems = {e: stack.enter_context(nc.semaphore("s_" + e)) for e in ENGS}
        self.dma_sem_count = {}
        self.dma_since_barrier = []
        self.nsem = 0

    def dsem(self, name):
        s = self.stack.enter_context(self.nc.semaphore(name))
        self.dma_sem_count[id(s)] = 0
        self.nsem += 1
        return s

    def op(self, eng, fn, reads=(), writes=(), dma_sem=None, extra_deps=()):
        o = Op(eng, fn)
        deps = []
        for r in reads:
            if r.last_w is not None:
                deps.append(r.last_w)
            r.readers.append(o)
        for w in writes:
            if w.last_w is not None:
                deps.append(w.last_w)
            deps.extend(w.readers)
            w.last_w = o
            w.readers = []
        deps.extend(extra_deps)
        seen = set()
        dd = []
        for d in deps:
            if d is o or id(d) in seen:
                continue
            seen.add(id(d))
            dd.append(d)
        best = {}
        keep = []
        for d in dd:
            if d.dma_sem is not None:
                keep.append(d)
            elif d.eng not in best or best[d.eng].idx < d.idx:
                best[d.eng] = d
        o.deps = keep + list(best.values())
        o.idx = len(self.ops)
        if dma_sem is not None:
            o.dma_sem = dma_sem
            self.dma_sem_count[id(dma_sem)] += 16
            o.dma_val = self.dma_sem_count[id(dma_sem)]
            self.dma_since_barrier.append(o)
        self.ops.append(o)
        return o

    def barrier(self):
        lasts = []
        for e in ENGS:
            for o in reversed(self.ops):
                if o.eng == e and o.dma_sem is None and o.fn is not None:
                    lasts.append(o)
                    break
        dmas = list(self.dma_since_barrier)
        self.dma_since_barrier = []
        for e in ENGS:
            self.op(e, None, extra_deps=lasts + dmas)

    def emit(self):
        nc = self.nc
        for o in self.ops:
            for d in o.deps:
                if d.dma_sem is None:
                    if d.eng == "tensor" and o.eng == "tensor":
                        continue
                    d.needs_sig = True
        counters = {e: 0 for e in ENGS}
        per_eng = {e: [] for e in ENGS}
        for o in self.ops:
            if o.dma_sem is None and o.needs_sig:
                counters[o.eng] += 1
                o.sig = counters[o.eng]
            per_eng[o.eng].append(o)
        sems = self.sems

        def run_engine(ename, eng):
            waited = {}
            for o in per_eng[ename]:
                for d in o.deps:
                    if d.dma_sem is not None:
                        key, sem, val = id(d.dma_sem), d.dma_sem, d.dma_val
                    else:
                        if d.eng == "tensor" and ename == "tensor":
                            continue
                        key, sem, val = d.eng, sems[d.eng], d.sig
                    if waited.get(key, 0) >= val:
                        continue
                    waited[key] = val
                    eng.wait_ge(sem, val)
                if o.fn is None:
                    continue
                inst = o.fn(eng)
                if o.dma_sem is not None:
                    inst.then_inc(o.dma_sem, 16)
                elif o.needs_sig:
                    inst.then_inc(sems[ename], 1)

        with nc.Block() as block:
            @block.tensor
            def _(e):
                run_engine("tensor", e)

            @block.vector
            def _(e):
                run_engine("vector", e)

            @block.scalar
            def _(e):
                run_engine("scalar", e)

            @block.gpsimd
            def _(e):
                run_engine("gpsimd", e)

            @block.sync
            def _(e):
                run_engine("sync", e)


class Ring:
    def __init__(self, P, st, nc, name, n, shape, dtype):
        self.t = [st.enter_context(nc.sbuf_tensor("%s%d" % (name, i), shape, dtype)) for i in range(n)]
        self.tok = [Tok("%s%d" % (name, i)) for i in range(n)]
        self.sem = [P.dsem("q_%s%d" % (name, i)) for i in range(n)]
        self.n = n
        self.i = -1

    def next(self):
        self.i = (self.i + 1) % self.n
        return self.t[self.i], self.tok[self.i], self.sem[self.i]


def _t5_bucket_np(rel):
    half, me = 16, 8
    ret = np.where(rel > 0, half, 0)
    n = np.abs(rel)
    nf = np.maximum(n, 1).astype(np.float32)
    large = me + (np.log(nf / np.float32(me)) / np.float32(math.log(128 / 8)) * np.float32(half - me)).astype(np.int32)
    large = np.minimum(large, half - 1)
    return ret + np.where(n < me, n, large)


def _consts():
    c = {}
    c["ident"] = np.eye(128, dtype=np.float32)
    c["antiid"] = np.eye(128, dtype=np.float32)[::-1].copy()
    ob = np.zeros((128, 128), np.float32)
    ob[:64, :64] = 1.0 / 64
    ob[64:, 64:] = 1.0 / 64
    c["onesblk"] = ob
    m = np.arange(1280)
    b = _t5_bucket_np(639 - m)
    oh = np.zeros((32, 1280), np.float32)
    oh[b, m] = 1.0
    c["ohg"] = oh
    s = np.arange(128)
    same = (s[:, None] // 64) == (s[None, :] // 64)
    c["mask_f"] = (same & (s[:, None] <= s[None, :])).astype(np.float32)
    c["mask_b"] = (same & (s[:, None] >= s[None, :])).astype(np.float32)
    return c


def build_l1(dbg=False):
    nc = bass.Bass("TRN2", target_bir_lowering=False)
    din = lambda n, s, d=F32: nc.dram_tensor(n, s, d, kind="ExternalInput").ap()
    xb = din("xb", [S, D] if dbg != "H" else [128, 128])
    cT = din("cT", [128, 16])
    w_ada = din("w_ada", [D, 6 * D] if dbg != "H" else [128, 128])
    b_adaT = din("b_adaT", [128, 96])
    g_mixT = din("g_mixT", [128, 16])
    w_own = din("w_own", [D, 2048])
    gq2 = din("gq2", [128, 1])
    gk2 = din("gk2", [128, 1])
    lamv = din("lamv", [1, 256])
    rb_own = din("rb_own", [32, 2])
    lbl = din("lbl", [128, 8])
    gsub = din("gsub", [1, 128])
    ghg = din("ghg", [1, 128])
    ident = din("ident", [128, 128])
    antiid = din("antiid", [128, 128])
    onesblk = din("onesblk", [128, 128])
    ohg = din("ohg", [32, 1280])
    mask_f = din("mask_f", [128, 128])
    mask_b = din("mask_b", [128, 128])
    mix = nc.dram_tensor("mix", [S, 512], BF16, kind="ExternalOutput").ap()
    modrow = nc.dram_tensor("modrow", [96, 128], F32, kind="ExternalOutput").ap()
    hT_d = nc.dram_tensor("hT_d", [128, 16, S], BF16).ap()
    G_d = nc.dram_tensor("G_d", [2, 1280], F32).ap()
    of_d = nc.dram_tensor("of_d", [2, S, 128], F32).ap()

    with contextlib.ExitStack() as st0:
        P = Prog(nc, st0)
        ps = [st0.enter_context(nc.psum_tensor("ps%d" % i, [128, 512], F32)) for i in range(8)]
        pst = [Tok("ps%d" % i) for i in range(8)]
        sb0 = lambda n, s, d=F32: st0.enter_context(nc.sbuf_tensor(n, s, d))
        identf = sb0("identf", [128, 128])
        identb = sb0("identb", [128, 128], BF16)
        antif = sb0("antif", [128, 128])
        onesb = sb0("onesb", [128, 128])
        modT = sb0("modT", [128, 96])
        A1 = sb0("A1", [128, 16])
        gq2t = sb0("gq2t", [128, 1])
        gk2t = sb0("gk2t", [128, 1])
        neglam = sb0("neglam", [128, 1])
        rbb = sb0("rbb", [128, 64])
        lbt = sb0("lbt", [128, 4])
        omlt = sb0("omlt", [128, 4])
        gsubb = sb0("gsubb", [128, 128])
        ghgb = sb0("ghgb", [128, 128])
        mskf = sb0("mskf", [128, 128])
        mskb = sb0("mskb", [128, 128])
        t_const = Tok("const")
        cs = P.dsem("q_const")
        ld = lambda dst, src: P.op("sync", lambda e: e.dma_start(out=dst, in_=src), writes=[t_const], dma_sem=cs)
        ld(identf[:], ident[:, :])
        ld(antif[:], antiid[:, :])
        ld(onesb[:], onesblk[:, :])
        ld(gq2t[:], gq2[:, :])
        ld(gk2t[:], gk2[:, :])
        ld(rbb[:], rb_own.rearrange("b h -> (b h)").rearrange("(o n) -> o n", o=1).partition_broadcast(128))
        ld(gsubb[:], gsub[0:1, :].partition_broadcast(128))
        ld(ghgb[:], ghg[0:1, :].partition_broadcast(128))
        ld(mskf[:], mask_f[:, :])
        ld(mskb[:], mask_b[:, :])
        P.barrier()
        P.op("vector", lambda e: e.tensor_copy(out=identb[:], in_=identf[:]), writes=[t_const])

        if dbg == "H":
            P.op("vector", lambda e: e.memset(lbt[:], 0.5), writes=[t_const])
            P.op("vector", lambda e: e.memset(omlt[:], 0.5), writes=[t_const])
            t_hTd = [Tok("hTd%d" % i) for i in range(16)]
            build_hgrn(nc, P, ps, pst, w_own, hT_d, t_hTd, of_d, mix, lbt, omlt, ghgb, mskf, mskb, identb, dbg_n=DBG_N[0])
            P.barrier()
            P.emit()
            return nc
        with contextlib.ExitStack() as st:
            sb = lambda n, s, d=F32: st.enter_context(nc.sbuf_tensor(n, s, d))
            ct = sb("ct", [128, 16])
            sct = sb("sct", [128, 16])
            bat = sb("bat", [128, 96])
            gmt = sb("gmt", [128, 16])
            lamt = sb("lamt", [128, 256])
            lamp = sb("lamp", [128, 128])
            lams = sb("lams", [128, 2])
            lbl_t = sb("lbl_t", [128, 8])
            modS = sb("modS", [96, 128])
            t0 = Tok("p0")
            s0 = P.dsem("q_p0")
            ld0 = lambda dst, src: P.op("sync", lambda e: e.dma_start(out=dst, in_=src), writes=[t0], dma_sem=s0)
            ld0(ct[:], cT[:, :])
            ld0(bat[:], b_adaT[:, :])
            ld0(gmt[:], g_mixT[:, :])
            ld0(lamt[:], lamv[0:1, :].partition_broadcast(128))
            ld0(lbl_t[:], lbl[:, :])
            P.barrier()
            t_sct = Tok("sct")
            P.op("scalar", lambda e: e.activation(out=sct[:], in_=ct[:], func=AF.Silu), writes=[t_sct])
            t_lam = Tok("lam")
            lam_init = 0.8 - 0.6 * math.exp(0.0)
            P.op("vector", lambda e: e.tensor_tensor(out=lamp[:, 0:64], in0=lamt[:, 0:64], in1=lamt[:, 64:128], op=ALU.mult), writes=[t_lam])
            P.op("vector", lambda e: e.tensor_tensor(out=lamp[:, 64:128], in0=lamt[:, 128:192], in1=lamt[:, 192:256], op=ALU.mult), writes=[t_lam])
            P.op("vector", lambda e: e.tensor_reduce(out=lams[:], in_=lamp[:].rearrange("p (a b) -> p a b", a=2), axis=AX.X, op=ALU.add), writes=[t_lam])
            P.op("scalar", lambda e: e.activation(out=lams[:], in_=lams[:], func=AF.Exp), writes=[t_lam])
            P.op("vector", lambda e: e.tensor_tensor(out=neglam[:], in0=lams[:, 1:2], in1=lams[:, 0:1], op=ALU.subtract), writes=[t_lam])
            P.op("vector", lambda e: e.tensor_scalar(out=neglam[:], in0=neglam[:], scalar1=-lam_init, scalar2=None, op0=ALU.add), writes=[t_lam])
            t_lb = Tok("lb")
            lv = lbl_t[:].rearrange("p (d s h) -> p d s h", d=2, s=2)
            P.op("vector", lambda e: e.tensor_tensor(out=lbt[:].rearrange("p (d h) -> p d h", d=2), in0=lv[:, :, 0, :], in1=lv[:, :, 1, :], op=ALU.subtract), writes=[t_lb])
            P.op("scalar", lambda e: e.activation(out=lbt[:], in_=lbt[:], func=AF.Sigmoid), writes=[t_lb])
            P.op("vector", lambda e: e.tensor_scalar(out=omlt[:], in0=lbt[:], scalar1=-1.0, scalar2=1.0, op0=ALU.mult, op1=ALU.add), writes=[t_lb])

            wring = Ring(P, st, nc, "wa", 2, [128, 16, 512], F32)
            t_mod = pst[0]
            for cb in range(24):
                wt, wtok, wsem = wring.next()
                P.op("sync", lambda e, wt=wt, cb=cb: e.dma_start(out=wt[:], in_=w_ada[:, cb * 512:(cb + 1) * 512].rearrange("(k p) n -> p k n", p=128)),
                     writes=[wtok], dma_sem=wsem)
                for t in range(4):
                    col = cb * 4 + t
                    for k in range(16):
                        P.op("tensor", lambda e, wt=wt, t=t, k=k, col=col: e.matmul(
                            out=ps[0][:, col:col + 1], lhsT=wt[:, k, t * 128:(t + 1) * 128], rhs=sct[:, k:k + 1],
                            start=(k == 0), stop=(k == 15)), reads=[wtok, t_sct], writes=[t_mod])
            t_modT = Tok("modT")
            P.op("vector", lambda e: e.tensor_tensor(out=modT[:], in0=ps[0][:, 0:96], in1=bat[:], op=ALU.add), reads=[t_mod], writes=[t_modT])
            P.op("vector", lambda e: e.scalar_tensor_tensor(out=A1[:], in0=modT[:, 16:32], scalar=1.0, in1=gmt[:], op0=ALU.add, op1=ALU.mult),
                 reads=[t_modT], writes=[t_modT])
            P.op("tensor", lambda e: e.transpose(out=ps[1][0:96, 0:128], in_=modT[:, 0:96], identity=identf[:]), reads=[t_modT], writes=[pst[1]])
            t_modS = Tok("modS")
            P.op("vector", lambda e: e.tensor_copy(out=modS[:], in_=ps[1][0:96, 0:128]), reads=[pst[1]], writes=[t_modS])
            P.op("sync", lambda e: e.dma_start(out=modrow[:, :], in_=modS[:]), reads=[t_modS], dma_sem=P.dsem("q_modrow"))
            P.barrier()
        sh1 = modT[:, 0:16]

        t_hTd = [Tok("hTd%d" % i) for i in range(16)]
        with contextlib.ExitStack() as st:
            xring = Ring(P, st, nc, "xr", 3, [128, D], F32)
            xnring = Ring(P, st, nc, "xn", 2, [128, D], BF16)
            hbring = Ring(P, st, nc, "hb", 2, [128, 16, 512], BF16)
            junk = st.enter_context(nc.sbuf_tensor("junk", [128, D], BF16))
            ssr = st.enter_context(nc.sbuf_tensor("ssr", [128, 64], F32))
            t_junk = Tok("junk")
            t_ss = Tok("ss")
            for nb in range(16):
                hb, hbtok, hbsem = hbring.next()
                for tt in range(4):
                    i = nb * 4 + tt
                    xt, xtok, xsem = xring.next()
                    xn, xntok, _ = xnring.next()
                    P.op("sync", lambda e, xt=xt, i=i: e.dma_start(out=xt[:], in_=xb[i * 128:(i + 1) * 128, :]), writes=[xtok], dma_sem=xsem)
                    P.op("scalar", lambda e, xt=xt, i=i: e.activation(out=junk[:], in_=xt[:], func=AF.Square, accum_out=ssr[:, i:i + 1]),
                         reads=[xtok], writes=[t_junk, t_ss])
                    P.op("vector", lambda e, i=i: e.tensor_scalar(out=ssr[:, i:i + 1], in0=ssr[:, i:i + 1], scalar1=1.0 / D, scalar2=EPS, op0=ALU.mult, op1=ALU.add), writes=[t_ss])
                    P.op("scalar", lambda e, i=i: e.activation(out=ssr[:, i:i + 1], in_=ssr[:, i:i + 1], func=AF.Sqrt), writes=[t_ss])
                    P.op("vector", lambda e, i=i: e.reciprocal(out=ssr[:, i:i + 1], in_=ssr[:, i:i + 1]), writes=[t_ss])
                    P.op("vector", lambda e, xt=xt, xn=xn, i=i: e.tensor_scalar(out=xn[:], in0=xt[:], scalar1=ssr[:, i:i + 1], scalar2=None, op0=ALU.mult),
                         reads=[xtok, t_ss], writes=[xntok])
                    for half in range(2):
                        pb = 2 + half
                        for c8 in range(8):
                            c = half * 8 + c8
                            P.op("tensor", lambda e, xn=xn, c=c, c8=c8, pb=pb: e.transpose(
                                out=ps[pb][:].bitcast(BF16)[:, c8 * 128:(c8 + 1) * 128], in_=xn[:, c * 128:(c + 1) * 128], identity=identb[:]),
                                reads=[xntok], writes=[pst[pb]])
                        for c8 in range(8):
                            c = half * 8 + c8
                            src = lambda pb=pb, c8=c8: ps[pb][:].bitcast(BF16)[:, c8 * 128:(c8 + 1) * 128]
                            if c % 2 == 0:
                                P.op("vector", lambda e, hb=hb, c=c, tt=tt, src=src: e.tensor_scalar(
                                    out=hb[:, c, tt * 128:(tt + 1) * 128], in0=src(), scalar1=A1[:, c:c + 1], scalar2=modT[:, c:c + 1], op0=ALU.mult, op1=ALU.add),
                                    reads=[pst[pb]], writes=[hbtok])
                            else:
                                P.op("scalar", lambda e, hb=hb, c=c, tt=tt, src=src: e.activation(
                                    out=hb[:, c, tt * 128:(tt + 1) * 128], in_=src(), func=AF.Identity, scale=A1[:, c:c + 1], bias=modT[:, c:c + 1]),
                                    reads=[pst[pb]], writes=[hbtok])
                P.op("sync", lambda e, hb=hb, nb=nb: e.dma_start(out=hT_d[:, :, nb * 512:(nb + 1) * 512], in_=hb[:]), reads=[hbtok], writes=[t_hTd[nb]], dma_sem=hbsem)
            P.barrier()

        if dbg == "1a":
            P.emit()
            return nc
        with contextlib.ExitStack() as st:
            sb = lambda n, s, d=F32: st.enter_context(nc.sbuf_tensor(n, s, d))
            wA = sb("wA", [128, 16, 768], BF16)
            qaT = [sb("qaT%d" % h, [128, S], BF16) for h in range(2)]
            kaT = [sb("kaT%d" % h, [128, S], BF16) for h in range(2)]
            va = sb("va", [128, 64, 2, 130], BF16)
            EB = sb("EB", [128, 6, 2, 512], BF16)
            t_wA = Tok("wA")
            t_q = [Tok("q0"), Tok("q1")]
            t_k = [Tok("k0"), Tok("k1")]
            t_va = Tok("va")
            t_EB = Tok("EB")
            P.op("vector", lambda e: e.memset(va[:, :, :, 128:130], 1.0), writes=[t_va])
            with contextlib.ExitStack() as st2:
                wsr = Ring(P, st2, nc, "wsA", 2, [128, 16, 256], F32)
                for g in range(3):
                    wt, wtok, wsem = wsr.next()
                    P.op("sync", lambda e, wt=wt, g=g: e.dma_start(out=wt[:], in_=w_own[:, g * 256:(g + 1) * 256].rearrange("(k p) n -> p k n", p=128)),
                         writes=[wtok], dma_sem=wsem)
                    P.op("vector", lambda e, wt=wt, g=g: e.tensor_copy(out=wA[:, :, g * 256:(g + 1) * 256], in_=wt[:]), reads=[wtok], writes=[t_wA])
                ohs = st2.enter_context(nc.sbuf_tensor("ohs", [32, 1280], F32))
                rbs = st2.enter_context(nc.sbuf_tensor("rbs", [32, 2], F32))
                Gs = st2.enter_context(nc.sbuf_tensor("Gs", [2, 1280], F32))
                Hk = st2.enter_context(nc.sbuf_tensor("Hk", [128, 512], F32))
                t_oh = Tok("oh")
                s_oh = P.dsem("q_oh")
                P.op("sync", lambda e: e.dma_start(out=ohs[:], in_=ohg[:, :]), writes=[t_oh], dma_sem=s_oh)
                s_rbs = P.dsem("q_rbs")
                t_rbs = Tok("rbs")
                P.op("sync", lambda e: e.dma_start(out=rbs[:], in_=rb_own[:, :]), writes=[t_rbs], dma_sem=s_rbs)
                t_Gs = Tok("Gs")
                for q3 in range(3):
                    w_ = 512 if q3 < 2 else 256
                    P.op("tensor", lambda e, q3=q3, w_=w_: e.matmul(out=ps[0][0:2, 0:w_], lhsT=rbs[:, :], rhs=ohs[:, q3 * 512:q3 * 512 + w_], start=True, stop=True),
                         reads=[t_oh, t_rbs], writes=[pst[0]])
                    P.op("vector", lambda e, q3=q3, w_=w_: e.tensor_copy(out=Gs[:, q3 * 512:q3 * 512 + w_], in_=ps[0][0:2, 0:w_]), reads=[pst[0]], writes=[t_Gs])
                t_Gd = Tok("Gd")
                s_G = P.dsem("q_G")
                P.op("sync", lambda e: e.dma_start(out=G_d[:, :], in_=Gs[:]), reads=[t_Gs], writes=[t_Gd], dma_sem=s_G)
                t_Hk = Tok("Hk")
                s_Hk = P.dsem("q_Hk")
                for oi in range(6):
                    base = 512 - (oi - 1) * 128
                    for hh in range(2):
                        src = bass.AP(tensor=G_d.tensor, offset=G_d[hh:hh + 1, base:base + 1].offset, ap=[[1, 128], [1, 512]])
                        P.op("sync", lambda e, src=src: e.dma_start(out=Hk[:], in_=src), reads=[t_Gd], writes=[t_Hk], dma_sem=s_Hk)
                        P.op("tensor", lambda e: e.matmul(out=ps[0][:, :], lhsT=antif[:], rhs=Hk[:], start=True, stop=True), reads=[t_Hk], writes=[pst[0]])
                        P.op("scalar", lambda e, oi=oi, hh=hh: e.activation(out=EB[:, oi, hh, :], in_=ps[0][:, :], func=AF.Exp), reads=[pst[0]], writes=[t_EB])
                P.barrier()
            hbring = Ring(P, st, nc, "hbA", 2, [128, 16, 512], BF16)
            sq = sb("sq", [128, 512])
            rs = sb("rs", [128, 512])
            t_sq, t_rs = Tok("sq"), Tok("rs")
            for nb in range(16):
                hb, hbtok, hbsem = hbring.next()
                P.op("sync", lambda e, hb=hb, nb=nb: e.dma_start(out=hb[:], in_=hT_d[:, :, nb * 512:(nb + 1) * 512]), reads=[t_hTd[nb]], writes=[hbtok], dma_sem=hbsem)
                for ci in range(4):
                    isk, hh = ci // 2, ci % 2
                    pb = ci % 2
                    for k in range(16):
                        P.op("tensor", lambda e, hb=hb, ci=ci, k=k, pb=pb: e.matmul(out=ps[pb][:, :], lhsT=wA[:, k, ci * 128:(ci + 1) * 128], rhs=hb[:, k, :],
                                                                                  start=(k == 0), stop=(k == 15)), reads=[t_wA, hbtok], writes=[pst[pb]])
                    P.op("scalar", lambda e, pb=pb: e.activation(out=sq[:], in_=ps[pb][:, :], func=AF.Square), reads=[pst[pb]], writes=[t_sq])
                    P.op("tensor", lambda e: e.matmul(out=ps[2][:, :], lhsT=onesb[:], rhs=sq[:], start=True, stop=True), reads=[t_sq], writes=[pst[2]])
                    P.op("scalar", lambda e: e.activation(out=rs[:], in_=ps[2][:, :], func=AF.Sqrt, bias=EPS, scale=1.0), reads=[pst[2]], writes=[t_rs])
                    P.op("vector", lambda e: e.reciprocal(out=rs[:], in_=rs[:]), writes=[t_rs])
                    dst = (kaT if isk else qaT)[hh]
                    dtok = (t_k if isk else t_q)[hh]
                    gsc = gk2t if isk else gq2t
                    P.op("vector", lambda e, dst=dst, pb=pb, gsc=gsc, nb=nb: e.scalar_tensor_tensor(
                        out=dst[:, nb * 512:(nb + 1) * 512], in0=ps[pb][:, :], scalar=gsc[:, 0:1], in1=rs[:], op0=ALU.mult, op1=ALU.mult),
                        reads=[pst[pb], t_rs], writes=[dtok])
                for tt in range(4):
                    pb = 3 + (tt % 2)
                    for k in range(16):
                        P.op("tensor", lambda e, hb=hb, tt=tt, k=k, pb=pb: e.matmul(out=ps[pb][:, 0:256], lhsT=hb[:, k, tt * 128:(tt + 1) * 128], rhs=wA[:, k, 512:768],
                                                                                  start=(k == 0), stop=(k == 15)), reads=[t_wA, hbtok], writes=[pst[pb]])
                    P.op("scalar", lambda e, tt=tt, pb=pb, nb=nb: e.activation(out=va[:, nb * 4 + tt, :, 0:128], in_=ps[pb][:, 0:256].rearrange("p (h d) -> p h d", h=2), func=AF.Copy),
                         reads=[pst[pb]], writes=[t_va])
            ptring = Ring(P, st, nc, "pt", 3, [128, 512], BF16)
            osb = sb("osb", [128, 2, 130])
            o1 = sb("o1", [128, 128])
            o2 = sb("o2", [128, 128])
            rec = sb("rec", [128, 4])
            mst = Ring(P, st, nc, "mst", 2, [128, 4, 128], BF16)
            t_fin = Tok("fin")
            junk2 = sb("junk2", [128, 128])
            for hh in range(2):
                farb = [rbb[:, 15 * 2 + hh:15 * 2 + hh + 1], rbb[:, 31 * 2 + hh:31 * 2 + hh + 1]]
                for qb in range(16):
                    for kt in range(64):
                        oi = kt - 4 * qb + 1
                        near = 0 <= oi < 6
                        for m in range(2):
                            sbk = 2 + ((kt * 2 + m) % 2)
                            P.op("tensor", lambda e, hh=hh, qb=qb, kt=kt, m=m, sbk=sbk: e.matmul(
                                out=ps[sbk][:, :], lhsT=kaT[hh][m * 64:(m + 1) * 64, kt * 128:(kt + 1) * 128],
                                rhs=qaT[hh][m * 64:(m + 1) * 64, qb * 512:(qb + 1) * 512], start=True, stop=True),
                                reads=[t_k[hh], t_q[hh]], writes=[pst[sbk]])
                            pt, pttok, _ = ptring.next()
                            if near:
                                P.op("scalar", lambda e, pt=pt, sbk=sbk: e.activation(out=pt[:], in_=ps[sbk][:, :], func=AF.Exp, scale=0.125), reads=[pst[sbk]], writes=[pttok])
                                P.op("vector", lambda e, pt=pt, oi=oi, hh=hh: e.tensor_tensor(out=pt[:], in0=pt[:], in1=EB[:, oi, hh, :], op=ALU.mult), reads=[t_EB], writes=[pttok])
                            else:
                                fb = farb[0] if oi < 0 else farb[1]
                                P.op("scalar", lambda e, pt=pt, sbk=sbk, fb=fb: e.activation(out=pt[:], in_=ps[sbk][:, :], func=AF.Exp, scale=0.125, bias=fb), reads=[pst[sbk]], writes=[pttok])
                            for s4 in range(4):
                                ob = 4 + 2 * m + s4 // 2
                                oc = (s4 % 2) * 256
                                P.op("tensor", lambda e, pt=pt, s4=s4, ob=ob, oc=oc, kt=kt, hh=hh: e.matmul(
                                    out=ps[ob][:, oc:oc + 130], lhsT=pt[:, s4 * 128:(s4 + 1) * 128], rhs=va[:, kt, hh, :],
                                    start=(kt == 0 and s4 % 2 == 0), stop=(kt == 63), skip_group_check=True),
                                    reads=[pttok, t_va], writes=[pst[ob]])
                    ms, mstok, mssem = mst.next()
                    for s4 in range(4):
                        oc = (s4 % 2) * 256
                        b1 = 4 + s4 // 2
                        b2 = 6 + s4 // 2
                        P.op("vector", lambda e, b1=b1, oc=oc: e.reciprocal(out=rec[:, 0:1], in_=ps[b1][:, oc + 128:oc + 129]), reads=[pst[b1]], writes=[t_fin])
                        P.op("vector", lambda e, b2=b2, oc=oc: e.reciprocal(out=rec[:, 1:2], in_=ps[b2][:, oc + 128:oc + 129]), reads=[pst[b2]], writes=[t_fin])
                        P.op("vector", lambda e: e.tensor_tensor(out=rec[:, 1:2], in0=rec[:, 1:2], in1=neglam[:], op=ALU.mult), writes=[t_fin])
                        P.op("vector", lambda e, b1=b1, oc=oc: e.tensor_scalar(out=o1[:], in0=ps[b1][:, oc:oc + 128], scalar1=rec[:, 0:1], scalar2=None, op0=ALU.mult), reads=[pst[b1]], writes=[t_fin])
                        P.op("vector", lambda e, b2=b2, oc=oc: e.scalar_tensor_tensor(out=o1[:], in0=ps[b2][:, oc:oc + 128], scalar=rec[:, 1:2], in1=o1[:], op0=ALU.mult, op1=ALU.add),
                             reads=[pst[b2]], writes=[t_fin])
                        P.op("scalar", lambda e: e.activation(out=junk2[:], in_=o1[:], func=AF.Square, accum_out=rec[:, 2:3]), writes=[t_fin])
                        P.op("vector", lambda e: e.tensor_scalar(out=rec[:, 2:3], in0=rec[:, 2:3], scalar1=1.0 / 128, scalar2=EPS, op0=ALU.mult, op1=ALU.add), writes=[t_fin])
                        P.op("scalar", lambda e: e.activation(out=rec[:, 2:3], in_=rec[:, 2:3], func=AF.Sqrt), writes=[t_fin])
                        P.op("vector", lambda e: e.reciprocal(out=rec[:, 3:4], in_=rec[:, 2:3]), writes=[t_fin])
                        P.op("vector", lambda e: e.tensor_scalar(out=o1[:], in0=o1[:], scalar1=rec[:, 3:4], scalar2=1.0 - lam_init, op0=ALU.mult, op1=ALU.mult), writes=[t_fin])
                        P.op("vector", lambda e, ms=ms, s4=s4: e.tensor_tensor(out=ms[:, s4, :], in0=o1[:], in1=gsubb[:], op=ALU.mult), reads=[t_fin], writes=[mstok])
                    P.op("sync", lambda e, ms=ms, qb=qb, hh=hh: e.dma_start(
                        out=mix[qb * 512:(qb + 1) * 512, hh * 128:(hh + 1) * 128].rearrange("(s p) d -> p s d", p=128), in_=ms[:]),
                        reads=[mstok], dma_sem=mssem)
            P.barrier()

        if dbg == "A":
            P.emit()
            return nc
        build_hgrn(nc, P, ps, pst, w_own, hT_d, t_hTd, of_d, mix, lbt, omlt, ghgb, mskf, mskb, identb)
        P.barrier()
        P.emit()
    return nc


EPS_AP = [None]


DBG_N = [0]


def build_hgrn(nc, P, ps, pst, w_own, hT_d, t_hTd, of_d, mix, lbt, omlt, ghgb, mskf, mskb, identb, dbg_n=0):
    with contextlib.ExitStack() as st:
        sb = lambda n, s, d=F32: st.enter_context(nc.sbuf_tensor(n, s, d))
        wH = sb("wH", [128, 16, 1280], BF16)
        t_wH = Tok("wH")
        with contextlib.ExitStack() as st2:
            wsr = Ring(P, st2, nc, "wsH", 2, [128, 16, 256], F32)
            for g in range(5):
                wt, wtok, wsem = wsr.next()
                P.op("sync", lambda e, wt=wt, g=g: e.dma_start(out=wt[:], in_=w_own[:, (3 + g) * 256:(4 + g) * 256].rearrange("(k p) n -> p k n", p=128)),
                     writes=[wtok], dma_sem=wsem)
                for hh in range(2):
                    P.op("vector", lambda e, wt=wt, g=g, hh=hh: e.tensor_copy(out=wH[:, :, hh * 640 + g * 128:hh * 640 + (g + 1) * 128], in_=wt[:, :, hh * 128:(hh + 1) * 128]),
                         reads=[wtok], writes=[t_wH])
            P.barrier()
        hbring = Ring(P, st, nc, "hbH", 2, [128, 16, 512], BF16)
        rmask = sb("rmask", [128, 512])
        cmlo = sb("cmlo", [128, 512])
        cmhi = sb("cmhi", [128, 512])
        t_rm = Tok("rm")
        P.op("vector", lambda e: e.memset(rmask[:], 1.0), writes=[t_rm])
        P.op("vector", lambda e: e.memset(rmask[:].rearrange("p (c t) -> p c t", t=64)[:, :, 0:1], 0.0), writes=[t_rm])
        P.op("vector", lambda e: e.memset(cmlo[:], 0.0), writes=[t_rm])
        P.op("vector", lambda e: e.memset(cmhi[:], 0.0), writes=[t_rm])
        P.op("vector", lambda e: e.memset(cmlo[:].rearrange("p (c t) -> p c t", t=128)[:, :, 0:64], 1.0), writes=[t_rm])
        P.op("vector", lambda e: e.memset(cmhi[:].rearrange("p (c t) -> p c t", t=128)[:, :, 64:128], 1.0), writes=[t_rm])
        H2 = range(2)
        Sf = [sb("Sf%d" % h, [128, 128]) for h in H2]
        Sb_ = [sb("Sb%d" % h, [128, 128], BF16) for h in H2]
        tmpS = [sb("tmpS%d" % h, [128, 128]) for h in H2]
        qs = [sb("qs%d" % h, [128, 512]) for h in H2]
        ff = [sb("ff%d" % h, [128, 512]) for h in H2]
        lg = [sb("lg%d" % h, [128, 512]) for h in H2]
        bb = [sb("bb%d" % h, [128, 512]) for h in H2]
        eb = [sb("eb%d" % h, [128, 512]) for h in H2]
        enb = [sb("enb%d" % h, [128, 512]) for h in H2]
        qfb = [sb("qfb%d" % h, [128, 512], BF16) for h in H2]
        qlo = [sb("qlo%d" % h, [128, 512], BF16) for h in H2]
        qhi = [sb("qhi%d" % h, [128, 512], BF16) for h in H2]
        kt_ = [sb("kt%d" % h, [128, 512], BF16) for h in H2]
        vt = [sb("vt%d" % h, [128, 4, 128], BF16) for h in H2]
        vlo = [sb("vlo%d" % h, [128, 4, 128], BF16) for h in H2]
        vhi = [sb("vhi%d" % h, [128, 4, 128], BF16) for h in H2]
        sg = [sb("sg%d" % h, [128, 4, 128]) for h in H2]
        kh2 = [sb("kh2%d" % h, [128, 128], BF16) for h in H2]
        scm = [sb("scm%d" % h, [128, 128], BF16) for h in H2]
        ob = [sb("ob%d" % h, [128, 4, 128]) for h in H2]
        ofl = [sb("ofl%d" % h, [128, 4, 128]) for h in H2]
        obb = [sb("obb%d" % h, [128, 4, 128], BF16) for h in H2]
        stat = [sb("stat%d" % h, [128, 4]) for h in H2]
        junk = [sb("junkh%d" % h, [128, 4, 128]) for h in H2]
        t_S = [Tok("S") for h in H2]
        tE = [Tok("tE") for h in H2]
        t_v = [Tok("v") for h in H2]
        t_sg = [Tok("sg") for h in H2]
        t_kh = [Tok("kh") for h in H2]
        t_scm = [Tok("scm") for h in H2]
        t_ob = [Tok("ob") for h in H2]
        t_ofl = [Tok("ofl") for h in H2]
        s_ob = [P.dsem("q_ob%d" % h) for h in H2]
        s_ofl = [P.dsem("q_ofl%d" % h) for h in H2]
        t_ofd = [[Tok("ofd") for _ in range(16)] for _ in H2]
        for hh in H2:
            P.op("vector", lambda e, hh=hh: e.memset(vlo[hh][:], 0.0), writes=[t_v[hh]])
            P.op("vector", lambda e, hh=hh: e.memset(vhi[hh][:], 0.0), writes=[t_v[hh]])
        for d in range(2):
            if dbg_n and d == 1:
                break
            for hh in H2:
                P.op("vector", lambda e, hh=hh: e.memset(Sf[hh][:], 0.0), writes=[t_S[hh]])
                P.op("vector", lambda e, hh=hh: e.memset(Sb_[hh][:], 0.0), writes=[t_S[hh]])
            blocks = range(16) if d == 0 else range(15, -1, -1)
            if dbg_n:
                blocks = range(1)
            mk = mskf if d == 0 else mskb
            for nb in blocks:
                hb, hbtok, hbsem = hbring.next()
                P.op("sync", lambda e, hb=hb, nb=nb: e.dma_start(out=hb[:], in_=hT_d[:, :, nb * 512:(nb + 1) * 512]), reads=[t_hTd[nb]], writes=[hbtok], dma_sem=hbsem)
                for hh in H2:
                    base = hh * 640
                    zc = base + (2 + d) * 128
                    li = d * 2 + hh
                    for k in range(16):
                        P.op("tensor", lambda e, hb=hb, k=k, base=base: e.matmul(out=ps[0][:, :], lhsT=wH[:, k, base:base + 128], rhs=hb[:, k, :], start=(k == 0), stop=(k == 15)),
                             reads=[t_wH, hbtok], writes=[pst[0]])
                    P.op("scalar", lambda e, hh=hh: e.activation(out=qs[hh][:], in_=ps[0][:, :], func=AF.Silu), reads=[pst[0]], writes=[tE[hh]])
                    for k in range(16):
                        P.op("tensor", lambda e, hb=hb, k=k, zc=zc: e.matmul(out=ps[1][:, :], lhsT=wH[:, k, zc:zc + 128], rhs=hb[:, k, :], start=(k == 0), stop=(k == 15)),
                             reads=[t_wH, hbtok], writes=[pst[1]])
                    P.op("scalar", lambda e, hh=hh: e.activation(out=ff[hh][:], in_=ps[1][:, :], func=AF.Sigmoid), reads=[pst[1]], writes=[tE[hh]])
                    P.op("vector", lambda e, hh=hh, li=li: e.tensor_scalar(out=ff[hh][:], in0=ff[hh][:], scalar1=omlt[:, li:li + 1], scalar2=lbt[:, li:li + 1], op0=ALU.mult, op1=ALU.add), writes=[tE[hh]])
                    P.op("scalar", lambda e, hh=hh: e.activation(out=lg[hh][:], in_=ff[hh][:], func=AF.Ln), writes=[tE[hh]])
                    P.op("vector", lambda e, hh=hh: e.tensor_scalar(out=ff[hh][:], in0=ff[hh][:], scalar1=-1.0, scalar2=1.0, op0=ALU.mult, op1=ALU.add), writes=[tE[hh]])
                    P.op("vector", lambda e, hh=hh: e.tensor_tensor_scan(out=bb[hh][:], data0=rmask[:], data1=lg[hh][:], initial=0.0, op0=ALU.mult, op1=ALU.add), reads=[t_rm], writes=[tE[hh]])
                    if d == 1:
                        b3 = bb[hh][:].rearrange("p (c t) -> p c t", t=64)
                        l3 = lg[hh][:].rearrange("p (c t) -> p c t", t=64)
                        P.op("vector", lambda e, hh=hh: e.tensor_tensor(out=lg[hh][:], in0=lg[hh][:], in1=bb[hh][:], op=ALU.subtract), writes=[tE[hh]])
                        P.op("vector", lambda e, hh=hh: e.tensor_copy(out=stat[hh][:, 0:4], in_=bb[hh][:].rearrange("p (c t) -> p c t", t=128)[:, :, 63]), writes=[tE[hh]])
                        P.op("vector", lambda e, hh=hh: e.tensor_copy(out=junk[hh][:, 0, 0:4], in_=bb[hh][:].rearrange("p (c t) -> p c t", t=128)[:, :, 127]), writes=[tE[hh]])
                        for c in range(8):
                            tot = stat[hh][:, c // 2:c // 2 + 1] if c % 2 == 0 else junk[hh][:, 0, c // 2:c // 2 + 1]
                            P.op("vector", lambda e, hh=hh, c=c, tot=tot: e.tensor_scalar(out=bb[hh][:, c * 64:(c + 1) * 64], in0=lg[hh][:, c * 64:(c + 1) * 64], scalar1=tot, scalar2=None, op0=ALU.add), writes=[tE[hh]])
                    P.op("scalar", lambda e, hh=hh: e.activation(out=eb[hh][:], in_=bb[hh][:], func=AF.Exp), writes=[tE[hh]])
                    P.op("scalar", lambda e, hh=hh: e.activation(out=enb[hh][:], in_=bb[hh][:], func=AF.Exp, scale=-1.0), writes=[tE[hh]])
                    P.op("vector", lambda e, hh=hh: e.tensor_tensor(out=qfb[hh][:], in0=qs[hh][:], in1=eb[hh][:], op=ALU.mult), writes=[tE[hh]])
                    P.op("vector", lambda e, hh=hh: e.tensor_tensor(out=qlo[hh][:], in0=qfb[hh][:], in1=cmlo[:], op=ALU.mult), writes=[tE[hh]])
                    P.op("vector", lambda e, hh=hh: e.tensor_tensor(out=qhi[hh][:], in0=qfb[hh][:], in1=cmhi[:], op=ALU.mult), writes=[tE[hh]])
                    P.op("vector", lambda e, hh=hh: e.tensor_tensor(out=kt_[hh][:], in0=ff[hh][:], in1=enb[hh][:], op=ALU.mult), writes=[tE[hh]])
                    for which in range(2 if d == 1 else 1):
                        cc = base + (1 if which == 0 else 4) * 128
                        pb = which
                        for tt in range(4):
                            for k in range(16):
                                P.op("tensor", lambda e, hb=hb, k=k, tt=tt, cc=cc, pb=pb: e.matmul(
                                    out=ps[pb][:, tt * 128:(tt + 1) * 128], lhsT=hb[:, k, tt * 128:(tt + 1) * 128], rhs=wH[:, k, cc:cc + 128],
                                    start=(k == 0), stop=(k == 15)), reads=[t_wH, hbtok], writes=[pst[pb]])
                        if which == 0:
                            src = lambda lo, hi: ps[0][lo:hi, :].rearrange("p (c d) -> p c d", c=4)
                            P.op("vector", lambda e, hh=hh, src=src: e.tensor_copy(out=vt[hh][:], in_=src(0, 128)), reads=[pst[0]], writes=[t_v[hh]])
                            P.op("vector", lambda e, hh=hh, src=src: e.tensor_copy(out=vlo[hh][0:64], in_=src(0, 64)), reads=[pst[0]], writes=[t_v[hh]])
                            P.op("scalar", lambda e, hh=hh, src=src: e.activation(out=vhi[hh][64:128], in_=src(64, 128), func=AF.Copy), reads=[pst[0]], writes=[t_v[hh]])
                        else:
                            P.op("scalar", lambda e, hh=hh: e.activation(out=sg[hh][:], in_=ps[1][:, :].rearrange("p (c d) -> p c d", c=4), func=AF.Silu),
                                 reads=[pst[1]], writes=[t_sg[hh]])
                    if d == 1:
                        P.op("sync", lambda e, hh=hh, nb=nb: e.dma_start(out=ofl[hh][:], in_=of_d[hh, nb * 512:(nb + 1) * 512, :].rearrange("(c p) d -> p c d", p=128)),
                             reads=[t_ofd[hh][nb]], writes=[t_ofl[hh]], dma_sem=s_ofl[hh])
                    pK, pO, pU = 2 + 3 * hh, 3 + 3 * hh, 4 + 3 * hh
                    tiles = range(4) if d == 0 else range(3, -1, -1)
                    for tt in tiles:
                        tsl = slice(tt * 128, (tt + 1) * 128)
                        P.op("tensor", lambda e, hh=hh, tsl=tsl, pK=pK: e.transpose(out=ps[pK][:].bitcast(BF16)[:, 0:128], in_=kt_[hh][:, tsl], identity=identb[:]),
                             reads=[tE[hh]], writes=[pst[pK]])
                        P.op("tensor", lambda e, hh=hh, tsl=tsl, pK=pK: e.matmul(out=ps[pK][:, 256:384], lhsT=kt_[hh][:, tsl], rhs=qfb[hh][:, tsl], start=True, stop=True, skip_group_check=True),
                             reads=[tE[hh]], writes=[pst[pK]])
                        P.op("scalar", lambda e, hh=hh, pK=pK: e.activation(out=kh2[hh][:], in_=ps[pK][:].bitcast(BF16)[:, 0:128], func=AF.Copy), reads=[pst[pK]], writes=[t_kh[hh]])
                        P.op("vector", lambda e, hh=hh, pK=pK, mk=mk: e.tensor_tensor(out=scm[hh][:], in0=ps[pK][:, 256:384], in1=mk[:], op=ALU.mult), reads=[pst[pK]], writes=[t_scm[hh]])
                        order = [(qlo, vlo, 2 * tt), (qhi, vhi, 2 * tt + 1)]
                        if d == 1:
                            order = order[::-1]
                        for oi, (qX, vX, c) in enumerate(order):
                            lastcol = c * 64 + (63 if d == 0 else 0)
                            P.op("tensor", lambda e, hh=hh, tsl=tsl, pO=pO, qX=qX, oi=oi: e.matmul(out=ps[pO][:, 0:128], lhsT=qX[hh][:, tsl], rhs=Sb_[hh][:], start=(oi == 0), stop=False, skip_group_check=True),
                                 reads=[tE[hh], t_S[hh]], writes=[pst[pO]])
                            if oi == 1:
                                P.op("tensor", lambda e, hh=hh, tt=tt, pO=pO: e.matmul(out=ps[pO][:, 0:128], lhsT=scm[hh][:], rhs=vt[hh][:, tt, :], start=False, stop=True, skip_group_check=True),
                                     reads=[t_scm[hh], t_v[hh]], writes=[pst[pO]])
                            P.op("tensor", lambda e, hh=hh, tt=tt, pU=pU, vX=vX: e.matmul(out=ps[pU][:, 0:128], lhsT=kh2[hh][:], rhs=vX[hh][:, tt, :], start=True, stop=True),
                                 reads=[t_kh[hh], t_v[hh]], writes=[pst[pU]])
                            P.op("vector", lambda e, hh=hh, pU=pU: e.tensor_tensor(out=tmpS[hh][:], in0=ps[pU][:, 0:128], in1=Sf[hh][:], op=ALU.add), reads=[pst[pU], t_S[hh]], writes=[t_S[hh]])
                            P.op("vector", lambda e, hh=hh, lastcol=lastcol: e.tensor_scalar(out=Sf[hh][:], in0=tmpS[hh][:], scalar1=eb[hh][:, lastcol:lastcol + 1], scalar2=None, op0=ALU.mult),
                                 reads=[tE[hh]], writes=[t_S[hh]])
                            P.op("vector", lambda e, hh=hh: e.tensor_copy(out=Sb_[hh][:], in_=Sf[hh][:]), writes=[t_S[hh]])
                        P.op("scalar", lambda e, hh=hh, tt=tt, pO=pO: e.activation(out=ob[hh][:, tt, :], in_=ps[pO][:, 0:128], func=AF.Copy), reads=[pst[pO]], writes=[t_ob[hh]])
                    if d == 0:
                        P.op("sync", lambda e, hh=hh, nb=nb: e.dma_start(out=of_d[hh, nb * 512:(nb + 1) * 512, :].rearrange("(c p) d -> p c d", p=128), in_=ob[hh][:]),
                             reads=[t_ob[hh]], writes=[t_ofd[hh][nb]], dma_sem=s_ob[hh])
                    else:
                        P.op("vector", lambda e, hh=hh: e.tensor_tensor(out=ob[hh][:], in0=ob[hh][:], in1=ofl[hh][:], op=ALU.add), reads=[t_ofl[hh]], writes=[t_ob[hh]])
                        P.op("vector", lambda e, hh=hh: e.tensor_tensor(out=junk[hh][:], in0=ob[hh][:], in1=ob[hh][:], op=ALU.mult), reads=[t_ob[hh], tE[hh]], writes=[t_sg[hh]])
                        P.op("vector", lambda e, hh=hh: e.tensor_reduce(out=stat[hh][:], in_=junk[hh][:], axis=AX.X, op=ALU.add), reads=[tE[hh]], writes=[t_sg[hh]])
                        P.op("vector", lambda e, hh=hh: e.tensor_scalar(out=stat[hh][:], in0=stat[hh][:], scalar1=1.0 / 128, scalar2=EPS, op0=ALU.mult, op1=ALU.add), writes=[t_sg[hh]])
                        P.op("scalar", lambda e, hh=hh: e.activation(out=stat[hh][:], in_=stat[hh][:], func=AF.Sqrt), writes=[t_sg[hh]])
                        P.op("vector", lambda e, hh=hh: e.reciprocal(out=stat[hh][:], in_=stat[hh][:]), writes=[t_sg[hh]])
                        for tt in range(4):
                            P.op("vector", lambda e, hh=hh, tt=tt: e.scalar_tensor_tensor(out=ob[hh][:, tt, :], in0=ob[hh][:, tt, :], scalar=stat[hh][:, tt:tt + 1], in1=sg[hh][:, tt, :], op0=ALU.mult, op1=ALU.mult),
                                 reads=[t_sg[hh]], writes=[t_ob[hh]])
                            P.op("vector", lambda e, hh=hh, tt=tt: e.tensor_tensor(out=obb[hh][:, tt, :], in0=ob[hh][:, tt, :], in1=ghgb[:], op=ALU.mult), writes=[t_ob[hh]])
                        P.op("sync", lambda e, hh=hh, nb=nb: e.dma_start(out=mix[nb * 512:(nb + 1) * 512, 256 + hh * 128:256 + (hh + 1) * 128].rearrange("(c p) d -> p c d", p=128), in_=obb[hh][:]),
                             reads=[t_ob[hh]], writes=[tE[hh]], dma_sem=s_ob[hh])
        P.barrier()


def l1_inputs(inputs, core):
    b, j = core // 4, core % 4
    f = lambda a: np.ascontiguousarray(a, dtype=np.float32)
    w_in = inputs["w_in"][0]
    offs = np.cumsum([0, 1024, 1024, 1024, 1024, 1024, 1024, 1024])
    cols = []
    for g in range(8):
        cols.append(w_in[:, offs[g] + j * 256: offs[g] + (j + 1) * 256])
    w_own = np.concatenate(cols, axis=1)
    lb = inputs["hgrn_lb_logits"]
    lbl = np.zeros((128, 2, 2, 2), np.float32)
    for d in range(2):
        for s_ in range(2):
            for hh in range(2):
                lbl[:, d, s_, hh] = lb[d, s_, (2 * j + hh) * 128:(2 * j + hh + 1) * 128]
    m = {
        "xb": f(inputs["x"][b]),
        "cT": f(inputs["c"][b].reshape(16, 128).T),
        "w_ada": f(inputs["w_ada"][0]),
        "b_adaT": f(inputs["b_ada"][0].reshape(96, 128).T),
        "g_mixT": f(inputs["g_mix"][0].reshape(16, 128).T),
        "w_own": f(w_own),
        "gq2": f(np.tile(inputs["g_q"][0], 2).reshape(128, 1)),
        "gk2": f(np.tile(inputs["g_k"][0], 2).reshape(128, 1)),
        "lamv": f(np.concatenate([inputs["lam_q1"][0], inputs["lam_k1"][0], inputs["lam_q2"][0], inputs["lam_k2"][0]]).reshape(1, 256)),
        "rb_own": f(inputs["rel_bias"][:, 2 * j:2 * j + 2]),
        "lbl": f(lbl.reshape(128, 8)),
        "gsub": f(inputs["g_sub"][0].reshape(1, 128)),
        "ghg": f(inputs["g_hgrn"][0].reshape(1, 128)),
    }
    m.update(_consts())
    return m


BIG = 1.0e4
CAPC = 192
ESLOTS = 8 * CAPC


def build_l2a():
    nc = bass.Bass("TRN2", target_bir_lowering=False)
    din = lambda n, s, d=F32: nc.dram_tensor(n, s, d, kind="ExternalInput").ap()
    xo = din("xo", [2048, D])
    cat = din("cat", [2048, D], BF16)
    w_out = din("w_out", [D, D])
    rows = din("rows", [6, D])
    w_router = din("w_router", [D, NE])
    rbias = din("rbias", [1, NE])
    wsg = din("wsg", [D, FF])
    wsu = din("wsu", [D, FF])
    wsd = din("wsd", [FF, D])
    ident = din("ident", [128, 128])
    tris = din("tris", [128, 128])
    iotae = din("iotae", [1, NE])
    srcoff = din("srcoff", [128, 1])
    destk_o = nc.dram_tensor("destk", [2048, 8], F32, kind="ExternalOutput").ap()
    gk_o = nc.dram_tensor("gk", [2048, 8], F32, kind="ExternalOutput").ap()
    x1s_o = nc.dram_tensor("x1s", [2048, D], F32, kind="ExternalOutput").ap()
    h2_o = nc.dram_tensor("h2", [2048, D], BF16, kind="ExternalOutput").ap()
    gates_o = nc.dram_tensor("gates", [2048, NE], F32, kind="ExternalOutput").ap()
    with contextlib.ExitStack() as st:
        P = Prog(nc, st)
        ps = [st.enter_context(nc.psum_tensor("ps%d" % i, [128, 512], F32)) for i in range(8)]
        pst = [Tok("ps%d" % i) for i in range(8)]
        sb = lambda n, s, d=F32: st.enter_context(nc.sbuf_tensor(n, s, d))
        identf = sb("identf", [128, 128])
        identb = sb("identb", [128, 128], BF16)
        gt1b = sb("gt1b", [128, D], BF16)
        A2b = sb("A2b", [128, D])
        sh2b = sb("sh2b", [128, D])
        gt2b = sb("gt2b", [128, D], BF16)
        rbb = sb("rbb", [128, NE])
        iob = sb("iob", [128, NE])
        trib = sb("trib", [128, 128], BF16)
        onesb_ = sb("onesb_", [128, 128], BF16)
        selsum = sb("selsum", [128, NE], BF16)
        selb = sb("selb", [128, NE], BF16)
        srct = sb("srct", [128, 1])
        idx8 = sb("idx8", [128, 8], U32)
        idxf = sb("idxf", [128, 8])
        posd = sb("posd", [128, NE])
        jk = sb("jk", [128, NE])
        dk_ = sb("dk_", [128, 16, 8])
        gk_ = sb("gk_", [128, 16, 8])
        pk_ = sb("pk_", [128, 8])
        wo = sb("wo", [128, 16, D], BF16)
        wr = sb("wr", [128, 16, NE])
        wgb = sb("wgb", [128, 16, FF], BF16)
        wub = sb("wub", [128, 16, FF], BF16)
        wdb = sb("wdb", [128, 4, D], BF16)
        t_c = Tok("c")
        cs = P.dsem("q_c")
        ld = lambda dst, src: P.op("sync", lambda e: e.dma_start(out=dst, in_=src), writes=[t_c], dma_sem=cs)
        ld(identf[:], ident[:, :])
        ld(A2b[:], rows[1:2, :].partition_broadcast(128))
        ld(sh2b[:], rows[2:3, :].partition_broadcast(128))
        ld(rbb[:], rbias[0:1, :].partition_broadcast(128))
        ld(iob[:], iotae[0:1, :].partition_broadcast(128))
        ld(srct[:], srcoff[:, :])
        ld(wr[:], w_router.rearrange("(k p) n -> p k n", p=128))
        t_wo = Tok("wo")
        with contextlib.ExitStack() as st2:
            st3 = contextlib.ExitStack()
            gfb = st3.enter_context(nc.sbuf_tensor("gfb", [128, D], F32))
            gtmp = st3.enter_context(nc.sbuf_tensor("gtmp", [128, 2, D], F32))
            tristg = st3.enter_context(nc.sbuf_tensor("tristg", [128, 128], F32))
            ld(tristg[:], tris[:, :])
            ld(gfb[:], rows[4:5, :].partition_broadcast(128))
            ld(gtmp[:, 0, :], rows[0:1, :].partition_broadcast(128))
            ld(gtmp[:, 1, :], rows[3:4, :].partition_broadcast(128))
            P.barrier()
            P.op("vector", lambda e: e.tensor_copy(out=trib[:], in_=tristg[:]), writes=[t_c])
            P.op("vector", lambda e: e.memset(onesb_[:], 1.0), writes=[t_c])
            P.op("vector", lambda e: e.memset(selsum[:], 0.0), writes=[t_c])
            P.barrier()
            ld(gtmp[:, 0, :], rows[0:1, :].partition_broadcast(128))
            P.barrier()
            P.op("vector", lambda e: e.tensor_copy(out=gt1b[:], in_=gtmp[:, 0, :]), writes=[t_c])
            P.op("vector", lambda e: e.tensor_copy(out=gt2b[:], in_=gtmp[:, 1, :]), writes=[t_c])
            P.op("vector", lambda e: e.tensor_copy(out=identb[:], in_=identf[:]), writes=[t_c])
            P.op("vector", lambda e: e.scalar_tensor_tensor(out=A2b[:], in0=A2b[:], scalar=1.0, in1=gfb[:], op0=ALU.add, op1=ALU.mult), writes=[t_c])
            P.barrier()
            st3.close()
            wsr = Ring(P, st2, nc, "wso", 2, [128, 16, 256], F32)
            for g in range(8):
                wt, wtok, wsem = wsr.next()
                P.op("sync", lambda e, wt=wt, g=g: e.dma_start(out=wt[:], in_=w_out[:, g * 256:(g + 1) * 256].rearrange("(k p) n -> p k n", p=128)), writes=[wtok], dma_sem=wsem)
                P.op("vector", lambda e, wt=wt, g=g: e.tensor_copy(out=wo[:, :, g * 256:(g + 1) * 256], in_=wt[:]), reads=[wtok], writes=[t_wo])
            for wi, (src, dst) in enumerate([(wsg, wgb), (wsu, wub)]):
                for g in range(2):
                    wt, wtok, wsem = wsr.next()
                    P.op("sync", lambda e, wt=wt, g=g, src=src: e.dma_start(out=wt[:], in_=src[:, g * 256:(g + 1) * 256].rearrange("(k p) n -> p k n", p=128)), writes=[wtok], dma_sem=wsem)
                    P.op("vector", lambda e, wt=wt, g=g, dst=dst: e.tensor_copy(out=dst[:, :, g * 256:(g + 1) * 256], in_=wt[:]), reads=[wtok], writes=[t_wo])
            for fc in range(4):
                wt, wtok, wsem = wsr.next()
                P.op("sync", lambda e, wt=wt, fc=fc: e.dma_start(out=wt[:].rearrange("p k n -> p (k n)")[:, 0:D], in_=wsd[fc * 128:(fc + 1) * 128, :]), writes=[wtok], dma_sem=wsem)
                P.op("vector", lambda e, wt=wt, fc=fc: e.tensor_copy(out=wdb[:, fc, :], in_=wt[:].rearrange("p k n -> p (k n)")[:, 0:D]), reads=[wtok], writes=[t_wo])
            P.barrier()
        cbr = Ring(P, st, nc, "cb", 1, [128, D], BF16)
        ctr = Ring(P, st, nc, "ct", 1, [128, 16, 128], BF16)
        x1r = Ring(P, st, nc, "x1", 1, [128, D], F32)
        h2r = Ring(P, st, nc, "h2f", 1, [128, D], F32)
        h2Tr = Ring(P, st, nc, "h2T", 1, [128, 16, 128], F32)
        gtr = Ring(P, st, nc, "gt", 2, [128, NE], F32)
        hTr = Ring(P, st, nc, "hTs", 1, [128, 4, 128], BF16)
        st_ = sb("st_", [128, 8])
        scs = sb("scs", [128, NE])
        bis = sb("bis", [128, NE])
        mskd = sb("mskd", [128, NE])
        m8 = sb("m8", [128, 8, 8])
        gs = sb("gs", [128, 8])
        gm8 = sb("gm8", [128, 8])
        pen = sb("pen", [128, 8])
        t8 = sb("t8", [128, 8])
        sgl = sb("sgl", [128, 512])
        t_r = Tok("route")
        t_junk = Tok("junk")
        t_sgl = Tok("sgl")
        for i in range(16):
            rsl = slice(i * 128, (i + 1) * 128)
            cb_, cbtok, cbsem = cbr.next()
            h2b, h2btok, h2bsem = cb_, cbtok, cbsem
            junk = cb_
            cT, cTtok, _ = ctr.next()
            h2Tb, h2Tbtok = cT, cTtok
            x1, x1tok, x1sem = x1r.next()
            P.op("sync", lambda e, x1=x1, rsl=rsl: e.dma_start(out=x1[:], in_=xo[rsl, :]), writes=[x1tok], dma_sem=x1sem)
            h2, h2tok, _ = h2r.next()
            h2T, h2Ttok, _ = h2Tr.next()
            gt, gttok, gtsem = gtr.next()
            hT, hTtok, _ = hTr.next()
            P.op("sync", lambda e, cb_=cb_, rsl=rsl: e.dma_start(out=cb_[:], in_=cat[rsl, :]), writes=[cbtok], dma_sem=cbsem)
            for half in range(2):
                pb = half
                for c8 in range(8):
                    c = half * 8 + c8
                    P.op("tensor", lambda e, cb_=cb_, c=c, c8=c8, pb=pb: e.transpose(out=ps[pb][:].bitcast(BF16)[:, c8 * 128:(c8 + 1) * 128], in_=cb_[:, c * 128:(c + 1) * 128], identity=identb[:]),
                         reads=[cbtok], writes=[pst[pb]])
                P.op("scalar", lambda e, cT=cT, half=half, pb=pb: e.activation(out=cT[:, half * 8:(half + 1) * 8, :], in_=ps[pb][:].bitcast(BF16).rearrange("p (c t) -> p c t", c=8), func=AF.Copy),
                     reads=[pst[pb]], writes=[cTtok])
            for cbk in range(4):
                pb = 2 + cbk
                csl = slice(cbk * 512, (cbk + 1) * 512)
                for k in range(16):
                    P.op("tensor", lambda e, cT=cT, k=k, csl=csl, pb=pb: e.matmul(out=ps[pb][:, :], lhsT=cT[:, k, :], rhs=wo[:, k, csl], start=(k == 0), stop=(k == 15)),
                         reads=[cTtok, t_wo], writes=[pst[pb]])
                P.op("vector", lambda e, h2=h2, pb=pb, csl=csl: e.tensor_tensor(out=h2[:, csl], in0=ps[pb][:, :], in1=gt1b[:, csl], op=ALU.mult), reads=[pst[pb]], writes=[h2tok])
                P.op("gpsimd", lambda e, x1=x1, h2=h2, csl=csl: e.tensor_tensor(out=x1[:, csl], in0=x1[:, csl], in1=h2[:, csl], op=ALU.add), reads=[h2tok], writes=[x1tok])
            P.op("scalar", lambda e, x1=x1: e.activation(out=junk[:], in_=x1[:], func=AF.Square, accum_out=st_[:, 0:1]), reads=[x1tok], writes=[cbtok, t_r])
            P.op("vector", lambda e: e.tensor_scalar(out=st_[:, 0:1], in0=st_[:, 0:1], scalar1=1.0 / D, scalar2=EPS, op0=ALU.mult, op1=ALU.add), writes=[t_r])
            P.op("scalar", lambda e: e.activation(out=st_[:, 0:1], in_=st_[:, 0:1], func=AF.Sqrt), writes=[t_r])
            P.op("vector", lambda e: e.reciprocal(out=st_[:, 1:2], in_=st_[:, 0:1]), writes=[t_r])
            P.op("vector", lambda e, x1=x1, h2=h2: e.scalar_tensor_tensor(out=h2[:], in0=x1[:], scalar=st_[:, 1:2], in1=A2b[:], op0=ALU.mult, op1=ALU.mult), reads=[x1tok, t_r], writes=[h2tok])
            P.op("gpsimd", lambda e, h2=h2: e.tensor_tensor(out=h2[:], in0=h2[:], in1=sh2b[:], op=ALU.add), writes=[h2tok])
            P.op("scalar", lambda e, h2=h2, h2b=h2b: e.activation(out=h2b[:], in_=h2[:], func=AF.Copy), reads=[h2tok], writes=[h2btok])
            P.op("sync", lambda e, h2b=h2b, rsl=rsl: e.dma_start(out=h2_o[rsl, :], in_=h2b[:]), reads=[h2btok], dma_sem=h2bsem)
            for q4 in range(4):
                pb = 2 + q4
                for c4 in range(4):
                    c = q4 * 4 + c4
                    P.op("tensor", lambda e, h2=h2, c=c, c4=c4, pb=pb: e.transpose(out=ps[pb][:, c4 * 128:(c4 + 1) * 128], in_=h2[:, c * 128:(c + 1) * 128], identity=identf[:]),
                         reads=[h2tok], writes=[pst[pb]])
                eng = "vector" if q4 % 2 == 0 else "scalar"
                if eng == "vector":
                    P.op("vector", lambda e, h2T=h2T, q4=q4, pb=pb: e.tensor_copy(out=h2T[:, q4 * 4:(q4 + 1) * 4, :], in_=ps[pb][:, :].rearrange("p (c t) -> p c t", c=4)), reads=[pst[pb]], writes=[h2Ttok])
                else:
                    P.op("scalar", lambda e, h2T=h2T, q4=q4, pb=pb: e.activation(out=h2T[:, q4 * 4:(q4 + 1) * 4, :], in_=ps[pb][:, :].rearrange("p (c t) -> p c t", c=4), func=AF.Copy), reads=[pst[pb]], writes=[h2Ttok])
            P.op("gpsimd", lambda e, h2T=h2T, h2Tb=h2Tb: e.tensor_copy(out=h2Tb[:], in_=h2T[:]), reads=[h2Ttok], writes=[h2Tbtok])
            for k in range(16):
                P.op("tensor", lambda e, h2T=h2T, k=k: e.matmul(out=ps[6][:, 0:NE], lhsT=h2T[:, k, :], rhs=wr[:, k, :], start=(k == 0), stop=(k == 15)), reads=[h2Ttok], writes=[pst[6]])
            P.op("scalar", lambda e: e.activation(out=scs[:], in_=ps[6][:, 0:NE], func=AF.Sigmoid), reads=[pst[6]], writes=[t_r])
            P.op("vector", lambda e: e.tensor_tensor(out=bis[:], in0=scs[:], in1=rbb[:], op=ALU.add), writes=[t_r])
            for g in range(8):
                P.op("vector", lambda e, g=g: e.max(out=m8[:, g, :], in_=bis[:, g * 32:(g + 1) * 32]), writes=[t_r])
            P.op("vector", lambda e: e.tensor_tensor(out=gs[:], in0=m8[:, :, 0], in1=m8[:, :, 1], op=ALU.add), writes=[t_r])
            P.op("vector", lambda e: e.max(out=gm8[:], in_=gs[:]), writes=[t_r])
            P.op("vector", lambda e: e.tensor_scalar(out=pen[:], in0=gs[:], scalar1=gm8[:, 3:4], scalar2=None, op0=ALU.is_ge), writes=[t_r])
            P.op("vector", lambda e: e.tensor_scalar(out=pen[:], in0=pen[:], scalar1=BIG, scalar2=-BIG, op0=ALU.mult, op1=ALU.add), writes=[t_r])
            for g in range(8):
                P.op("vector", lambda e, g=g: e.tensor_scalar(out=mskd[:, g * 32:(g + 1) * 32], in0=bis[:, g * 32:(g + 1) * 32], scalar1=pen[:, g:g + 1], scalar2=None, op0=ALU.add), writes=[t_r])
            P.op("vector", lambda e: e.max(out=t8[:], in_=mskd[:]), writes=[t_r])
            P.op("vector", lambda e: e.max_index(out=idx8[:], in_max=t8[:], in_values=mskd[:]), writes=[t_r])
            P.op("vector", lambda e: e.tensor_copy(out=idxf[:], in_=idx8[:]), writes=[t_r])
            P.op("vector", lambda e: e.tensor_scalar(out=mskd[:], in0=mskd[:], scalar1=t8[:, 7:8], scalar2=None, op0=ALU.is_ge), writes=[t_r])
            P.op("vector", lambda e: e.tensor_copy(out=selb[:], in_=mskd[:]), writes=[t_r])
            P.op("tensor", lambda e: e.matmul(out=ps[7][:, 0:NE], lhsT=trib[:], rhs=selb[:], start=True, stop=False), reads=[t_r], writes=[pst[7]])
            P.op("tensor", lambda e: e.matmul(out=ps[7][:, 0:NE], lhsT=onesb_[:], rhs=selsum[:], start=False, stop=True), reads=[t_r], writes=[pst[7]])
            P.op("vector", lambda e: e.tensor_copy(out=posd[:], in_=ps[7][:, 0:NE]), reads=[pst[7]], writes=[t_r])
            P.op("vector", lambda e: e.tensor_tensor(out=selsum[:], in0=selsum[:], in1=selb[:], op=ALU.add), writes=[t_r])
            P.op("vector", lambda e: e.tensor_tensor(out=mskd[:], in0=mskd[:], in1=scs[:], op=ALU.mult), writes=[t_r])
            P.op("vector", lambda e: e.tensor_reduce(out=st_[:, 2:3], in_=mskd[:], axis=AX.X, op=ALU.add), writes=[t_r])
            P.op("vector", lambda e: e.reciprocal(out=st_[:, 3:4], in_=st_[:, 2:3]), writes=[t_r])
            P.op("vector", lambda e, gt=gt: e.tensor_scalar(out=gt[:], in0=mskd[:], scalar1=st_[:, 3:4], scalar2=2.5, op0=ALU.mult, op1=ALU.mult), reads=[t_r], writes=[gttok])
            P.op("sync", lambda e, gt=gt, rsl=rsl: e.dma_start(out=gates_o[rsl, :], in_=gt[:]), reads=[gttok], dma_sem=gtsem)
            for k in range(8):
                P.op("vector", lambda e, gt=gt, k=k, i=i: e.scalar_tensor_tensor(out=jk[:], in0=iob[:], scalar=idxf[:, k:k + 1], in1=gt[:], op0=ALU.is_equal, op1=ALU.mult, accum_out=gk_[:, i, k:k + 1]),
                     reads=[gttok], writes=[t_r])
                P.op("vector", lambda e, k=k: e.scalar_tensor_tensor(out=jk[:], in0=iob[:], scalar=idxf[:, k:k + 1], in1=posd[:], op0=ALU.is_equal, op1=ALU.mult, accum_out=pk_[:, k:k + 1]), writes=[t_r])
            P.op("vector", lambda e, i=i: e.scalar_tensor_tensor(out=dk_[:, i, :], in0=idxf[:], scalar=float(ESLOTS), in1=pk_[:], op0=ALU.mult, op1=ALU.add), writes=[t_r])
            P.op("vector", lambda e, i=i: e.tensor_scalar(out=dk_[:, i, :], in0=dk_[:, i, :], scalar1=srct[:, 0:1], scalar2=None, op0=ALU.add), writes=[t_r])
            P.op("vector", lambda e: e.tensor_scalar(out=pk_[:], in0=pk_[:], scalar1=float(CAPC), scalar2=-2.0e6, op0=ALU.is_ge, op1=ALU.mult), writes=[t_r])
            P.op("vector", lambda e, i=i: e.tensor_tensor(out=dk_[:, i, :], in0=dk_[:, i, :], in1=pk_[:], op=ALU.add), writes=[t_r])
            for wi, (wsrc, pb) in enumerate([(wgb, 6), (wub, 7)]):
                for ft in range(4):
                    for k in range(16):
                        P.op("tensor", lambda e, h2Tb=h2Tb, wsrc=wsrc, ft=ft, k=k, pb=pb: e.matmul(out=ps[pb][:, ft * 128:(ft + 1) * 128], lhsT=wsrc[:, k, ft * 128:(ft + 1) * 128], rhs=h2Tb[:, k, :],
                                                                                                 start=(k == 0), stop=(k == 15)), reads=[h2Tbtok, t_wo], writes=[pst[pb]])
            P.op("scalar", lambda e: e.activation(out=sgl[:], in_=ps[6][:, :], func=AF.Silu), reads=[pst[6]], writes=[t_sgl])
            P.op("vector", lambda e, hT=hT: e.tensor_tensor(out=hT[:].rearrange("p f t -> p (f t)"), in0=sgl[:], in1=ps[7][:, :], op=ALU.mult), reads=[pst[7], t_sgl], writes=[hTtok])
            for cbk in range(4):
                pb = 2 + cbk
                csl = slice(cbk * 512, (cbk + 1) * 512)
                for fc in range(4):
                    P.op("tensor", lambda e, hT=hT, fc=fc, csl=csl, pb=pb: e.matmul(out=ps[pb][:, :], lhsT=hT[:, fc, :], rhs=wdb[:, fc, csl], start=(fc == 0), stop=(fc == 3)),
                         reads=[hTtok, t_wo], writes=[pst[pb]])
                P.op("vector", lambda e, h2=h2, pb=pb, csl=csl: e.tensor_tensor(out=h2[:, csl], in0=ps[pb][:, :], in1=gt2b[:, csl], op=ALU.mult), reads=[pst[pb]], writes=[h2tok])
                P.op("gpsimd", lambda e, x1=x1, h2=h2, csl=csl: e.tensor_tensor(out=x1[:, csl], in0=x1[:, csl], in1=h2[:, csl], op=ALU.add), reads=[h2tok], writes=[x1tok])
            P.op("sync", lambda e, x1=x1, rsl=rsl: e.dma_start(out=x1s_o[rsl, :], in_=x1[:]), reads=[x1tok], dma_sem=x1sem)
        sK = P.dsem("q_dk")
        P.op("sync", lambda e: e.dma_start(out=destk_o.rearrange("(i p) k -> p i k", p=128), in_=dk_[:]), reads=[t_r], dma_sem=sK)
        P.op("sync", lambda e: e.dma_start(out=gk_o.rearrange("(i p) k -> p i k", p=128), in_=gk_[:]), reads=[t_r], dma_sem=sK)
        P.barrier()
        P.emit()
    return nc


NTOK = 2 * S
NEL = 32


def build_l2b_dense(nblk=NTOK // 512, nel=NEL):
    nc = bass.Bass("TRN2", target_bir_lowering=False)
    din = lambda n, s, d=F32: nc.dram_tensor(n, s, d, kind="ExternalInput").ap()
    h2a = din("h2a", [NTOK, D], BF16)
    gto = din("gto", [NTOK, NEL])
    wg = din("wg", [NEL, D, FF])
    wu = din("wu", [NEL, D, FF])
    wd = din("wd", [NEL, FF, D])
    ident = din("ident", [128, 128])
    part = nc.dram_tensor("part", [NTOK, D], F32, kind="ExternalOutput").ap()
    wgb_d = nc.dram_tensor("wgb_d", [NEL, 128, 16, FF], BF16).ap()
    wub_d = nc.dram_tensor("wub_d", [NEL, 128, 16, FF], BF16).ap()
    wdb_d = nc.dram_tensor("wdb_d", [NEL, 128, 4, D], BF16).ap()
    with contextlib.ExitStack() as st:
        P = Prog(nc, st)
        ps = [st.enter_context(nc.psum_tensor("ps%d" % i, [128, 512], F32)) for i in range(8)]
        pst = [Tok("ps%d" % i) for i in range(8)]
        sb = lambda n, s, d=F32: st.enter_context(nc.sbuf_tensor(n, s, d))
        identf = sb("identf", [128, 128])
        identb = sb("identb", [128, 128], BF16)
        t_c = Tok("c")
        P.op("sync", lambda e: e.dma_start(out=identf[:], in_=ident[:, :]), writes=[t_c], dma_sem=P.dsem("q_c"))
        P.op("vector", lambda e: e.tensor_copy(out=identb[:], in_=identf[:]), reads=[t_c], writes=[t_c])
        t_wd = [[Tok("wd") for _ in range(3)] for _ in range(nel)]
        with contextlib.ExitStack() as st2:
            stg = Ring(P, st2, nc, "stg", 3, [128, 8192], F32)
            cst = Ring(P, st2, nc, "cst", 3, [128, 8192], BF16)
            ci = 0
            for e_ in range(nel):
                for mi in range(3):
                    sg_, sgtok, sgsem = stg.next()
                    cb_, cbtok, cbsem = cst.next()
                    if mi < 2:
                        src = (wg, wu)[mi][e_].rearrange("(k p) f -> p k f", p=128)
                        dst = (wgb_d, wub_d)[mi][e_]
                        view = lambda t: t[:].rearrange("p (k f) -> p k f", k=16)
                    else:
                        src = wd[e_].rearrange("(k p) n -> p k n", p=128)
                        dst = wdb_d[e_]
                        view = lambda t: t[:].rearrange("p (k n) -> p k n", k=4)
                    P.op("sync", lambda e, sg_=sg_, src=src, view=view: e.dma_start(out=view(sg_), in_=src), writes=[sgtok], dma_sem=sgsem)
                    ceng = ("vector", "gpsimd", "scalar")[ci % 3]
                    ci += 1
                    if ceng == "scalar":
                        P.op("scalar", lambda e, sg_=sg_, cb_=cb_: e.activation(out=cb_[:], in_=sg_[:], func=AF.Copy), reads=[sgtok], writes=[cbtok])
                    else:
                        P.op(ceng, lambda e, sg_=sg_, cb_=cb_: e.tensor_copy(out=cb_[:], in_=sg_[:]), reads=[sgtok], writes=[cbtok])
                    P.op("sync", lambda e, cb_=cb_, dst=dst, view=view: e.dma_start(out=dst, in_=view(cb_)), reads=[cbtok], writes=[t_wd[e_][mi]], dma_sem=cbsem)
            P.barrier()
        h2r = Ring(P, st, nc, "h2t", 1, [128, 4, D], BF16)
        h2Tr = Ring(P, st, nc, "h2T", 2, [128, 16, 512], BF16)
        gr = Ring(P, st, nc, "gr", 2, [128, 4, NEL], F32)
        accr = Ring(P, st, nc, "acc", 1, [128, 4, D], F32)
        wgr = Ring(P, st, nc, "wgr", 2, [128, 16, FF], BF16)
        wur = Ring(P, st, nc, "wur", 2, [128, 16, FF], BF16)
        wdr = Ring(P, st, nc, "wdr", 2, [128, 4, D], BF16)
        hTr = Ring(P, st, nc, "hT", 2, [128, 4, 512], BF16)
        sglr = Ring(P, st, nc, "sgl", 2, [128, 512], F32)
        for nb in range(nblk):
            h2t, h2tok, h2sem = h2r.next()
            h2T, h2Ttok, _ = h2Tr.next()
            g_, gtok, gsem = gr.next()
            acc, acctok, accsem = accr.next()
            rows = slice(nb * 512, (nb + 1) * 512)
            P.op("sync", lambda e, h2t=h2t, rows=rows: e.dma_start(out=h2t[:], in_=h2a[rows, :].rearrange("(t p) d -> p t d", p=128)), writes=[h2tok], dma_sem=h2sem)
            P.op("sync", lambda e, g_=g_, rows=rows: e.dma_start(out=g_[:], in_=gto[rows, :].rearrange("(t p) n -> p t n", p=128)), writes=[gtok], dma_sem=gsem)
            P.op("gpsimd", lambda e, acc=acc: e.memset(acc[:], 0.0), writes=[acctok])
            for tt in range(4):
                for half in range(2):
                    pb = half
                    for c8 in range(8):
                        c = half * 8 + c8
                        P.op("tensor", lambda e, h2t=h2t, tt=tt, c=c, c8=c8, pb=pb: e.transpose(out=ps[pb][:].bitcast(BF16)[:, c8 * 128:(c8 + 1) * 128], in_=h2t[:, tt, c * 128:(c + 1) * 128], identity=identb[:]),
                             reads=[h2tok], writes=[pst[pb]])
                    P.op("scalar", lambda e, h2T=h2T, tt=tt, half=half, pb=pb: e.activation(out=h2T[:, half * 8:(half + 1) * 8, tt * 128:(tt + 1) * 128], in_=ps[pb][:].bitcast(BF16).rearrange("p (c t) -> p c t", c=8), func=AF.Copy),
                         reads=[pst[pb]], writes=[h2Ttok])
            for e_ in range(nel):
                wgt, wgtok, wgsem = wgr.next()
                wut, wutok, wusem = wur.next()
                wdt, wdtok, wdsem = wdr.next()
                P.op("sync", lambda e, wgt=wgt, e_=e_: e.dma_start(out=wgt[:], in_=wgb_d[e_]), reads=[t_wd[e_][0]], writes=[wgtok], dma_sem=wgsem)
                P.op("sync", lambda e, wut=wut, e_=e_: e.dma_start(out=wut[:], in_=wub_d[e_]), reads=[t_wd[e_][1]], writes=[wutok], dma_sem=wusem)
                P.op("sync", lambda e, wdt=wdt, e_=e_: e.dma_start(out=wdt[:], in_=wdb_d[e_]), reads=[t_wd[e_][2]], writes=[wdtok], dma_sem=wdsem)
                hT, hTtok, _ = hTr.next()
                for ft in range(4):
                    pg, pu = 2 + (ft % 2) * 2, 3 + (ft % 2) * 2
                    fsl = slice(ft * 128, (ft + 1) * 128)
                    for k in range(16):
                        P.op("tensor", lambda e, wgt=wgt, h2T=h2T, k=k, fsl=fsl, pg=pg: e.matmul(out=ps[pg][:, :], lhsT=wgt[:, k, fsl], rhs=h2T[:, k, :], start=(k == 0), stop=(k == 15)),
                             reads=[wgtok, h2Ttok], writes=[pst[pg]])
                    for k in range(16):
                        P.op("tensor", lambda e, wut=wut, h2T=h2T, k=k, fsl=fsl, pu=pu: e.matmul(out=ps[pu][:, :], lhsT=wut[:, k, fsl], rhs=h2T[:, k, :], start=(k == 0), stop=(k == 15)),
                             reads=[wutok, h2Ttok], writes=[pst[pu]])
                    sgl, sgltok, _ = sglr.next()
                    P.op("scalar", lambda e, sgl=sgl, pg=pg: e.activation(out=sgl[:], in_=ps[pg][:, :], func=AF.Silu), reads=[pst[pg]], writes=[sgltok])
                    P.op("vector", lambda e, hT=hT, ft=ft, sgl=sgl, pu=pu: e.tensor_tensor(out=hT[:, ft, :], in0=sgl[:], in1=ps[pu][:, :], op=ALU.mult), reads=[pst[pu], sgltok], writes=[hTtok])
                for tt in range(4):
                    for cbk in range(4):
                        pb = 6 + ((tt * 4 + cbk) % 2)
                        csl = slice(cbk * 512, (cbk + 1) * 512)
                        for fc in range(4):
                            P.op("tensor", lambda e, hT=hT, wdt=wdt, tt=tt, fc=fc, csl=csl, pb=pb: e.matmul(out=ps[pb][:, :], lhsT=hT[:, fc, tt * 128:(tt + 1) * 128], rhs=wdt[:, fc, csl], start=(fc == 0), stop=(fc == 3)),
                                 reads=[hTtok, wdtok], writes=[pst[pb]])
                        P.op("vector", lambda e, acc=acc, g_=g_, tt=tt, csl=csl, pb=pb, e_=e_: e.scalar_tensor_tensor(out=acc[:, tt, csl], in0=ps[pb][:, :], scalar=g_[:, tt, e_:e_ + 1], in1=acc[:, tt, csl], op0=ALU.mult, op1=ALU.add),
                             reads=[pst[pb], gtok], writes=[acctok])
            P.op("sync", lambda e, acc=acc, rows=rows: e.dma_start(out=part[rows, :].rearrange("(t p) d -> p t d", p=128), in_=acc[:]), reads=[acctok], dma_sem=accsem)
        P.barrier()
        P.emit()
    return nc


NOWN = NEL * ESLOTS
ZROW = NOWN + 128


def build_l2b(ntile=NTOK // 128, nel=NEL):
    nc = bass.Bass("TRN2", target_bir_lowering=False)
    din = lambda n, s, d=F32: nc.dram_tensor(n, s, d, kind="ExternalInput").ap()
    h2a = din("h2a", [NTOK, D], BF16)
    destk = din("destk", [NTOK, 8])
    gkin = din("gkin", [NTOK, 8])
    lohi = din("lohi", [128, 4])
    wg = din("wg", [NEL, D, FF])
    wu = din("wu", [NEL, D, FF])
    wd = din("wd", [NEL, FF, D])
    ident = din("ident", [128, 128])
    part = nc.dram_tensor("part", [NTOK, D], F32, kind="ExternalOutput").ap()
    wgb_d = nc.dram_tensor("wgb_d", [NEL, 128, 16, FF], BF16).ap()
    wub_d = nc.dram_tensor("wub_d", [NEL, 128, 16, FF], BF16).ap()
    wdb_d = nc.dram_tensor("wdb_d", [NEL, 128, 4, D], BF16).ap()
    xe_d = nc.dram_tensor("xe_d", [NOWN + 256, D], BF16).ap()
    y_d = nc.dram_tensor("y_d", [NOWN + 256, D], BF16).ap()
    NT = NTOK // 128
    with contextlib.ExitStack() as st:
        P = Prog(nc, st)
        ps = [st.enter_context(nc.psum_tensor("ps%d" % i, [128, 512], F32)) for i in range(8)]
        pst = [Tok("ps%d" % i) for i in range(8)]
        sb = lambda n, s, d=F32: st.enter_context(nc.sbuf_tensor(n, s, d))
        identf = sb("identf", [128, 128])
        identb = sb("identb", [128, 128], BF16)
        lh = sb("lh", [128, 4])
        sidx = sb("sidx", [128, NT * 8], I32)
        gidx = sb("gidx", [128, NT * 8], I32)
        wk = sb("wk", [128, NT * 8])
        t_c = Tok("c")
        cs = P.dsem("q_c")
        P.op("sync", lambda e: e.dma_start(out=identf[:], in_=ident[:, :]), writes=[t_c], dma_sem=cs)
        P.op("sync", lambda e: e.dma_start(out=lh[:], in_=lohi[:, :]), writes=[t_c], dma_sem=cs)
        t_idx = Tok("idx")
        with contextlib.ExitStack() as st2:
            dall = st2.enter_context(nc.sbuf_tensor("dall", [128, NT * 8], F32))
            own = st2.enter_context(nc.sbuf_tensor("own", [128, NT * 8], F32))
            tmp = st2.enter_context(nc.sbuf_tensor("tmpi", [128, NT * 8], F32))
            P.op("sync", lambda e: e.dma_start(out=dall[:].rearrange("p (i k) -> p i k", k=8), in_=destk.rearrange("(i p) k -> p i k", p=128)), writes=[t_c], dma_sem=cs)
            P.op("sync", lambda e: e.dma_start(out=wk[:].rearrange("p (i k) -> p i k", k=8), in_=gkin.rearrange("(i p) k -> p i k", p=128)), writes=[t_c], dma_sem=cs)
            P.barrier()
            P.op("vector", lambda e: e.tensor_copy(out=identb[:], in_=identf[:]), writes=[t_idx])
            V = lambda fn: P.op("vector", fn, writes=[t_idx])
            V(lambda e: e.tensor_scalar(out=own[:], in0=dall[:], scalar1=lh[:, 0:1], scalar2=None, op0=ALU.is_ge))
            V(lambda e: e.tensor_scalar(out=tmp[:], in0=dall[:], scalar1=lh[:, 1:2], scalar2=None, op0=ALU.is_lt))
            V(lambda e: e.tensor_tensor(out=own[:], in0=own[:], in1=tmp[:], op=ALU.mult))
            V(lambda e: e.tensor_tensor(out=wk[:], in0=wk[:], in1=own[:], op=ALU.mult))
            V(lambda e: e.tensor_scalar(out=dall[:], in0=dall[:], scalar1=lh[:, 0:1], scalar2=None, op0=ALU.subtract))
            V(lambda e: e.tensor_scalar(out=tmp[:], in0=dall[:], scalar1=lh[:, 2:3], scalar2=None, op0=ALU.subtract))
            V(lambda e: e.tensor_tensor(out=tmp[:], in0=tmp[:], in1=own[:], op=ALU.mult))
            V(lambda e: e.tensor_scalar(out=tmp[:], in0=tmp[:], scalar1=lh[:, 2:3], scalar2=None, op0=ALU.add))
            V(lambda e: e.tensor_copy(out=sidx[:], in_=tmp[:]))
            V(lambda e: e.tensor_scalar(out=tmp[:], in0=dall[:], scalar1=lh[:, 3:4], scalar2=None, op0=ALU.subtract))
            V(lambda e: e.tensor_tensor(out=tmp[:], in0=tmp[:], in1=own[:], op=ALU.mult))
            V(lambda e: e.tensor_scalar(out=tmp[:], in0=tmp[:], scalar1=lh[:, 3:4], scalar2=None, op0=ALU.add))
            V(lambda e: e.tensor_copy(out=gidx[:], in_=tmp[:]))
            P.barrier()
        t_wd = [[Tok("wd") for _ in range(3)] for _ in range(nel)]
        with contextlib.ExitStack() as st2:
            stg = Ring(P, st2, nc, "stg", 3, [128, 8192], F32)
            cst = Ring(P, st2, nc, "cst", 3, [128, 8192], BF16)
            hr = Ring(P, st2, nc, "hr", 3, [128, D], BF16)
            zt = st2.enter_context(nc.sbuf_tensor("zt", [128, D], BF16))
            P.op("vector", lambda e: e.memset(zt[:], 0.0), writes=[t_idx])
            P.op("sync", lambda e: e.dma_start(out=y_d[ZROW:ZROW + 128, :], in_=zt[:]), reads=[t_idx], dma_sem=P.dsem("q_z"))
            sc_sem = [P.dsem("q_sc%d" % i) for i in range(3)]
            ci = 0
            def disp(i):
                ht, htok, hsem = hr.next()
                P.op("sync", lambda e, ht=ht, i=i: e.dma_start(out=ht[:], in_=h2a[i * 128:(i + 1) * 128, :]), writes=[htok], dma_sem=hsem)
                for k in range(8):
                    col = i * 8 + k
                    P.op("gpsimd", lambda e, ht=ht, col=col: e.indirect_dma_start(out=xe_d[:, :], out_offset=bass.IndirectOffsetOnAxis(ap=sidx[:, col:col + 1], axis=0), in_=ht[:], in_offset=None),
                         reads=[htok, t_idx], dma_sem=sc_sem[hr.i])
            ncast = nel * 3
            dnext = 0
            for e_ in range(nel):
                for mi in range(3):
                    want = ((e_ * 3 + mi + 1) * ntile) // ncast
                    while dnext < want:
                        disp(dnext)
                        dnext += 1
                    sg_, sgtok, sgsem = stg.next()
                    cb_, cbtok, cbsem = cst.next()
                    if mi < 2:
                        src = (wg, wu)[mi][e_].rearrange("(k p) f -> p k f", p=128)
                        dst = (wgb_d, wub_d)[mi][e_]
                        view = lambda t: t[:].rearrange("p (k f) -> p k f", k=16)
                    else:
                        src = wd[e_].rearrange("(k p) n -> p k n", p=128)
                        dst = wdb_d[e_]
                        view = lambda t: t[:].rearrange("p (k n) -> p k n", k=4)
                    P.op("sync", lambda e, sg_=sg_, src=src, view=view: e.dma_start(out=view(sg_), in_=src), writes=[sgtok], dma_sem=sgsem)
                    ceng = ("vector", "scalar")[ci % 2]
                    ci += 1
                    if ceng == "scalar":
                        P.op("scalar", lambda e, sg_=sg_, cb_=cb_: e.activation(out=cb_[:], in_=sg_[:], func=AF.Copy), reads=[sgtok], writes=[cbtok])
                    else:
                        P.op(ceng, lambda e, sg_=sg_, cb_=cb_: e.tensor_copy(out=cb_[:], in_=sg_[:]), reads=[sgtok], writes=[cbtok])
                    P.op("sync", lambda e, cb_=cb_, dst=dst, view=view: e.dma_start(out=dst, in_=view(cb_)), reads=[cbtok], writes=[t_wd[e_][mi]], dma_sem=cbsem)
            P.barrier()
        with contextlib.ExitStack() as st2:
            h2r = Ring(P, st2, nc, "h2t", 1, [128, 4, D], BF16)
            h2Tr = Ring(P, st2, nc, "h2T", 2, [128, 16, 512], BF16)
            yr = Ring(P, st2, nc, "yt", 1, [128, 4, D], BF16)
            wgr = Ring(P, st2, nc, "wgr", 2, [128, 16, FF], BF16)
            wur = Ring(P, st2, nc, "wur", 2, [128, 16, FF], BF16)
            wdr = Ring(P, st2, nc, "wdr", 2, [128, 4, D], BF16)
            hTr = Ring(P, st2, nc, "hT", 2, [128, 4, 512], BF16)
            sglr = Ring(P, st2, nc, "sgl", 2, [128, 512], F32)
            nsb = ESLOTS // 512
            for e_ in range(nel):
                wgt, wgtok, wgsem = wgr.next()
                wut, wutok, wusem = wur.next()
                wdt, wdtok, wdsem = wdr.next()
                P.op("sync", lambda e, wgt=wgt, e_=e_: e.dma_start(out=wgt[:], in_=wgb_d[e_]), reads=[t_wd[e_][0]], writes=[wgtok], dma_sem=wgsem)
                P.op("sync", lambda e, wut=wut, e_=e_: e.dma_start(out=wut[:], in_=wub_d[e_]), reads=[t_wd[e_][1]], writes=[wutok], dma_sem=wusem)
                P.op("sync", lambda e, wdt=wdt, e_=e_: e.dma_start(out=wdt[:], in_=wdb_d[e_]), reads=[t_wd[e_][2]], writes=[wdtok], dma_sem=wdsem)
                for sb_ in range(nsb):
                    r0 = e_ * ESLOTS + sb_ * 512
                    h2t, h2tok, h2sem = h2r.next()
                    h2T, h2Ttok, _ = h2Tr.next()
                    yt, ytok, ysem = yr.next()
                    P.op("sync", lambda e, h2t=h2t, r0=r0: e.dma_start(out=h2t[:], in_=xe_d[r0:r0 + 512, :].rearrange("(t p) d -> p t d", p=128)), writes=[h2tok], dma_sem=h2sem)
                    for tt in range(4):
                        for half in range(2):
                            pb = half
                            for c8 in range(8):
                                c = half * 8 + c8
                                P.op("tensor", lambda e, h2t=h2t, tt=tt, c=c, c8=c8, pb=pb: e.transpose(out=ps[pb][:].bitcast(BF16)[:, c8 * 128:(c8 + 1) * 128], in_=h2t[:, tt, c * 128:(c + 1) * 128], identity=identb[:]),
                                     reads=[h2tok], writes=[pst[pb]])
                            P.op("scalar", lambda e, h2T=h2T, tt=tt, half=half, pb=pb: e.activation(out=h2T[:, half * 8:(half + 1) * 8, tt * 128:(tt + 1) * 128], in_=ps[pb][:].bitcast(BF16).rearrange("p (c t) -> p c t", c=8), func=AF.Copy),
                                 reads=[pst[pb]], writes=[h2Ttok])
                    hT, hTtok, _ = hTr.next()
                    for ft in range(4):
                        pg, pu = 2 + (ft % 2) * 2, 3 + (ft % 2) * 2
                        fsl = slice(ft * 128, (ft + 1) * 128)
                        for k in range(16):
                            P.op("tensor", lambda e, wgt=wgt, h2T=h2T, k=k, fsl=fsl, pg=pg: e.matmul(out=ps[pg][:, :], lhsT=wgt[:, k, fsl], rhs=h2T[:, k, :], start=(k == 0), stop=(k == 15)),
                                 reads=[wgtok, h2Ttok], writes=[pst[pg]])
                        for k in range(16):
                            P.op("tensor", lambda e, wut=wut, h2T=h2T, k=k, fsl=fsl, pu=pu: e.matmul(out=ps[pu][:, :], lhsT=wut[:, k, fsl], rhs=h2T[:, k, :], start=(k == 0), stop=(k == 15)),
                                 reads=[wutok, h2Ttok], writes=[pst[pu]])
                        sgl, sgltok, _ = sglr.next()
                        P.op("scalar", lambda e, sgl=sgl, pg=pg: e.activation(out=sgl[:], in_=ps[pg][:, :], func=AF.Silu), reads=[pst[pg]], writes=[sgltok])
                        P.op("vector", lambda e, hT=hT, ft=ft, sgl=sgl, pu=pu: e.tensor_tensor(out=hT[:, ft, :], in0=sgl[:], in1=ps[pu][:, :], op=ALU.mult), reads=[pst[pu], sgltok], writes=[hTtok])
                    for tt in range(4):
                        for cbk in range(4):
                            pb = 6 + ((tt * 4 + cbk) % 2)
                            csl = slice(cbk * 512, (cbk + 1) * 512)
                            for fc in range(4):
                                P.op("tensor", lambda e, hT=hT, wdt=wdt, tt=tt, fc=fc, csl=csl, pb=pb: e.matmul(out=ps[pb][:, :], lhsT=hT[:, fc, tt * 128:(tt + 1) * 128], rhs=wdt[:, fc, csl], start=(fc == 0), stop=(fc == 3)),
                                     reads=[hTtok, wdtok], writes=[pst[pb]])
                            if (tt * 4 + cbk) % 2 == 0:
                                P.op("vector", lambda e, yt=yt, tt=tt, csl=csl, pb=pb: e.tensor_copy(out=yt[:, tt, csl], in_=ps[pb][:, :]), reads=[pst[pb]], writes=[ytok])
                            else:
                                P.op("scalar", lambda e, yt=yt, tt=tt, csl=csl, pb=pb: e.activation(out=yt[:, tt, csl], in_=ps[pb][:, :], func=AF.Copy), reads=[pst[pb]], writes=[ytok])
                    P.op("sync", lambda e, yt=yt, r0=r0: e.dma_start(out=y_d[r0:r0 + 512, :].rearrange("(t p) d -> p t d", p=128), in_=yt[:]), reads=[ytok], dma_sem=ysem)
            P.barrier()
        with contextlib.ExitStack() as st2:
            gr_ = Ring(P, st2, nc, "gy", 6, [128, D], BF16)
            ar = Ring(P, st2, nc, "acc", 2, [128, D], F32)
            for i in range(ntile):
                acc, acctok, accsem = ar.next()
                for k in range(8):
                    col = i * 8 + k
                    yk, yktok, yksem = gr_.next()
                    P.op("gpsimd", lambda e, yk=yk, col=col: e.indirect_dma_start(out=yk[:], out_offset=None, in_=y_d[:, :], in_offset=bass.IndirectOffsetOnAxis(ap=gidx[:, col:col + 1], axis=0)),
                         writes=[yktok], dma_sem=yksem)
                    if k == 0:
                        P.op("vector", lambda e, acc=acc, yk=yk, col=col: e.tensor_scalar(out=acc[:], in0=yk[:], scalar1=wk[:, col:col + 1], scalar2=None, op0=ALU.mult), reads=[yktok], writes=[acctok])
                    else:
                        P.op("vector", lambda e, acc=acc, yk=yk, col=col: e.scalar_tensor_tensor(out=acc[:], in0=yk[:], scalar=wk[:, col:col + 1], in1=acc[:], op0=ALU.mult, op1=ALU.add), reads=[yktok], writes=[acctok])
                P.op("sync", lambda e, acc=acc, i=i: e.dma_start(out=part[i * 128:(i + 1) * 128, :], in_=acc[:]), reads=[acctok], dma_sem=accsem)
            P.barrier()
        P.emit()
    return nc


def build_l3():
    nc = bass.Bass("TRN2", target_bir_lowering=False)
    din = lambda n, s, d=F32: nc.dram_tensor(n, s, d, kind="ExternalInput").ap()
    x1s = din("x1s", [2048, D])
    parts = din("parts", [8, 2048, D])
    gt2 = din("gt2", [1, D])
    out = nc.dram_tensor("out", [2048, D], F32, kind="ExternalOutput").ap()
    with contextlib.ExitStack() as st:
        P = Prog(nc, st)
        sb = lambda n, s, d=F32: st.enter_context(nc.sbuf_tensor(n, s, d))
        gt2b = sb("gt2b", [128, D])
        t_c = Tok("c")
        P.op("sync", lambda e: e.dma_start(out=gt2b[:], in_=gt2[0:1, :].partition_broadcast(128)), writes=[t_c], dma_sem=P.dsem("q_c"))
        pr = Ring(P, st, nc, "pr", 4, [128, D], F32)
        xr = Ring(P, st, nc, "xr", 2, [128, D], F32)
        ar = Ring(P, st, nc, "ar", 2, [128, D], F32)
        for i in range(16):
            rsl = slice(i * 128, (i + 1) * 128)
            xt, xtok, xsem = xr.next()
            acc, acctok, accsem = ar.next()
            P.op("sync", lambda e, xt=xt, rsl=rsl: e.dma_start(out=xt[:], in_=x1s[rsl, :]), writes=[xtok], dma_sem=xsem)
            for c in range(8):
                pt, ptok, psem = pr.next()
                P.op("sync", lambda e, pt=pt, c=c, rsl=rsl: e.dma_start(out=pt[:], in_=parts[c, rsl, :]), writes=[ptok], dma_sem=psem)
                eng = "vector" if c % 2 == 0 else "gpsimd"
                if c == 0:
                    P.op("vector", lambda e, acc=acc, pt=pt: e.tensor_copy(out=acc[:], in_=pt[:]), reads=[ptok], writes=[acctok])
                else:
                    P.op(eng, lambda e, acc=acc, pt=pt: e.tensor_tensor(out=acc[:], in0=acc[:], in1=pt[:], op=ALU.add), reads=[ptok], writes=[acctok])
            P.op("vector", lambda e, acc=acc: e.tensor_tensor(out=acc[:], in0=acc[:], in1=gt2b[:], op=ALU.mult), reads=[t_c], writes=[acctok])
            P.op("gpsimd", lambda e, acc=acc, xt=xt: e.tensor_tensor(out=acc[:], in0=acc[:], in1=xt[:], op=ALU.add), reads=[xtok], writes=[acctok])
            P.op("sync", lambda e, acc=acc, rsl=rsl: e.dma_start(out=out[rsl, :], in_=acc[:]), reads=[acctok], dma_sem=accsem)
        P.barrier()
        P.emit()
    return nc


def _run(nc, maps):
    return run_bass_kernel_spmd(nc, maps, core_ids=list(range(len(maps)))).results


def run_l1(inputs):
    r1 = _run(build_l1(), [l1_inputs(inputs, c) for c in range(8)])
    cat = np.zeros((2, S, D), r1[0]["mix"].dtype)
    for c in range(8):
        b, j = c // 4, c % 4
        m = r1[c]["mix"]
        cat[b, :, j * 256:(j + 1) * 256] = m[:, 0:256]
        cat[b, :, 1024 + j * 256:1024 + (j + 1) * 256] = m[:, 256:512]
    mods = [r1[0]["modrow"].reshape(-1), r1[4]["modrow"].reshape(-1)]
    return cat, mods


def run_l2a(inputs, cat, mods):
    f = lambda a: np.ascontiguousarray(a, dtype=np.float32)
    ident = np.eye(128, dtype=np.float32)
    maps = []
    for c in range(8):
        b, r = c // 4, c % 4
        mod = mods[b]
        rows = np.stack([mod[2 * D:3 * D], mod[4 * D:5 * D], mod[3 * D:4 * D], mod[5 * D:6 * D], inputs["g_ffn"][0], np.zeros(D, np.float32)])
        maps.append({
            "xo": f(inputs["x"][b, r * 2048:(r + 1) * 2048]), "cat": np.ascontiguousarray(cat[b, r * 2048:(r + 1) * 2048]),
            "w_out": f(inputs["w_out"][0]), "rows": f(rows), "w_router": f(inputs["w_router"][0]),
            "rbias": f(inputs["router_bias"][0].reshape(1, NE)), "wsg": f(inputs["w_sh_gate"][0]), "wsu": f(inputs["w_sh_up"][0]),
            "wsd": f(inputs["w_sh_down"][0]), "ident": ident,
            "tris": np.triu(np.ones((128, 128), np.float32), 1), "iotae": np.arange(NE, dtype=np.float32).reshape(1, NE),
            "srcoff": np.full((128, 1), c * CAPC, np.float32),
        })
    r = _run(build_l2a(), maps)
    x1s = np.concatenate([r[c]["x1s"] for c in range(8)], 0)
    h2 = np.concatenate([r[c]["h2"] for c in range(8)], 0)
    gates = np.concatenate([r[c]["gates"] for c in range(8)], 0)
    destk = np.concatenate([r[c]["destk"] for c in range(8)], 0)
    gk = np.concatenate([r[c]["gk"] for c in range(8)], 0)
    return x1s, h2, gates, destk, gk


def l2b_maps(inputs, h2, destk, gk, cores=range(8)):
    f = lambda a: np.ascontiguousarray(a, dtype=np.float32)
    ident = np.eye(128, dtype=np.float32)
    maps = []
    for c in cores:
        es = slice(c * NEL, (c + 1) * NEL)
        lohi = np.zeros((128, 4), np.float32)
        lohi[:, 0] = c * NOWN
        lohi[:, 1] = (c + 1) * NOWN
        lohi[:, 2] = NOWN + np.arange(128)
        lohi[:, 3] = ZROW
        maps.append({"h2a": h2, "destk": f(destk), "gkin": f(gk), "lohi": lohi, "wg": f(inputs["w_exp_gate"][0, es]), "wu": f(inputs["w_exp_up"][0, es]),
                     "wd": f(inputs["w_exp_down"][0, es]), "ident": ident})
    return maps


def run_l2b(inputs, h2, destk, gk):
    r = _run(build_l2b(), l2b_maps(inputs, h2, destk, gk))
    return [r[c]["part"] for c in range(8)]


def run_l3(x1s, parts, mods):
    maps = []
    for c in range(8):
        b = c // 4
        rs = slice(c * 2048, (c + 1) * 2048)
        maps.append({"x1s": np.ascontiguousarray(x1s[rs]), "parts": np.ascontiguousarray(np.stack([p[rs] for p in parts])),
                     "gt2": np.ascontiguousarray(mods[b][5 * D:6 * D].reshape(1, D))})
    r = _run(build_l3(), maps)
    return np.concatenate([r[c]["out"] for c in range(8)], 0).reshape(2, S, D)


def kernel(**inputs):
    cat, mods = run_l1(inputs)
    x1s, h2, gates, destk, gk = run_l2a(inputs, cat, mods)
    parts = run_l2b(inputs, h2, destk, gk)
    return run_l3(x1s, parts, mods)
```
